# Optimizing a Trainium2 kernel written in Bass

```python
import math
import jax, jax.numpy as jnp
from jax import lax
import numpy as np

D_MODEL = 2048
BATCH = 4
SEQ = 4096
DEPTH = 2

NORM_EPS = 1e-6
BLOCK = 128
ROPE_THETA = 10000.0

SSD_INNER = 2048
SSD_HEAD_DIM = 64
SSD_HEADS = SSD_INNER // SSD_HEAD_DIM
SSD_GROUPS = 4
SSD_REP = SSD_HEADS // SSD_GROUPS
SSD_STATE = 128
SSD_CONV = 4
SSD_CONV_CH = SSD_INNER + 2 * SSD_GROUPS * SSD_STATE

RET_HEADS = 8
RET_QK_DIM = 128
RET_V_DIM = 256
RET_QK = RET_HEADS * RET_QK_DIM
RET_V = RET_HEADS * RET_V_DIM

DIL_PATTERNS = ((128, 1), (512, 4), (2048, 16))
DIL_GROUPS = len(DIL_PATTERNS)
DIL_HEADS = 8
DIL_HEAD_DIM = 128
DIL_WIDTH = DIL_HEADS * DIL_HEAD_DIM

N_BRANCHES = 3

SPLITS = (
    SSD_INNER,
    SSD_CONV_CH,
    SSD_HEADS,
    RET_QK,
    RET_QK,
    RET_V,
    RET_V,
    DIL_GROUPS * 3 * DIL_WIDTH,
    DIL_WIDTH,
    N_BRANCHES * D_MODEL,
)
D_IN = sum(SPLITS)
SPLIT_POINTS = [int(v) for v in np.cumsum(SPLITS)[:-1]]

kernel_name = 'hybrid_ssd_retention_dilated_gated_merge'


def rms_norm(t, g):
    tf = t.astype(jnp.float32)
    tf = tf * lax.rsqrt(jnp.mean(tf * tf, axis=-1, keepdims=True) + NORM_EPS)
    return tf * g.astype(jnp.float32)


def rope_tables(s, dim):
    inv_freq = ROPE_THETA ** (-jnp.arange(0, dim, 2, dtype=jnp.float32) / dim)
    ang = jnp.arange(s, dtype=jnp.float32)[:, None] * inv_freq[None, :]
    return jnp.cos(ang), jnp.sin(ang)


def apply_rope(t, cos, sin):
    tf = t.astype(jnp.float32)
    half = tf.shape[-1] // 2
    t1, t2 = tf[..., :half], tf[..., half:]
    c = cos[None, :, None, :]
    s_ = sin[None, :, None, :]
    return jnp.concatenate([t1 * c - t2 * s_, t1 * s_ + t2 * c], axis=-1)


def causal_depthwise_conv(u, w, bias):
    c = u.shape[-1]
    out = lax.conv_general_dilated(
        u, w[:, None, :].astype(u.dtype), window_strides=(1,),
        padding=[(SSD_CONV - 1, 0)], dimension_numbers=('NWC', 'WIO', 'NWC'),
        feature_group_count=c)
    return out + bias.astype(u.dtype)


def ssd_chunked(xh, dt, a_neg, bm, cm):
    b, s, g, r, p = xh.shape
    n = bm.shape[-1]
    nc = s // BLOCK
    xc = xh.reshape(b, nc, BLOCK, g, r, p)
    dtc = dt.reshape(b, nc, BLOCK, g, r)
    bc = bm.reshape(b, nc, BLOCK, g, n)
    cc = cm.reshape(b, nc, BLOCK, g, n)
    acs = jnp.cumsum(dtc * a_neg, axis=2)
    causal = jnp.tril(jnp.ones((BLOCK, BLOCK), dtype=bool))
    seg = acs[:, :, :, None] - acs[:, :, None]
    decay = jnp.exp(jnp.where(causal[:, :, None, None], seg, -jnp.inf))
    cb = jnp.einsum('bclgn,bcsgn->bclsg', cc, bc)
    xdt = xc * dtc[..., None]
    y_diag = jnp.einsum('bclsgr,bcsgrp->bclgrp', cb[..., None] * decay, xdt)
    to_end = jnp.exp(acs[:, :, -1:] - acs)
    states = jnp.einsum('bcsgn,bcsgrp->bcgrpn', bc, xdt * to_end[..., None])
    chunk_decay = jnp.exp(acs[:, :, -1])

    def step(h, inp):
        st, dec = inp
        return h * dec[..., None, None] + st, h

    h0 = jnp.zeros((b, g, r, p, n), jnp.float32)
    _, h_in = lax.scan(step, h0, (jnp.moveaxis(states, 1, 0), jnp.moveaxis(chunk_decay, 1, 0)))
    h_in = jnp.moveaxis(h_in, 0, 1)
    y_off = jnp.einsum('bclgn,bcgrpn->bclgrp', cc, h_in) * jnp.exp(acs)[..., None]
    return (y_diag + y_off).reshape(b, s, g, r, p)


def ssd_branch(z, xbc, dt_raw, conv_w, conv_b, dt_bias, a_log, d_skip, norm_g):
    b, s, _ = xbc.shape
    xbc = jax.nn.silu(causal_depthwise_conv(xbc, conv_w, conv_b).astype(jnp.float32))
    xs = xbc[..., :SSD_INNER]
    bm = xbc[..., SSD_INNER:SSD_INNER + SSD_GROUPS * SSD_STATE].reshape(b, s, SSD_GROUPS, SSD_STATE)
    cm = xbc[..., SSD_INNER + SSD_GROUPS * SSD_STATE:].reshape(b, s, SSD_GROUPS, SSD_STATE)
    xh = xs.reshape(b, s, SSD_GROUPS, SSD_REP, SSD_HEAD_DIM)
    dt = jax.nn.softplus(dt_raw.astype(jnp.float32) + dt_bias.astype(jnp.float32))
    dt = dt.reshape(b, s, SSD_GROUPS, SSD_REP)
    a_neg = -jnp.exp(a_log.astype(jnp.float32)).reshape(SSD_GROUPS, SSD_REP)
    y = ssd_chunked(xh, dt, a_neg, bm, cm)
    y = y + xh * d_skip.astype(jnp.float32).reshape(SSD_GROUPS, SSD_REP)[..., None]
    y = y.reshape(b, s, SSD_INNER) * jax.nn.silu(z.astype(jnp.float32))
    return rms_norm(y, norm_g)


def retention_chunked(q, k, v, log_gamma):
    b, s, h, dk = q.shape
    dv = v.shape[-1]
    nc = s // BLOCK
    qc = q.reshape(b, nc, BLOCK, h, dk)
    kc = k.reshape(b, nc, BLOCK, h, dk)
    vc = v.reshape(b, nc, BLOCK, h, dv)
    idx = jnp.arange(BLOCK, dtype=jnp.float32)
    rel = idx[:, None] - idx[None, :]
    dmask = jnp.where((rel >= 0)[..., None], jnp.exp(jnp.maximum(rel, 0.0)[..., None] * log_gamma), 0.0)
    scores = jnp.einsum('bclhd,bcshd->bchls', qc, kc) * jnp.transpose(dmask, (2, 0, 1))
    inner = jnp.einsum('bchls,bcshd->bclhd', scores, vc)
    kdec = jnp.exp((BLOCK - 1 - idx)[:, None] * log_gamma)
    kv = jnp.einsum('bcshd,bcshe->bchde', kc * kdec[..., None], vc)
    chunk_decay = jnp.exp(BLOCK * log_gamma)

    def step(state, kv_c):
        return state * chunk_decay[:, None, None] + kv_c, state

    r0 = jnp.zeros((b, h, dk, dv), jnp.float32)
    _, r_in = lax.scan(step, r0, jnp.moveaxis(kv, 1, 0))
    r_in = jnp.moveaxis(r_in, 0, 1)
    qdec = jnp.exp((idx + 1.0)[:, None] * log_gamma)
    cross = jnp.einsum('bclhd,bchde->bclhe', qc * qdec[..., None], r_in)
    return (inner + cross).reshape(b, s, h, dv)


def retention_branch(q, k, v, gate, cos, sin):
    b, s, _ = q.shape
    qh = apply_rope(q.reshape(b, s, RET_HEADS, RET_QK_DIM), cos, sin)
    kh = apply_rope(k.reshape(b, s, RET_HEADS, RET_QK_DIM), cos, sin) * (RET_QK_DIM ** -0.5)
    vh = v.reshape(b, s, RET_HEADS, RET_V_DIM).astype(jnp.float32)
    log_gamma = jnp.log(1.0 - jnp.exp2(-5.0 - jnp.arange(RET_HEADS, dtype=jnp.float32)))
    o = retention_chunked(qh, kh, vh, log_gamma)
    o = o * lax.rsqrt(jnp.mean(o * o, axis=-1, keepdims=True) + NORM_EPS)
    return jax.nn.silu(gate.astype(jnp.float32)) * o.reshape(b, s, RET_V)


def dilated_window_attention(q, k, v, dilation, n_back):
    b, s, h, d = q.shape
    span = dilation * BLOCK
    s_pad = -(-s // span) * span
    pad = ((0, 0), (0, s_pad - s), (0, 0), (0, 0))
    L = s_pad // dilation
    nb = L // BLOCK

    def split(t):
        t = jnp.pad(t, pad).reshape(b, L, dilation, h, d).transpose(0, 2, 1, 3, 4)
        return t.reshape(b, dilation, nb, BLOCK, h, d)

    def with_prev(t):
        prev = jnp.pad(t, ((0, 0), (0, 0), (1, 0), (0, 0), (0, 0), (0, 0)))[:, :, :-1]
        return jnp.concatenate([prev, t], axis=3)

    qs = split(q)
    kb = with_prev(split(k))
    vb = with_prev(split(v))
    scores = jnp.einsum('brnqhd,brnkhd->brnhqk', qs, kb) * (d ** -0.5)
    qi = jnp.arange(BLOCK)[:, None]
    kj = jnp.arange(2 * BLOCK)[None, :]
    dist = qi + BLOCK - kj
    band = (dist >= 0) & (dist <= n_back)
    first = band & (kj >= BLOCK)
    mask = jnp.where((jnp.arange(nb) == 0)[:, None, None], first, band)
    scores = jnp.where(mask[None, None, :, None], scores, -jnp.inf)
    m = jnp.max(scores, axis=-1, keepdims=True)
    p = jnp.exp(scores - m)
    den = jnp.sum(p, axis=-1)
    o = jnp.einsum('brnhqk,brnkhd->brnqhd', p, vb) / jnp.moveaxis(den, 3, -1)[..., None]
    lse = jnp.moveaxis(m[..., 0] + jnp.log(den), 3, -1)
    o = o.reshape(b, dilation, L, h, d).transpose(0, 2, 1, 3, 4).reshape(b, s_pad, h, d)[:, :s]
    lse = lse.reshape(b, dilation, L, h).transpose(0, 2, 1, 3).reshape(b, s_pad, h)[:, :s]
    return o, lse


def dilated_branch(qkv, gate, cos, sin):
    b, s, _ = qkv.shape
    qkv = qkv.reshape(b, s, DIL_GROUPS, 3, DIL_HEADS, DIL_HEAD_DIM)
    outs, lses = [], []
    for gi, (window, dilation) in enumerate(DIL_PATTERNS):
        qg = apply_rope(qkv[:, :, gi, 0], cos, sin)
        kg = apply_rope(qkv[:, :, gi, 1], cos, sin)
        vg = qkv[:, :, gi, 2].astype(jnp.float32)
        o, lse = dilated_window_attention(qg, kg, vg, dilation, window // dilation)
        outs.append(o)
        lses.append(lse)
    o = jnp.stack(outs, axis=0)
    wts = jax.nn.softmax(jnp.stack(lses, axis=0), axis=0)
    o = jnp.sum(wts[..., None] * o, axis=0).reshape(b, s, DIL_WIDTH)
    return jax.nn.silu(gate.astype(jnp.float32)) * o


def setup_inputs(seed: int = 0) -> dict:
    key = jax.random.key(seed)
    ks = jax.random.split(key, 16)
    f32 = jnp.float32

    def nrm(k, shape, scale):
        return jax.random.normal(k, shape, f32) * scale

    x = jax.random.normal(ks[0], (BATCH, SEQ, D_MODEL), f32)
    norm_g = 1.0 + nrm(ks[1], (DEPTH, D_MODEL), 0.1)
    w_in = nrm(ks[2], (DEPTH, D_MODEL, D_IN), D_MODEL ** -0.5)
    conv_w = nrm(ks[3], (DEPTH, SSD_CONV, SSD_CONV_CH), SSD_CONV ** -0.5)
    conv_b = nrm(ks[4], (DEPTH, SSD_CONV_CH), 0.01)
    dt0 = jnp.exp(jax.random.uniform(ks[5], (DEPTH, SSD_HEADS), f32, math.log(1e-3), math.log(1e-1)))
    dt_bias = dt0 + jnp.log(-jnp.expm1(-dt0))
    a_log = jnp.log(jax.random.uniform(ks[6], (DEPTH, SSD_HEADS), f32, 1.0, 16.0))
    d_skip = 1.0 + nrm(ks[7], (DEPTH, SSD_HEADS), 0.1)
    ssd_norm_g = 1.0 + nrm(ks[8], (DEPTH, SSD_INNER), 0.1)
    w_o_ssd = nrm(ks[9], (DEPTH, SSD_INNER, D_MODEL), SSD_INNER ** -0.5)
    w_o_ret = nrm(ks[10], (DEPTH, RET_V, D_MODEL), RET_V ** -0.5)
    w_o_dil = nrm(ks[11], (DEPTH, DIL_WIDTH, D_MODEL), DIL_WIDTH ** -0.5)
    w_out = nrm(ks[12], (DEPTH, D_MODEL, D_MODEL), D_MODEL ** -0.5)
    final_norm_g = 1.0 + nrm(ks[13], (D_MODEL,), 0.1)
    return {'x': x, 'norm_g': norm_g, 'w_in': w_in, 'conv_w': conv_w, 'conv_b': conv_b,
            'dt_bias': dt_bias, 'a_log': a_log, 'd_skip': d_skip, 'ssd_norm_g': ssd_norm_g,
            'w_o_ssd': w_o_ssd, 'w_o_ret': w_o_ret, 'w_o_dil': w_o_dil, 'w_out': w_out,
            'final_norm_g': final_norm_g}


def reference(x, norm_g, w_in, conv_w, conv_b, dt_bias, a_log, d_skip, ssd_norm_g,
              w_o_ssd, w_o_ret, w_o_dil, w_out, final_norm_g):
    b, s, _ = x.shape
    cos_r, sin_r = rope_tables(s, RET_QK_DIM)
    cos_d, sin_d = rope_tables(s, DIL_HEAD_DIM)
    for layer in range(DEPTH):
        h = rms_norm(x, norm_g[layer]).astype(x.dtype)
        proj = h @ w_in[layer]
        (z, xbc, dt_raw, rq, rk, rv, rg, dqkv, dg, mg) = jnp.split(proj, SPLIT_POINTS, axis=-1)
        y_a = ssd_branch(z, xbc, dt_raw, conv_w[layer], conv_b[layer], dt_bias[layer],
                         a_log[layer], d_skip[layer], ssd_norm_g[layer]).astype(x.dtype)
        y_b = retention_branch(rq, rk, rv, rg, cos_r, sin_r).astype(x.dtype)
        y_c = dilated_branch(dqkv, dg, cos_d, sin_d).astype(x.dtype)
        gates = jax.nn.sigmoid(mg.astype(jnp.float32)).reshape(b, s, N_BRANCHES, D_MODEL)
        merged = (gates[:, :, 0] * (y_a @ w_o_ssd[layer]).astype(jnp.float32)
                  + gates[:, :, 1] * (y_b @ w_o_ret[layer]).astype(jnp.float32)
                  + gates[:, :, 2] * (y_c @ w_o_dil[layer]).astype(jnp.float32))
        x = x + merged.astype(x.dtype) @ w_out[layer]
    return rms_norm(x, final_norm_g).astype(x.dtype)
```

```python
import math
from contextlib import ExitStack

import numpy as np
import concourse.bass as bass
import concourse.mybir as mybir
from concourse.bass_utils import run_bass_kernel_spmd

F32 = mybir.dt.float32
BF16 = mybir.dt.bfloat16
AF = mybir.ActivationFunctionType
ALU = mybir.AluOpType

D = 2048
DEPTH = 2
EPS = 1e-6
NRING = 12
SAME_ENG_SYNC = False

SPL = dict(z=(0, 2048), xbc=(2048, 5120), dt=(5120, 5152), rq=(5152, 6176), rk=(6176, 7200),
           rv=(7200, 9248), rg=(9248, 11296), dqkv=(11296, 20512), dg=(20512, 21536), mg=(21536, 27680))
TM_Z, TM_RQ, TM_RK, TM_RV, TM_RG, TM_DQKV, TM_DG = 0, 2048, 3072, 4096, 6144, 8192, 17408
TM_W = 18432
N_TMBLK = 36
N_FMBLK = 18
N_BLK = N_TMBLK + 1 + N_FMBLK


def tm_block_kind(b):
    c = b * 512
    if c < TM_RQ:
        return 'silu'
    if c < TM_RK:
        return 'rope_q'
    if c < TM_RV:
        return 'rope_k'
    if c < TM_RG:
        return 'copy'
    if c < TM_DQKV:
        return 'silu'
    if c < TM_DG:
        j = ((c - TM_DQKV) // 1024) % 3
        return ('rope_q', 'rope_k', 'copy')[j]
    return 'silu'


class Buf:
    __slots__ = ('t', 'w', 'r', 'multi', 'small')

    def __init__(self, t=None, multi=False, small=False):
        self.t = t
        self.w = {}
        self.r = {}
        self.multi = multi
        self.small = small

    def __getitem__(self, idx):
        return self.t[idx]


class Eng:
    def __init__(self, name, e, sem):
        self.name, self.e, self.sem = name, e, sem
        self.cnt = 0
        self.seen = {}


class TK:
    def __init__(self, nc, es):
        self.nc = nc
        mk = lambda n: es.enter_context(nc.semaphore(n))
        self.pe = Eng('pe', nc.tensor, mk('s_pe'))
        self.act = Eng('act', nc.scalar, mk('s_act'))
        self.dve = Eng('dve', nc.vector, mk('s_dve'))
        self.pool = Eng('pool', nc.gpsimd, mk('s_pool'))
        self.sp = Eng('sp', nc.sync, mk('s_sp'))
        self.engs = [self.pe, self.act, self.dve, self.pool, self.sp]
        self.dq = {}
        for E in (self.sp, self.pool, self.act):
            self.dq[E.name] = dict(sems=[mk(f'd_{E.name}{i}') for i in range(NRING)], vals=[0] * NRING, i=0)

    def _wait(self, E, toks):
        for tk_ in toks:
            sem, val, owner = tk_[0], tk_[1], tk_[2]
            small = len(tk_) > 3 and tk_[3]
            if val <= 0:
                continue
            if owner is E and (E is self.pe or not (SAME_ENG_SYNC or small)):
                continue
            k = id(sem)
            if E.seen.get(k, 0) >= val:
                continue
            E.e.wait_ge(sem, val)
            E.seen[k] = val

    @staticmethod
    def _deps(reads, writes):
        toks = []
        for b in reads:
            toks += [t + (True,) for t in b.w.values()]
        for b in writes:
            toks += [t + (True,) for t in b.w.values()]
            toks += list(b.r.values())
        return toks

    @staticmethod
    def _record(tok, reads, writes):
        k = id(tok[0])
        for b in reads:
            o = b.r.get(k)
            if o is None or o[1] < tok[1]:
                b.r[k] = tok
        for b in writes:
            if b.multi:
                o = b.w.get(k)
                if o is None or o[1] < tok[1]:
                    b.w[k] = tok
            else:
                b.w = {k: tok}
                b.r = {}

    def op(self, E, fn, reads=(), writes=(), inc=True):
        self._wait(E, self._deps(reads, writes))
        ins = fn(E.e)
        if inc:
            E.cnt += 1
            ins.then_inc(E.sem, 1)
            tok = (E.sem, E.cnt, E)
        else:
            tok = (E.sem, E.cnt + 1, E)
        self._record(tok, reads, writes)
        return tok

    def dma(self, E, out, in_, reads=(), writes=()):
        q = self.dq[E.name]
        k = q['i'] % NRING
        q['i'] += 1
        sem = q['sems'][k]
        prev = q['vals'][k]
        toks = self._deps(reads, writes)
        toks.append((sem, prev, None))
        self._wait(E, toks)
        E.e.dma_start(out=out, in_=in_).then_inc(sem, 16)
        q['vals'][k] = prev + 16
        tok = (sem, prev + 16, None)
        self._record(tok, reads, writes)
        return tok

    def barrier(self):
        toks = [(E.sem, E.cnt, E) for E in self.engs if E.cnt]
        for q in self.dq.values():
            for sem, val in zip(q['sems'], q['vals']):
                toks.append((sem, val, None))
        for E in self.engs:
            self._wait(E, toks)


class Prog:
    def __init__(self, S, depth=DEPTH, debug=False, stages='ABC'):
        self.S, self.depth, self.debug, self.stages = S, depth, debug, stages
        self.NCH = S // 128
        nc = self.nc = bass.Bass("TRN2", target_bir_lowering=False)
        self.es = ExitStack()
        self.tk = TK(nc, self.es)
        self.dram = {}
        self.dbuf = {}

    def din(self, name, shape, dt=F32):
        self.dram[name] = self.nc.dram_tensor(name, list(shape), dt, kind="ExternalInput").ap()
        return self.dram[name]

    def dscr(self, name, shape, dt, out=False):
        kind = "ExternalOutput" if (out or self.debug) else "Internal"
        self.dram[name] = self.nc.dram_tensor(name, list(shape), dt, kind=kind).ap()
        return self.dram[name]

    def db(self, *key):
        b = self.dbuf.get(key)
        if b is None:
            b = self.dbuf[key] = Buf(None, multi=True)
        return b

    def _uid(self, name):
        self._n = getattr(self, '_n', 0) + 1
        return f'{name}_u{self._n}'

    def sb(self, st, name, shape, dt=F32):
        small = int(np.prod(shape[1:])) <= 512
        return Buf(st.enter_context(self.nc.sbuf_tensor(self._uid('s_' + name), list(shape), dt)), small=small)

    def ps(self, st, name, shape, dt=F32):
        return Buf(st.enter_context(self.nc.psum_tensor(self._uid('p_' + name), list(shape), dt)))

    def build(self):
        S, NCH, tk = self.S, self.NCH, self.tk
        nc = self.nc
        self.din('xT', [D, S])
        self.din('w_in', [self.depth, N_BLK, 128, 16, 512])
        self.din('w_o', [self.depth, 56, 128, 2048])
        self.din('cvec', [self.depth, 128, 16 + 24 * 5 + 32 * 3])
        self.din('grow', [self.depth, 128, 2048])
        self.din('fgc', [128, 16])
        self.din('rope', [128, NCH, 128])
        self.din('ctab', [128, 128 * 6 + 8 * 128 * 2 + 8])
        for l in range(self.depth):
            self.dscr(f'wb{l}', [N_BLK, 128, 16, 512], BF16)
            self.dscr(f'wob{l}', [56, 128, 2048], BF16)
            self.dscr(f'tm{l}', [S, TM_W], BF16)
            self.dscr(f'dt{l}', [S, 32], F32)
            self.dscr(f'xbcT{l}', [24, 128, S], BF16)
            self.dscr(f'gT{l}', [48, 128, S], BF16)
            self.dscr(f'y{l}', [S, 5120], BF16)
            self.dscr(f'od{l}', [3, S, 8, 129], F32)
            self.dscr(f'xn{l}', [D, S], F32)
        self.dscr('outT', [D, S], F32, out=True)
        if self.debug:
            self.dscr('dbgA', [S, 1032], F32)
            self.dscr('dbgB', [S, 1024], F32)
            self.dscr('dbgC', [S, 8], F32)

        with ExitStack() as gs:
            self.gs = gs
            self.consts(gs)
            self.cast_weights()
            for l in range(self.depth):
                xin = self.dram['xT'] if l == 0 else self.dram[f'xn{l - 1}']
                xkey = ('xT',) if l == 0 else ('xn', l - 1)
                if 'A' in self.stages:
                    self.stage_A(l, xin, xkey)
                if 'B' in self.stages or '1' in self.stages:
                    self.stage_B1(l)
                if 'B' in self.stages or '2' in self.stages:
                    self.stage_B2(l)
                if 'B' in self.stages or '3' in self.stages:
                    self.stage_B3(l)
                if 'C' in self.stages:
                    self.stage_C(l, xin, xkey)
            if 'C' in self.stages:
                self.stage_final()
            tk.barrier()
        self.es.close()
        return nc

    def consts(self, gs):
        tk, d = self.tk, self.dram
        self.c32 = self.sb(gs, 'c32', [128, 768], F32)
        self.cbf = self.sb(gs, 'cbf', [128, 768], BF16)
        tk.dma(tk.sp, out=self.c32[:], in_=d['ctab'][:, 0:768], writes=[self.c32])
        tk.op(tk.dve, lambda e: e.tensor_copy(out=self.cbf[:], in_=self.c32[:]), reads=[self.c32], writes=[self.cbf])
        self.ident_f = lambda: self.c32[:, 0:128]
        self.ones_f = lambda: self.c32[:, 128:256]
        self.U_f = lambda: self.c32[:, 256:384]
        self.Us_f = lambda: self.c32[:, 384:512]
        self.ident_b = lambda: self.cbf[:, 0:128]
        self.negcur_b = lambda: self.cbf[:, 512:640]
        self.negprev_b = lambda: self.cbf[:, 640:768]

    def cast_weights(self):
        tk, d = self.tk, self.dram
        for l in range(self.depth):
            src = d['w_in'][l].rearrange("b p (q k) c -> b (p q) (k c)", q=4)
            dst = d[f'wb{l}'].rearrange("b p (q k) c -> b (p q) (k c)", q=4)
            for b in range(N_BLK):
                tk.dma(tk.pool, out=dst[b], in_=src[b], writes=[self.db('wb', l, b)])
            src = d['w_o'][l]
            dst = d[f'wob{l}']
            for b in range(56):
                tk.dma(tk.pool, out=dst[b], in_=src[b], writes=[self.db('wob', l, b)])

    def stage_A(self, l, xin, xkey):
        tk, d, S = self.tk, self.dram, self.S
        pe, act, dve, pool, sp = tk.pe, tk.act, tk.dve, tk.pool, tk.sp
        TS = 1024
        NSUP = S // TS
        with ExitStack() as st:
            hT = self.sb(st, 'hT', [128, 16, TS], BF16)
            xs = [self.sb(st, f'xs{i}', [128, 16, 128], F32) for i in range(2)]
            sq = self.sb(st, 'sq', [128, 16, 128], F32)
            rstd = self.sb(st, 'rstd', [128, 128], F32)
            wb = [self.sb(st, f'wb{i}', [128, 16, 512], BF16) for i in range(2)]
            ropet = self.sb(st, 'ropet', [128, 8, 128], F32)
            cv = self.sb(st, 'cvA', [128, 232], F32)
            halo = self.sb(st, 'halo', [128, 24, 3], F32)
            u = [self.sb(st, f'u{i}', [128, 515], F32) for i in range(2)]
            acc = [self.sb(st, f'acc{i}', [128, 512], F32) for i in range(2)]
            tsb = [self.sb(st, f'tsb{i}', [128, 512], F32) for i in range(2)]
            ra = [self.sb(st, f'ra{i}', [128, 512], F32) for i in range(2)]
            rb = [self.sb(st, f'rb{i}', [128, 512], F32) for i in range(2)]
            ob = [self.sb(st, f'ob{i}', [128, 512], BF16) for i in range(4)]
            dts = [self.sb(st, f'dts{i}', [128, 32], F32) for i in range(2)]
            psA = [self.ps(st, f'psA{i}', [128, 512], F32) for i in range(4)]
            pss = self.ps(st, 'pss', [128, 128], F32)

            tk.dma(sp, out=cv[:], in_=d['cvec'][l], writes=[cv])
            tk.op(dve, lambda e: e.memset(halo[:], 0.0), writes=[halo])
            xview = xin.rearrange("(k p) s -> p k s", p=128)
            tmd, dtd, xbd, gtd = d[f'tm{l}'], d[f'dt{l}'], d[f'xbcT{l}'], d[f'gT{l}']
            cnt = dict(ps=0, ob=0, ts=0, u=0, dt=0)

            for su in range(NSUP):
                for c8 in range(8):
                    gc = su * 8 + c8
                    x_ = xs[gc % 2]
                    tk.dma(sp, out=x_[:], in_=xview[:, :, gc * 128:(gc + 1) * 128],
                           reads=[self.db(*xkey, gc // 4)], writes=[x_])
                    tk.op(act, lambda e: e.activation(out=sq[:], in_=x_[:], func=AF.Square), reads=[x_], writes=[sq])
                    for k in range(16):
                        tk.op(pe, lambda e: e.matmul(pss[:], lhsT=self.ones_f(), rhs=sq[:, k, :], start=(k == 0), stop=(k == 15)),
                              reads=[sq, self.c32], writes=[pss], inc=(k == 15))
                    tk.op(act, lambda e: e.activation(out=rstd[:], in_=pss[:], func=AF.Sqrt, bias=EPS, scale=1.0 / D),
                          reads=[pss], writes=[rstd])
                    tk.op(dve, lambda e: e.reciprocal(out=rstd[:], in_=rstd[:]), reads=[rstd], writes=[rstd])
                    for k in range(16):
                        tk.op(dve, lambda e: e.scalar_tensor_tensor(out=hT[:, k, c8 * 128:(c8 + 1) * 128], in0=x_[:, k, :],
                                                                    scalar=cv[:, k:k + 1], in1=rstd[:], op0=ALU.mult, op1=ALU.mult),
                              reads=[x_, cv, rstd], writes=[hT])
                tk.dma(sp, out=ropet[:], in_=d['rope'][:, su * 8:(su + 1) * 8, :], writes=[ropet])

                for blk in range(N_BLK):
                    w_ = wb[blk % 2]
                    tk.dma(sp, out=w_[:], in_=d[f'wb{l}'][blk], reads=[self.db('wb', l, blk)], writes=[w_])
                    if blk <= N_TMBLK:
                        kind = 'dt' if blk == N_TMBLK else tm_block_kind(blk)
                        ncol = 32 if kind == 'dt' else 512
                        for c8 in range(8):
                            gc = su * 8 + c8
                            ps_ = psA[cnt['ps'] % 4]
                            cnt['ps'] += 1
                            for k in range(16):
                                tk.op(pe, lambda e: e.matmul(ps_[:, 0:ncol], lhsT=hT[:, k, c8 * 128:(c8 + 1) * 128], rhs=w_[:, k, 0:ncol],
                                                             start=(k == 0), stop=(k == 15)),
                                      reads=[hT, w_], writes=[ps_], inc=(k == 15))
                            rows = slice(gc * 128, (gc + 1) * 128)
                            if kind == 'dt':
                                t_ = dts[cnt['dt'] % 2]
                                cnt['dt'] += 1
                                tk.op(dve, lambda e: e.tensor_tensor(out=t_[:], in0=ps_[:, 0:32], in1=cv[:, 136:168], op=ALU.add),
                                      reads=[ps_, cv], writes=[t_])
                                tk.op(act, lambda e: e.activation(out=t_[:], in_=t_[:], func=AF.Exp), reads=[t_], writes=[t_])
                                tk.op(act, lambda e: e.activation(out=t_[:], in_=t_[:], func=AF.Ln, bias=1.0), reads=[t_], writes=[t_])
                                tk.dma(pool, out=dtd[rows, :], in_=t_[:], reads=[t_], writes=[self.db('dt', l, gc)])
                                continue
                            o_ = ob[cnt['ob'] % 4]
                            cnt['ob'] += 1
                            if kind == 'silu':
                                tk.op(act, lambda e: e.activation(out=o_[:], in_=ps_[:], func=AF.Silu), reads=[ps_], writes=[o_])
                            elif kind == 'copy':
                                tk.op(dve, lambda e: e.tensor_copy(out=o_[:], in_=ps_[:]), reads=[ps_], writes=[o_])
                            else:
                                j = cnt['ts'] % 2
                                cnt['ts'] += 1
                                t_, a_, b_ = tsb[j], ra[j], rb[j]
                                sc = 1.0 if kind == 'rope_q' else 128.0 ** -0.5
                                tk.op(act, lambda e: e.activation(out=t_[:], in_=ps_[:], func=AF.Copy, scale=sc), reads=[ps_], writes=[t_])
                                v4 = lambda b: b[:].rearrange("p (h two e) -> p h two e", h=4, two=2)
                                cosb = ropet[:, c8, 0:64].unsqueeze(1).unsqueeze(1).to_broadcast([128, 4, 2, 64])
                                sinb = ropet[:, c8, 64:128].unsqueeze(1).unsqueeze(1).to_broadcast([128, 4, 2, 64])
                                tk.op(dve, lambda e: e.tensor_tensor(out=v4(a_), in0=v4(t_), in1=cosb, op=ALU.mult), reads=[t_, ropet], writes=[a_])
                                tk.op(pool, lambda e: e.tensor_tensor(out=v4(b_), in0=v4(t_), in1=sinb, op=ALU.mult), reads=[t_, ropet], writes=[b_])
                                tk.op(dve, lambda e: e.tensor_tensor(out=v4(o_)[:, :, 0, :], in0=v4(a_)[:, :, 0, :], in1=v4(b_)[:, :, 1, :], op=ALU.subtract),
                                      reads=[a_, b_], writes=[o_])
                                tk.op(pool, lambda e: e.tensor_tensor(out=v4(o_)[:, :, 1, :], in0=v4(a_)[:, :, 1, :], in1=v4(b_)[:, :, 0, :], op=ALU.add),
                                      reads=[a_, b_], writes=[o_])
                            tk.dma(pool, out=tmd[rows, blk * 512:(blk + 1) * 512], in_=o_[:], reads=[o_], writes=[self.db('tm', l, gc)])
                    else:
                        fb = blk - N_TMBLK - 1
                        for ct4 in range(4):
                            ct = fb * 4 + ct4
                            for tt in range(2):
                                ps_ = psA[cnt['ps'] % 4]
                                cnt['ps'] += 1
                                for k in range(16):
                                    tk.op(pe, lambda e: e.matmul(ps_[:], lhsT=w_[:, k, ct4 * 128:(ct4 + 1) * 128], rhs=hT[:, k, tt * 512:(tt + 1) * 512],
                                                                 start=(k == 0), stop=(k == 15)),
                                          reads=[hT, w_], writes=[ps_], inc=(k == 15))
                                tok0 = su * TS + tt * 512
                                o_ = ob[cnt['ob'] % 4]
                                cnt['ob'] += 1
                                if ct >= 24:
                                    tk.op(act, lambda e: e.activation(out=o_[:], in_=ps_[:], func=AF.Sigmoid), reads=[ps_], writes=[o_])
                                    tk.dma(pool, out=gtd[ct - 24, :, tok0:tok0 + 512], in_=o_[:], reads=[o_], writes=[self.db('gT', l, tok0 // 512)])
                                    continue
                                u_ = u[cnt['u'] % 2]
                                a_ = acc[cnt['u'] % 2]
                                cnt['u'] += 1
                                tk.op(dve, lambda e: e.tensor_copy(out=u_[:, 0:3], in_=halo[:, ct, :]), reads=[halo], writes=[u_])
                                tk.op(act, lambda e: e.activation(out=u_[:, 3:515], in_=ps_[:], func=AF.Copy), reads=[ps_], writes=[u_])
                                tk.op(dve, lambda e: e.tensor_copy(out=halo[:, ct, :], in_=u_[:, 512:515]), reads=[u_], writes=[halo])
                                cw = lambda kk: cv[:, 16 + ct * 5 + kk:16 + ct * 5 + kk + 1]
                                tk.op(dve, lambda e: e.tensor_scalar(out=a_[:], in0=u_[:, 3:515], scalar1=cw(3), scalar2=cw(4), op0=ALU.mult, op1=ALU.add),
                                      reads=[u_, cv], writes=[a_])
                                for kk in range(3):
                                    tk.op(dve, lambda e: e.scalar_tensor_tensor(out=a_[:], in0=u_[:, kk:kk + 512], scalar=cw(kk), in1=a_[:],
                                                                                op0=ALU.mult, op1=ALU.add), reads=[u_, cv, a_], writes=[a_])
                                tk.op(act, lambda e: e.activation(out=o_[:], in_=a_[:], func=AF.Silu), reads=[a_], writes=[o_])
                                tk.dma(pool, out=xbd[ct, :, tok0:tok0 + 512], in_=o_[:], reads=[o_], writes=[self.db('xbcT', l, tok0 // 512)])
            tk.barrier()


def _blocks(w, nblk):
    return np.ascontiguousarray(w.reshape(16, 128, nblk, 512).transpose(2, 1, 0, 3))


def prep_weights(inp, depth=DEPTH):
    w_in_l, w_o_l, cvec_l, grow_l = [], [], [], []
    for l in range(depth):
        w = inp['w_in'][l]
        sl = lambda n: w[:, SPL[n][0]:SPL[n][1]]
        tm = np.concatenate([sl('z'), sl('rq'), sl('rk'), sl('rv'), sl('rg'), sl('dqkv'), sl('dg')], axis=1)
        dtp = np.zeros((D, 512), np.float32)
        dtp[:, 0:32] = sl('dt')
        fm = np.concatenate([sl('xbc'), sl('mg')], axis=1)
        w_in_l.append(np.concatenate([_blocks(tm, N_TMBLK), _blocks(dtp, 1), _blocks(fm, N_FMBLK)], axis=0))
        wo = np.concatenate([inp['w_o_ssd'][l], inp['w_o_ret'][l], inp['w_o_dil'][l], inp['w_out'][l]], axis=0)
        w_o_l.append(np.ascontiguousarray(wo.reshape(56, 128, 2048)))
        cv = np.zeros((128, 232), np.float32)
        cv[:, 0:16] = inp['norm_g'][l].reshape(16, 128).T
        cw = inp['conv_w'][l].reshape(4, 24, 128)
        cb = inp['conv_b'][l].reshape(24, 128)
        for ct in range(24):
            cv[:, 16 + ct * 5:16 + ct * 5 + 4] = cw[:, ct, :].T
            cv[:, 16 + ct * 5 + 4] = cb[ct]
        cv[:, 136:168] = inp['dt_bias'][l][None, :]
        cv[:, 168:200] = inp['a_log'][l][None, :]
        cv[:, 200:232] = inp['d_skip'][l][None, :]
        cvec_l.append(cv)
        grow_l.append(np.ascontiguousarray(np.broadcast_to(inp['ssd_norm_g'][l][None, :], (128, 2048))))
    fgc = np.ascontiguousarray(inp['final_norm_g'].reshape(16, 128).T)
    return dict(w_in=np.stack(w_in_l), w_o=np.stack(w_o_l), cvec=np.stack(cvec_l), grow=np.stack(grow_l), fgc=fgc)


def prep_consts(S):
    NCH = S // 128
    pos = np.arange(S, dtype=np.float32)
    inv = (10000.0 ** (-np.arange(0, 128, 2, dtype=np.float32) / 128.0)).astype(np.float32)
    ang = pos[:, None] * inv[None, :]
    rope = np.concatenate([np.cos(ang), np.sin(ang)], axis=1).astype(np.float32)
    rope = np.ascontiguousarray(rope.reshape(NCH, 128, 128).transpose(1, 0, 2))
    i = np.arange(128)
    ct = np.zeros((128, 768 + 2048 + 8), np.float32)
    ct[:, 0:128] = np.eye(128)
    ct[:, 128:256] = 1.0
    ct[:, 256:384] = (i[:, None] <= i[None, :])
    ct[:, 384:512] = (i[:, None] > i[None, :])
    ct[:, 512:640] = np.where(i[None, :] >= i[:, None], 0.0, -30000.0)
    ct[:, 640:768] = np.where(i[:, None] >= i[None, :], 0.0, -30000.0)
    lg = np.log(1.0 - np.exp2(-5.0 - np.arange(8, dtype=np.float64)))
    for h in range(8):
        rel = (i[None, :] - i[:, None]).astype(np.float64)
        ct[:, 768 + h * 128:768 + (h + 1) * 128] = np.where(rel >= 0, np.exp(np.maximum(rel, 0) * lg[h]), 0.0)
        ct[:, 1792 + h * 128:1792 + (h + 1) * 128] = np.exp((i[None, :] + 1.0) * lg[h])
        ct[:, 2816 + h] = np.exp((127.0 - i) * lg[h])
    return dict(rope=rope, ctab=ct)


_CACHE = {}


def kernel(**inp):
    x = np.asarray(inp['x'], np.float32)
    B, S, _ = x.shape
    if 'nc' not in _CACHE:
        _CACHE['nc'] = Prog(S).build()
    nc = _CACHE['nc']
    shared = dict(prep_weights(inp))
    shared.update(prep_consts(S))
    in_maps = []
    for b in range(B):
        m = dict(shared)
        m['xT'] = np.ascontiguousarray(x[b].T)
        in_maps.append(m)
    res = run_bass_kernel_spmd(nc, in_maps, core_ids=list(range(B)))
    out = np.stack([np.ascontiguousarray(r['outT'].T) for r in res.results], axis=0)
    return out.astype(np.float32)


def _tr(tk, ps_bf, slot, src_ap, src_bufs, P, last=True):
    tk.op(tk.pe, lambda e: e.transpose(out=ps_bf.t[:].bitcast(BF16)[:, slot * 128:(slot + 1) * 128], in_=src_ap, identity=P.ident_b()),
          reads=list(src_bufs) + [P.cbf], writes=[ps_bf], inc=last)


def stage_B1(self, l):
    tk, d, S, NCH = self.tk, self.dram, self.S, self.NCH
    pe, act, dve, pool, sp = tk.pe, tk.act, tk.dve, tk.pool, tk.sp
    with ExitStack() as st:
        xbl = self.sb(st, 'xbl', [128, 24, 512], BF16)
        x_tok_2 = [self.sb(st, f'x_tok{i}', [128, 2048], BF16) for i in range(2)]
        B_tok_2 = [self.sb(st, f'B_tok{i}', [128, 512], BF16) for i in range(2)]
        zs_2 = [self.sb(st, f'zs{i}', [128, 2048], BF16) for i in range(2)]
        xdt_2 = [self.sb(st, f'xdt{i}', [128, 32, 64], BF16) for i in range(2)]
        xdte_2 = [self.sb(st, f'xdte{i}', [128, 32, 64], BF16) for i in range(2)]
        lall = self.sb(st, 'lall', [128, 32, 128], F32)
        cbm_2 = [self.sb(st, f'cbm{i}', [128, 4, 128], F32) for i in range(2)]
        Lm = [self.sb(st, f'Lm{i}', [128, 4, 128], F32) for i in range(2)]
        M_2 = [self.sb(st, f'M{i}', [128, 32, 128], BF16) for i in range(2)]
        yt_2 = [self.sb(st, f'yt{i}', [128, 2048], F32) for i in range(2)]
        tmp = self.sb(st, 'tmp', [128, 2048], F32)
        Hs = self.sb(st, 'Hs', [128, 2048], F32)
        Hb = self.sb(st, 'Hb', [128, 2048], BF16)
        grow = self.sb(st, 'grow', [128, 2048], F32)
        ya_2 = [self.sb(st, f'ya{i}', [128, 2048], BF16) for i in range(2)]
        cv = self.sb(st, 'cvB', [128, 232], F32)
        arow = self.sb(st, 'arow', [128, 32], F32)
        dt_t_2 = [self.sb(st, f'dt_t{i}', [128, 32], F32) for i in range(2)]
        dta_2 = [self.sb(st, f'dta{i}', [128, 32], F32) for i in range(2)]
        sS_2 = [self.sb(st, f'sS{i}', [128, 64], F32) for i in range(2)]
        eacs_2 = [self.sb(st, f'eacs{i}', [128, 32], F32) for i in range(2)]
        tend_2 = [self.sb(st, f'tend{i}', [128, 32], F32) for i in range(2)]
        cdr_2 = [self.sb(st, f'cdr{i}', [128, 32], F32) for i in range(2)]
        ss_2 = [self.sb(st, f'ss{i}', [128, 1], F32) for i in range(2)]
        psT = self.ps(st, 'psT', [128, 512], F32)
        psS = self.ps(st, 'psS', [128, 64], F32)
        psC = self.ps(st, 'psC', [128, 512], F32)
        psG = [self.ps(st, f'psG{i}', [128, 512], F32) for i in range(2)]
        psD = self.ps(st, 'psD', [128, 512], F32)
        psO = self.ps(st, 'psO', [128, 512], F32)
        psN = self.ps(st, 'psN', [128, 512], F32)

        tk.dma(sp, out=cv[:], in_=d['cvec'][l], writes=[cv])
        tk.dma(sp, out=grow[:], in_=d['grow'][l], writes=[grow])
        tk.op(act, lambda e: e.activation(out=arow[:], in_=cv[:, 168:200], func=AF.Exp), reads=[cv], writes=[arow])
        tk.op(dve, lambda e: e.tensor_scalar(out=arow[:], in0=arow[:], scalar1=-1.0, scalar2=None, op0=ALU.mult), reads=[arow], writes=[arow])
        tk.op(dve, lambda e: e.memset(Hs[:], 0.0), writes=[Hs])
        tk.op(dve, lambda e: e.memset(Hb[:], 0.0), writes=[Hb])
        tmd, dtd, xbd, yd = d[f'tm{l}'], d[f'dt{l}'], d[f'xbcT{l}'], d[f'y{l}']
        bc3 = lambda ap, n: ap.unsqueeze(2).to_broadcast([128, ap.shape[1], n])
        for gc in range(NCH):
            rows = slice(gc * 128, (gc + 1) * 128)
            sub = gc % 4
            x_tok = x_tok_2[gc % 2]
            B_tok = B_tok_2[gc % 2]
            zs = zs_2[gc % 2]
            xdt = xdt_2[gc % 2]
            xdte = xdte_2[gc % 2]
            cbm = cbm_2[gc % 2]
            M = M_2[gc % 2]
            yt = yt_2[gc % 2]
            ya = ya_2[gc % 2]
            dt_t = dt_t_2[gc % 2]
            dta = dta_2[gc % 2]
            sS = sS_2[gc % 2]
            eacs = eacs_2[gc % 2]
            tend = tend_2[gc % 2]
            cdr = cdr_2[gc % 2]
            ss = ss_2[gc % 2]
            ts_ = slice(sub * 128, (sub + 1) * 128)
            if sub == 0:
                tk.dma(sp, out=xbl[:], in_=xbd[:, :, gc * 128:gc * 128 + 512].rearrange("t p s -> p t s"),
                       reads=[self.db('xbcT', l, gc // 4)], writes=[xbl])
            tk.dma(sp, out=dt_t[:], in_=dtd[rows, :], reads=[self.db('dt', l, gc)], writes=[dt_t])
            tk.dma(sp, out=zs[:], in_=tmd[rows, TM_Z:TM_Z + 2048], reads=[self.db('tm', l, gc)], writes=[zs])
            for half in range(2):
                for i in range(8):
                    _tr(tk, psT, i, xbl[:, half * 8 + i, ts_], [xbl], self, last=(i == 7))
                tk.op(act, lambda e: e.activation(out=x_tok[:, half * 1024:(half + 1) * 1024], in_=psT.t[:].bitcast(BF16), func=AF.Copy),
                      reads=[psT], writes=[x_tok])
            for i in range(4):
                _tr(tk, psT, i, xbl[:, 16 + i, ts_], [xbl], self, last=(i == 3))
            tk.op(act, lambda e: e.activation(out=B_tok[:], in_=psT.t[:].bitcast(BF16)[:, 0:512], func=AF.Copy), reads=[psT], writes=[B_tok])
            tk.op(dve, lambda e: e.tensor_tensor(out=dta[:], in0=dt_t[:], in1=arow[:], op=ALU.mult), reads=[dt_t, arow], writes=[dta])
            tk.op(pe, lambda e: e.matmul(psS[:, 0:32], lhsT=self.U_f(), rhs=dta[:], start=True, stop=True), reads=[dta, self.c32], writes=[psS], inc=False)
            tk.op(pe, lambda e: e.matmul(psS[:, 32:64], lhsT=self.ones_f(), rhs=dta[:], start=True, stop=True), reads=[dta, self.c32], writes=[psS])
            tk.op(act, lambda e: e.activation(out=sS[:], in_=psS[:], func=AF.Copy), reads=[psS], writes=[sS])
            tk.op(act, lambda e: e.activation(out=eacs[:], in_=sS[:, 0:32], func=AF.Exp), reads=[sS], writes=[eacs])
            tk.op(dve, lambda e: e.tensor_tensor(out=tend[:], in0=sS[:, 32:64], in1=sS[:, 0:32], op=ALU.subtract), reads=[sS], writes=[tend])
            tk.op(act, lambda e: e.activation(out=tend[:], in_=tend[:], func=AF.Exp), reads=[tend], writes=[tend])
            tk.op(act, lambda e: e.activation(out=cdr[:], in_=sS[:, 32:64], func=AF.Exp), reads=[sS], writes=[cdr])
            xv = x_tok[:].rearrange("p (h e) -> p h e", h=32)
            tk.op(dve, lambda e: e.tensor_tensor(out=xdt[:], in0=xv, in1=bc3(dt_t[:], 64), op=ALU.mult), reads=[x_tok, dt_t], writes=[xdt])
            tk.op(pool, lambda e: e.tensor_tensor(out=xdte[:], in0=xdt[:], in1=bc3(tend[:], 64), op=ALU.mult), reads=[xdt, tend], writes=[xdte])
            usb = self.Us_f().unsqueeze(1).to_broadcast([128, 16, 128])
            tk.op(dve, lambda e: e.tensor_tensor(out=lall[:, 0:16, :], in0=bc3(dta[:, 0:16], 128), in1=usb, op=ALU.mult), reads=[dta, self.c32], writes=[lall])
            tk.op(pool, lambda e: e.tensor_tensor(out=lall[:, 16:32, :], in0=bc3(dta[:, 16:32], 128), in1=usb, op=ALU.mult), reads=[dta, self.c32], writes=[lall])
            for g in range(4):
                tk.op(pe, lambda e: e.matmul(psC[:, g * 128:(g + 1) * 128], lhsT=xbl[:, 16 + g, ts_], rhs=xbl[:, 20 + g, ts_], start=True, stop=True),
                      reads=[xbl], writes=[psC], inc=(g == 3))
            ub = self.U_f().unsqueeze(1).to_broadcast([128, 4, 128])
            tk.op(dve, lambda e: e.tensor_tensor(out=cbm[:], in0=psC[:].rearrange("p (g e) -> p g e", g=4), in1=ub, op=ALU.mult), reads=[psC, self.c32], writes=[cbm])
            for hq in range(8):
                pg, lm = psG[hq % 2], Lm[hq % 2]
                for i in range(4):
                    tk.op(pe, lambda e: e.matmul(pg[:, i * 128:(i + 1) * 128], lhsT=lall[:, hq * 4 + i, :], rhs=self.U_f(), start=True, stop=True),
                          reads=[lall, self.c32], writes=[pg], inc=(i == 3))
                tk.op(act, lambda e: e.activation(out=lm[:], in_=pg[:].rearrange("p (g e) -> p g e", g=4), func=AF.Exp), reads=[pg], writes=[lm])
                cb_b = cbm[:, hq // 2, :].unsqueeze(1).to_broadcast([128, 4, 128])
                tk.op(dve, lambda e: e.tensor_tensor(out=M[:, hq * 4:(hq + 1) * 4, :], in0=lm[:], in1=cb_b, op=ALU.mult), reads=[lm, cbm], writes=[M])
            for g in range(4):
                gs_ = slice(g * 512, (g + 1) * 512)
                for i in range(8):
                    h = g * 8 + i
                    tk.op(pe, lambda e: e.matmul(psD[:, i * 64:(i + 1) * 64], lhsT=M[:, h, :], rhs=xdt[:, h, :], start=True, stop=True),
                          reads=[M, xdt], writes=[psD], inc=(i == 7))
                tk.op(pe, lambda e: e.matmul(psO[:], lhsT=xbl[:, 20 + g, ts_], rhs=Hb[:, gs_], start=True, stop=True), reads=[xbl, Hb], writes=[psO])
                tk.op(pe, lambda e: e.matmul(psN[:], lhsT=B_tok[:, g * 128:(g + 1) * 128], rhs=xdte[:, g * 8:(g + 1) * 8, :], start=True, stop=True),
                      reads=[B_tok, xdte], writes=[psN])
                v8 = lambda ap: ap.rearrange("p (h e) -> p h e", h=8)
                tk.op(dve, lambda e: e.tensor_tensor(out=v8(yt[:, gs_]), in0=v8(psO[:]), in1=bc3(eacs[:, g * 8:(g + 1) * 8], 64), op=ALU.mult),
                      reads=[psO, eacs], writes=[yt])
                tk.op(dve, lambda e: e.tensor_tensor(out=yt[:, gs_], in0=psD[:], in1=yt[:, gs_], op=ALU.add), reads=[psD, yt], writes=[yt])
                tk.op(pool, lambda e: e.tensor_tensor(out=v8(Hs[:, gs_]), in0=v8(Hs[:, gs_]), in1=bc3(cdr[:, g * 8:(g + 1) * 8], 64), op=ALU.mult),
                      reads=[Hs, cdr], writes=[Hs])
                tk.op(dve, lambda e: e.tensor_tensor(out=Hs[:, gs_], in0=psN[:], in1=Hs[:, gs_], op=ALU.add), reads=[psN, Hs], writes=[Hs])
                tk.op(act, lambda e: e.activation(out=Hb[:, gs_], in_=Hs[:, gs_], func=AF.Copy), reads=[Hs], writes=[Hb])
            tk.op(pool, lambda e: e.tensor_tensor(out=tmp[:].rearrange("p (h e) -> p h e", h=32), in0=xv, in1=bc3(cv[:, 200:232], 64), op=ALU.mult),
                  reads=[x_tok, cv], writes=[tmp])
            tk.op(pool, lambda e: e.tensor_tensor(out=yt[:], in0=yt[:], in1=tmp[:], op=ALU.add), reads=[yt, tmp], writes=[yt])
            tk.op(pool, lambda e: e.tensor_tensor(out=yt[:], in0=yt[:], in1=zs[:], op=ALU.mult), reads=[yt, zs], writes=[yt])
            tk.op(act, lambda e: e.activation(out=tmp[:], in_=yt[:], func=AF.Square, accum_out=ss[:]), reads=[yt], writes=[tmp, ss])
            tk.op(act, lambda e: e.activation(out=ss[:], in_=ss[:], func=AF.Sqrt, bias=EPS, scale=1.0 / 2048), reads=[ss], writes=[ss])
            tk.op(dve, lambda e: e.reciprocal(out=ss[:], in_=ss[:]), reads=[ss], writes=[ss])
            tk.op(dve, lambda e: e.scalar_tensor_tensor(out=ya[:], in0=yt[:], scalar=ss[:, 0:1], in1=grow[:], op0=ALU.mult, op1=ALU.mult),
                  reads=[yt, ss, grow], writes=[ya])
            tk.dma(pool, out=yd[rows, 0:2048], in_=ya[:], reads=[ya], writes=[self.db('y', l, gc)])
        tk.barrier()


Prog.stage_B1 = stage_B1


def stage_B2(self, l):
    tk, d, S, NCH = self.tk, self.dram, self.S, self.NCH
    pe, act, dve, pool, sp = tk.pe, tk.act, tk.dve, tk.pool, tk.sp
    cdh = [float(np.exp(128.0 * np.log(1.0 - 2.0 ** (-5.0 - h)))) for h in range(8)]
    with ExitStack() as st:
        rt = [self.sb(st, f'rt{i}', [128, 6144], BF16) for i in range(2)]
        tab = self.sb(st, 'rtab', [128, 2056], F32)
        qT_2 = [self.sb(st, f'qT{i}', [128, 8, 128], BF16) for i in range(2)]
        qTd_2 = [self.sb(st, f'qTd{i}', [128, 8, 128], BF16) for i in range(2)]
        kT_2 = [self.sb(st, f'kT{i}', [128, 8, 128], BF16) for i in range(2)]
        kdk_2 = [self.sb(st, f'kdk{i}', [128, 8, 128], BF16) for i in range(2)]
        ST = [self.sb(st, f'ST{i}', [128, 4, 128], BF16) for i in range(2)]
        osb_2 = [self.sb(st, f'osb{i}', [128, 2048], F32) for i in range(2)]
        junk = self.sb(st, 'junk', [128, 256], F32)
        ss8_2 = [self.sb(st, f'ss8{i}', [128, 8], F32) for i in range(2)]
        Rs = self.sb(st, 'Rs', [128, 2048], F32)
        Rb = self.sb(st, 'Rb', [128, 2048], BF16)
        yb_2 = [self.sb(st, f'yb{i}', [128, 2048], BF16) for i in range(2)]
        psT = self.ps(st, 'psT2', [128, 512], F32)
        psSc = [self.ps(st, f'psSc{i}', [128, 512], F32) for i in range(2)]
        psO = [self.ps(st, f'psO2{i}', [128, 256], F32) for i in range(2)]
        psK = [self.ps(st, f'psK{i}', [128, 256], F32) for i in range(2)]
        tk.dma(sp, out=tab[:], in_=d['ctab'][:, 768:768 + 2056], writes=[tab])
        tk.op(dve, lambda e: e.memset(Rs[:], 0.0), writes=[Rs])
        tk.op(dve, lambda e: e.memset(Rb[:], 0.0), writes=[Rb])
        tmd, yd = d[f'tm{l}'], d[f'y{l}']
        psTb = lambda: psT.t[:].bitcast(BF16)
        for gc in range(NCH):
            rows = slice(gc * 128, (gc + 1) * 128)
            r_ = rt[gc % 2]
            qT = qT_2[gc % 2]
            qTd = qTd_2[gc % 2]
            kT = kT_2[gc % 2]
            kdk = kdk_2[gc % 2]
            osb = osb_2[gc % 2]
            ss8 = ss8_2[gc % 2]
            yb = yb_2[gc % 2]
            tk.dma(sp, out=r_[:], in_=tmd[rows, TM_RQ:TM_DQKV], reads=[self.db('tm', l, gc)], writes=[r_])
            q_tok = lambda h: r_[:, h * 128:(h + 1) * 128]
            k_tok = lambda h: r_[:, 1024 + h * 128:1024 + (h + 1) * 128]
            v_tok = lambda h: r_[:, 2048 + h * 256:2048 + (h + 1) * 256]
            rgs = lambda h: r_[:, 4096 + h * 256:4096 + (h + 1) * 256]
            for h in range(8):
                _tr(tk, psT, h, q_tok(h), [r_], self, last=(h == 7))
            tk.op(act, lambda e: e.activation(out=qT[:].rearrange("p h e -> p (h e)"), in_=psTb(), func=AF.Copy), reads=[psT], writes=[qT])
            tk.op(dve, lambda e: e.tensor_tensor(out=qTd[:].rearrange("p h e -> p (h e)"), in0=qT[:].rearrange("p h e -> p (h e)"), in1=tab[:, 1024:2048], op=ALU.mult),
                  reads=[qT, tab], writes=[qTd])
            for h in range(8):
                _tr(tk, psT, h, k_tok(h), [r_], self, last=(h == 7))
            tk.op(act, lambda e: e.activation(out=kT[:].rearrange("p h e -> p (h e)"), in_=psTb(), func=AF.Copy), reads=[psT], writes=[kT])
            tk.op(pool, lambda e: e.tensor_tensor(out=kdk[:], in0=r_[:, 1024:2048].rearrange("p (h e) -> p h e", h=8),
                                                  in1=tab[:, 2048:2056].unsqueeze(2).to_broadcast([128, 8, 128]), op=ALU.mult),
                  reads=[r_, tab], writes=[kdk])
            for hq in range(2):
                pc, st_ = psSc[hq], ST[hq]
                for i in range(4):
                    h = hq * 4 + i
                    tk.op(pe, lambda e: e.matmul(pc[:, i * 128:(i + 1) * 128], lhsT=kT[:, h, :], rhs=qT[:, h, :], start=True, stop=True),
                          reads=[kT, qT], writes=[pc], inc=(i == 3))
                tk.op(dve, lambda e: e.tensor_tensor(out=st_[:].rearrange("p h e -> p (h e)"), in0=pc[:], in1=tab[:, hq * 512:(hq + 1) * 512], op=ALU.mult),
                      reads=[pc, tab], writes=[st_])
            for h in range(8):
                po, pk = psO[h % 2], psK[h % 2]
                hs = slice(h * 256, (h + 1) * 256)
                tk.op(pe, lambda e: e.matmul(po[:], lhsT=ST[h // 4][:, h % 4, :], rhs=v_tok(h), start=True, stop=False),
                      reads=[ST[h // 4], r_], writes=[po], inc=False)
                tk.op(pe, lambda e: e.matmul(po[:], lhsT=qTd[:, h, :], rhs=Rb[:, hs], start=False, stop=True), reads=[qTd, Rb], writes=[po])
                tk.op(pe, lambda e: e.matmul(pk[:], lhsT=kdk[:, h, :], rhs=v_tok(h), start=True, stop=True), reads=[kdk, r_], writes=[pk])
                tk.op(dve, lambda e: e.tensor_copy(out=osb[:, hs], in_=po[:]), reads=[po], writes=[osb])
                tk.op(act, lambda e: e.activation(out=junk[:], in_=osb[:, hs], func=AF.Square, accum_out=ss8[:, h:h + 1]), reads=[osb], writes=[junk, ss8])
                tk.op(pool, lambda e: e.tensor_scalar(out=Rs[:, hs], in0=Rs[:, hs], scalar1=cdh[h], scalar2=None, op0=ALU.mult), reads=[Rs], writes=[Rs])
                tk.op(dve, lambda e: e.tensor_tensor(out=Rs[:, hs], in0=pk[:], in1=Rs[:, hs], op=ALU.add), reads=[Rs, pk], writes=[Rs])
                tk.op(act, lambda e: e.activation(out=Rb[:, hs], in_=Rs[:, hs], func=AF.Copy), reads=[Rs], writes=[Rb])
            tk.op(act, lambda e: e.activation(out=ss8[:], in_=ss8[:], func=AF.Sqrt, bias=EPS, scale=1.0 / 256), reads=[ss8], writes=[ss8])
            tk.op(dve, lambda e: e.reciprocal(out=ss8[:], in_=ss8[:]), reads=[ss8], writes=[ss8])
            for h in range(8):
                hs = slice(h * 256, (h + 1) * 256)
                tk.op(dve, lambda e: e.scalar_tensor_tensor(out=yb[:, hs], in0=osb[:, hs], scalar=ss8[:, h:h + 1], in1=rgs(h), op0=ALU.mult, op1=ALU.mult),
                      reads=[osb, ss8, r_], writes=[yb])
            tk.dma(pool, out=yd[rows, 2048:4096], in_=yb[:], reads=[yb], writes=[self.db('y', l, gc)])
        tk.barrier()


def stage_B3(self, l):
    tk, d, S, NCH = self.tk, self.dram, self.S, self.NCH
    pe, act, dve, pool, sp = tk.pe, tk.act, tk.dve, tk.pool, tk.sp
    DIL = (1, 4, 16)
    with ExitStack() as st:
        qtok_2 = [self.sb(st, f'qtok{i}', [128, NCH, 128], BF16) for i in range(2)]
        ktok_2 = [self.sb(st, f'ktok{i}', [128, NCH, 128], BF16) for i in range(2)]
        qT_2 = [self.sb(st, f'dqT{i}', [128, S], BF16) for i in range(2)]
        kT_2 = [self.sb(st, f'dkT{i}', [128, S], BF16) for i in range(2)]
        vaug = [self.sb(st, f'vaug{i}', [128, NCH, 130], BF16) for i in range(2)]
        pT = [self.sb(st, f'pT{i}', [128, 256], BF16) for i in range(3)]
        osb = [self.sb(st, f'dosb{i}', [128, NCH, 129], F32) for i in range(2)]
        psT = [self.ps(st, f'psT3{i}', [128, 512], F32) for i in range(2)]
        psS = [self.ps(st, f'psS3{i}', [128, 512], F32) for i in range(3)]
        psO = [self.ps(st, f'psO3{i}', [128, 512], F32) for i in range(3)]
        for i in range(2):
            tk.op(dve, lambda e: e.memset(vaug[i][:], 1.0), writes=[vaug[i]])
        tmd, odd = d[f'tm{l}'], d[f'od{l}']
        alltm = [self.db('tm', l, gc) for gc in range(NCH)]
        tmv = tmd.rearrange("(c p) w -> p c w", p=128)
        it = 0
        nb = 0
        for g in range(3):
            Dl = DIL[g]
            nsp = S // (128 * Dl)
            tmr = tmd.rearrange("(m i r) w -> i m r w", i=128, r=Dl)
            for h in range(8):
                cq = TM_DQKV + g * 3072 + h * 128
                va, ob_ = vaug[it % 2], osb[it % 2]
                qtok, ktok, qT, kT = qtok_2[it % 2], ktok_2[it % 2], qT_2[it % 2], kT_2[it % 2]
                it += 1
                tk.dma(sp, out=qtok[:], in_=tmv[:, :, cq:cq + 128], reads=alltm, writes=[qtok])
                tk.dma(sp, out=ktok[:], in_=tmv[:, :, cq + 1024:cq + 1152], reads=alltm, writes=[ktok])
                rn = min(Dl, 8)
                for m in range(nsp):
                    for r0 in range(0, Dl, rn):
                        b0 = m * Dl + r0
                        tk.dma(sp, out=va[:, b0:b0 + rn, 0:128], in_=tmr[:, m, r0:r0 + rn, cq + 2048:cq + 2176], reads=alltm, writes=[va])
                for src, dst in ((qtok, qT), (ktok, kT)):
                    for c8 in range(NCH // 8):
                        p_ = psT[c8 % 2]
                        for i in range(8):
                            _tr(tk, p_, i, src[:, c8 * 8 + i, :], [src], self, last=(i == 7))
                        eng = act
                        if eng is act:
                            tk.op(act, lambda e: e.activation(out=dst[:, c8 * 1024:(c8 + 1) * 1024], in_=p_.t[:].bitcast(BF16), func=AF.Copy),
                                  reads=[p_], writes=[dst])
                        else:
                            tk.op(dve, lambda e: e.tensor_copy(out=dst[:, c8 * 1024:(c8 + 1) * 1024], in_=p_.t[:].bitcast(BF16)), reads=[p_], writes=[dst])
                for m in range(nsp):
                    for r in range(Dl):
                        bi = m * Dl + r
                        cur = slice(m * 128 * Dl + r, m * 128 * Dl + r + 127 * Dl + 1, Dl)
                        ps_, p_, po = psS[nb % 3], pT[nb % 3], psO[nb % 3]
                        nb += 1
                        lo = 0 if m > 0 else 128
                        if m > 0:
                            prv = slice((m - 1) * 128 * Dl + r, (m - 1) * 128 * Dl + r + 127 * Dl + 1, Dl)
                            tk.op(pe, lambda e: e.matmul(ps_[:, 0:128], lhsT=kT[:, prv], rhs=qT[:, cur], start=True, stop=False),
                                  reads=[kT, qT], writes=[ps_], inc=False)
                            tk.op(pe, lambda e: e.matmul(ps_[:, 0:128], lhsT=self.ident_b(), rhs=self.negprev_b(), start=False, stop=True),
                                  reads=[self.cbf], writes=[ps_], inc=False)
                        tk.op(pe, lambda e: e.matmul(ps_[:, 128:256], lhsT=kT[:, cur], rhs=qT[:, cur], start=True, stop=False),
                              reads=[kT, qT], writes=[ps_], inc=False)
                        tk.op(pe, lambda e: e.matmul(ps_[:, 128:256], lhsT=self.ident_b(), rhs=self.negcur_b(), start=False, stop=True),
                              reads=[self.cbf], writes=[ps_])
                        tk.op(act, lambda e: e.activation(out=p_[:, lo:256], in_=ps_[:, lo:256], func=AF.Exp), reads=[ps_], writes=[p_])
                        if m > 0:
                            tk.op(pe, lambda e: e.matmul(po[:, 0:129], lhsT=p_[:, 0:128], rhs=va[:, bi - Dl, 0:129], start=True, stop=False),
                                  reads=[p_, va], writes=[po], inc=False)
                        tk.op(pe, lambda e: e.matmul(po[:, 0:129], lhsT=p_[:, 128:256], rhs=va[:, bi, 0:129], start=(m == 0), stop=True),
                              reads=[p_, va], writes=[po])
                        tk.op(dve, lambda e: e.tensor_copy(out=ob_[:, bi, :], in_=po[:, 0:129]), reads=[po], writes=[ob_])
                odr = odd[g].rearrange("(m i r) h e -> i m r h e", i=128, r=Dl)
                for m in range(nsp):
                    for r0 in range(0, Dl, rn):
                        b0 = m * Dl + r0
                        tk.dma(pool, out=odr[:, m, r0:r0 + rn, h, :], in_=ob_[:, b0:b0 + rn, :], reads=[ob_], writes=[self.db('od', l)])
        tk.barrier()
        with ExitStack() as st2:
            o3 = [self.sb(st2, f'o3{i}', [128, 3, 8 * 129], F32) for i in range(2)]
            dgs = [self.sb(st2, f'dgs{i}', [128, 1024], BF16) for i in range(2)]
            rden = self.sb(st2, 'rden', [128, 8], F32)
            sm = self.sb(st2, 'sm3', [128, 8 * 129], F32)
            yn = self.sb(st2, 'yn3', [128, 8, 128], F32)
            yc = [self.sb(st2, f'yc{i}', [128, 1024], BF16) for i in range(2)]
            yd = d[f'y{l}']
            for gc in range(NCH):
                rows = slice(gc * 128, (gc + 1) * 128)
                o_, g_, y_ = o3[gc % 2], dgs[gc % 2], yc[gc % 2]
                tk.dma(sp, out=o_[:], in_=odd[:, rows, :, :].rearrange("g p h e -> p g (h e)"), reads=[self.db('od', l)], writes=[o_])
                tk.dma(sp, out=g_[:], in_=tmd[rows, TM_DG:TM_DG + 1024], reads=[self.db('tm', l, gc)], writes=[g_])
                tk.op(dve, lambda e: e.tensor_tensor(out=sm[:], in0=o_[:, 0, :], in1=o_[:, 1, :], op=ALU.add), reads=[o_], writes=[sm])
                tk.op(dve, lambda e: e.tensor_tensor(out=sm[:], in0=sm[:], in1=o_[:, 2, :], op=ALU.add), reads=[o_, sm], writes=[sm])
                sv = sm[:].rearrange("p (h e) -> p h e", h=8)
                tk.op(dve, lambda e: e.tensor_copy(out=rden[:].unsqueeze(2), in_=sv[:, :, 128:129]), reads=[sm], writes=[rden])
                tk.op(dve, lambda e: e.reciprocal(out=rden[:], in_=rden[:]), reads=[rden], writes=[rden])
                tk.op(dve, lambda e: e.tensor_tensor(out=yn[:], in0=sv[:, :, 0:128], in1=rden[:].unsqueeze(2).to_broadcast([128, 8, 128]), op=ALU.mult),
                      reads=[sm, rden], writes=[yn])
                if self.debug:
                    tk.dma(pool, out=d['dbgA'][rows, :], in_=sm[:], reads=[sm], writes=[self.db('dbg')])
                    tk.dma(pool, out=d['dbgB'][rows, :], in_=yn[:].rearrange("p h e -> p (h e)"), reads=[yn], writes=[self.db('dbg')])
                tk.op(pool, lambda e: e.tensor_tensor(out=y_[:].rearrange("p (h e) -> p h e", h=8), in0=yn[:],
                                                      in1=g_[:].rearrange("p (h e) -> p h e", h=8), op=ALU.mult), reads=[yn, g_], writes=[y_])
                tk.dma(pool, out=yd[rows, 4096:5120], in_=y_[:], reads=[y_], writes=[self.db('y', l, gc)])
            tk.barrier()


Prog.stage_B2 = stage_B2
Prog.stage_B3 = stage_B3


def stage_C(self, l, xin, xkey):
    tk, d, S, NCH = self.tk, self.dram, self.S, self.NCH
    pe, act, dve, pool, sp = tk.pe, tk.act, tk.dve, tk.pool, tk.sp
    NT = S // 512
    with ExitStack() as st:
        ytok = [self.sb(st, f'ytok{i}', [128, 5120], BF16) for i in range(2)]
        yT = self.sb(st, 'yT', [128, 40, 512], BF16)
        wa = [self.sb(st, f'wa{i}', [128, 40, 128], BF16) for i in range(2)]
        wo = [self.sb(st, f'wo{i}', [128, 16, 128], BF16) for i in range(2)]
        gt = [self.sb(st, f'gt{i}', [128, 3, 512], BF16) for i in range(2)]
        mT = self.sb(st, 'mT', [128, 16, 512], BF16)
        mf = [self.sb(st, f'mf{i}', [128, 512], F32) for i in range(2)]
        mg = [self.sb(st, f'mg{i}', [128, 512], F32) for i in range(2)]
        xt = [self.sb(st, f'xt{i}', [128, 512], F32) for i in range(2)]
        psT = [self.ps(st, f'psTc{i}', [128, 512], F32) for i in range(2)]
        psP = [self.ps(st, f'psP{i}', [128, 512], F32) for i in range(3)]
        psX = [self.ps(st, f'psX{i}', [128, 512], F32) for i in range(2)]
        yd, gtd, xnd = d[f'y{l}'], d[f'gT{l}'], d[f'xn{l}']
        wob = d[f'wob{l}'].rearrange("k p (j c) -> j p k c", c=128)
        gview = gtd.rearrange("(b j) p s -> j p b s", b=3)
        allwob = [self.db('wob', l, b) for b in range(56)]
        nt = 0
        for tt in range(NT):
            tok0 = tt * 512
            for c4 in range(4):
                gc = tt * 4 + c4
                y_ = ytok[gc % 2]
                tk.dma(sp, out=y_[:], in_=yd[gc * 128:(gc + 1) * 128, :], reads=[self.db('y', l, gc)], writes=[y_])
                for t8 in range(5):
                    p_ = psT[nt % 2]
                    nt += 1
                    for i in range(8):
                        _tr(tk, p_, i, y_[:, (t8 * 8 + i) * 128:(t8 * 8 + i + 1) * 128], [y_], self, last=(i == 7))
                    tk.op(act, lambda e: e.activation(out=yT[:, t8 * 8:(t8 + 1) * 8, c4 * 128:(c4 + 1) * 128],
                                                      in_=p_.t[:].bitcast(BF16).rearrange("p (t e) -> p t e", t=8), func=AF.Copy), reads=[p_], writes=[yT])
            for jj in range(16):
                w_, g_ = wa[jj % 2], gt[jj % 2]
                tk.dma(sp, out=w_[:], in_=wob[jj, :, 0:40, :], reads=allwob, writes=[w_])
                tk.dma(sp, out=g_[:], in_=gview[jj, :, :, tok0:tok0 + 512], reads=[self.db('gT', l, tt)], writes=[g_])
                for br, (k0, nk) in enumerate(((0, 16), (16, 16), (32, 8))):
                    for k in range(nk):
                        tk.op(pe, lambda e: e.matmul(psP[br][:], lhsT=w_[:, k0 + k, :], rhs=yT[:, k0 + k, :], start=(k == 0), stop=(k == nk - 1)),
                              reads=[w_, yT], writes=[psP[br]], inc=(k == nk - 1))
                f_, h_ = mf[jj % 2], mg[jj % 2]
                tk.op(dve, lambda e: e.tensor_tensor(out=f_[:], in0=psP[0][:], in1=g_[:, 0, :], op=ALU.mult), reads=[psP[0], g_], writes=[f_])
                tk.op(dve, lambda e: e.tensor_tensor(out=h_[:], in0=psP[1][:], in1=g_[:, 1, :], op=ALU.mult), reads=[psP[1], g_], writes=[h_])
                tk.op(pool, lambda e: e.tensor_tensor(out=f_[:], in0=f_[:], in1=h_[:], op=ALU.add), reads=[f_, h_], writes=[f_])
                tk.op(dve, lambda e: e.tensor_tensor(out=h_[:], in0=psP[2][:], in1=g_[:, 2, :], op=ALU.mult), reads=[psP[2], g_], writes=[h_])
                tk.op(pool, lambda e: e.tensor_tensor(out=mT[:, jj, :], in0=f_[:], in1=h_[:], op=ALU.add), reads=[f_, h_], writes=[mT])
            for jj in range(16):
                w_, x_, px = wo[jj % 2], xt[jj % 2], psX[jj % 2]
                tk.dma(sp, out=w_[:], in_=wob[jj, :, 40:56, :], reads=allwob, writes=[w_])
                tk.dma(sp, out=x_[:], in_=xin[jj * 128:(jj + 1) * 128, tok0:tok0 + 512], reads=[self.db(*xkey, tt)], writes=[x_])
                for k in range(16):
                    tk.op(pe, lambda e: e.matmul(px[:], lhsT=w_[:, k, :], rhs=mT[:, k, :], start=(k == 0), stop=(k == 15)),
                          reads=[w_, mT], writes=[px], inc=(k == 15))
                tk.op(dve, lambda e: e.tensor_tensor(out=x_[:], in0=px[:], in1=x_[:], op=ALU.add), reads=[px, x_], writes=[x_])
                tk.dma(pool, out=xnd[jj * 128:(jj + 1) * 128, tok0:tok0 + 512], in_=x_[:], reads=[x_], writes=[self.db('xn', l, tt)])
        tk.barrier()


def stage_final(self):
    tk, d, S, NCH = self.tk, self.dram, self.S, self.NCH
    pe, act, dve, pool, sp = tk.pe, tk.act, tk.dve, tk.pool, tk.sp
    l = self.depth - 1
    with ExitStack() as st:
        xs = [self.sb(st, f'fx{i}', [128, 16, 128], F32) for i in range(2)]
        sq = self.sb(st, 'fsq', [128, 16, 128], F32)
        rstd = self.sb(st, 'frstd', [128, 128], F32)
        gc_ = self.sb(st, 'fg', [128, 16], F32)
        pss = self.ps(st, 'fpss', [128, 128], F32)
        tk.dma(sp, out=gc_[:], in_=d['fgc'], writes=[gc_])
        xview = d[f'xn{l}'].rearrange("(k p) s -> p k s", p=128)
        oview = d['outT'].rearrange("(k p) s -> p k s", p=128)
        for gc in range(NCH):
            x_ = xs[gc % 2]
            cs = slice(gc * 128, (gc + 1) * 128)
            tk.dma(sp, out=x_[:], in_=xview[:, :, cs], reads=[self.db('xn', l, gc // 4)], writes=[x_])
            tk.op(act, lambda e: e.activation(out=sq[:], in_=x_[:], func=AF.Square), reads=[x_], writes=[sq])
            for k in range(16):
                tk.op(pe, lambda e: e.matmul(pss[:], lhsT=self.ones_f(), rhs=sq[:, k, :], start=(k == 0), stop=(k == 15)),
                      reads=[sq, self.c32], writes=[pss], inc=(k == 15))
            tk.op(act, lambda e: e.activation(out=rstd[:], in_=pss[:], func=AF.Sqrt, bias=EPS, scale=1.0 / D), reads=[pss], writes=[rstd])
            tk.op(dve, lambda e: e.reciprocal(out=rstd[:], in_=rstd[:]), reads=[rstd], writes=[rstd])
            for k in range(16):
                tk.op(dve, lambda e: e.scalar_tensor_tensor(out=x_[:, k, :], in0=x_[:, k, :], scalar=gc_[:, k:k + 1], in1=rstd[:],
                                                            op0=ALU.mult, op1=ALU.mult), reads=[x_, gc_, rstd], writes=[x_])
            tk.dma(pool, out=oview[:, :, cs], in_=x_[:], reads=[x_], writes=[self.db('out')])
        tk.barrier()


Prog.stage_C = stage_C
Prog.stage_final = stage_final
```

```python
import math
from contextlib import ExitStack

import numpy as np
import concourse.bass as bass
import concourse.mybir as mybir
from concourse.bass_utils import run_bass_kernel_spmd

F32 = mybir.dt.float32
BF16 = mybir.dt.bfloat16
AF = mybir.ActivationFunctionType
ALU = mybir.AluOpType

D = 2048
DEPTH = 2
EPS = 1e-6
NRING = 12
SAME_ENG_SYNC = False

SPL = dict(z=(0, 2048), xbc=(2048, 5120), dt=(5120, 5152), rq=(5152, 6176), rk=(6176, 7200),
           rv=(7200, 9248), rg=(9248, 11296), dqkv=(11296, 20512), dg=(20512, 21536), mg=(21536, 27680))
TM_Z, TM_RQ, TM_RK, TM_RV, TM_RG, TM_DQKV, TM_DG = 0, 2048, 3072, 4096, 6144, 8192, 17408
TM_W = 18432
N_TMBLK = 36
N_FMBLK = 18
N_BLK = N_TMBLK + 1 + N_FMBLK


def tm_block_kind(b):
    c = b * 512
    if c < TM_RQ:
        return 'silu'
    if c < TM_RK:
        return 'rope_q'
    if c < TM_RV:
        return 'rope_k'
    if c < TM_RG:
        return 'copy'
    if c < TM_DQKV:
        return 'silu'
    if c < TM_DG:
        j = ((c - TM_DQKV) // 1024) % 3
        return ('rope_q', 'rope_k', 'copy')[j]
    return 'silu'


class Buf:
    __slots__ = ('t', 'w', 'r', 'multi', 'small')

    def __init__(self, t=None, multi=False, small=False):
        self.t = t
        self.w = {}
        self.r = {}
        self.multi = multi
        self.small = small

    def __getitem__(self, idx):
        return self.t[idx]


class Eng:
    def __init__(self, name, e, sem):
        self.name, self.e, self.sem = name, e, sem
        self.cnt = 0
        self.seen = {}


class TK:
    def __init__(self, nc, es):
        self.nc = nc
        mk = lambda n: es.enter_context(nc.semaphore(n))
        self.pe = Eng('pe', nc.tensor, mk('s_pe'))
        self.act = Eng('act', nc.scalar, mk('s_act'))
        self.dve = Eng('dve', nc.vector, mk('s_dve'))
        self.pool = Eng('pool', nc.gpsimd, mk('s_pool'))
        self.sp = Eng('sp', nc.sync, mk('s_sp'))
        self.engs = [self.pe, self.act, self.dve, self.pool, self.sp]
        self.dq = {}
        for E in (self.sp, self.pool, self.act):
            self.dq[E.name] = dict(sems=[mk(f'd_{E.name}{i}') for i in range(NRING)], vals=[0] * NRING, i=0)

    def _wait(self, E, toks):
        for tk_ in toks:
            sem, val, owner = tk_[0], tk_[1], tk_[2]
            small = len(tk_) > 3 and tk_[3]
            if val <= 0:
                continue
            if owner is E and (E is self.pe or not (SAME_ENG_SYNC or small)):
                continue
            k = id(sem)
            if E.seen.get(k, 0) >= val:
                continue
            E.e.wait_ge(sem, val)
            E.seen[k] = val

    @staticmethod
    def _deps(reads, writes):
        toks = []
        for b in reads:
            toks += [t + (True,) for t in b.w.values()]
        for b in writes:
            toks += [t + (True,) for t in b.w.values()]
            toks += list(b.r.values())
        return toks

    @staticmethod
    def _record(tok, reads, writes):
        k = id(tok[0])
        for b in reads:
            o = b.r.get(k)
            if o is None or o[1] < tok[1]:
                b.r[k] = tok
        for b in writes:
            if b.multi:
                o = b.w.get(k)
                if o is None or o[1] < tok[1]:
                    b.w[k] = tok
            else:
                b.w = {k: tok}
                b.r = {}

    def op(self, E, fn, reads=(), writes=(), inc=True):
        self._wait(E, self._deps(reads, writes))
        ins = fn(E.e)
        if inc:
            E.cnt += 1
            ins.then_inc(E.sem, 1)
            tok = (E.sem, E.cnt, E)
        else:
            tok = (E.sem, E.cnt + 1, E)
        self._record(tok, reads, writes)
        return tok

    def dma(self, E, out, in_, reads=(), writes=()):
        q = self.dq[E.name]
        k = q['i'] % NRING
        q['i'] += 1
        sem = q['sems'][k]
        prev = q['vals'][k]
        toks = self._deps(reads, writes)
        toks.append((sem, prev, None))
        self._wait(E, toks)
        E.e.dma_start(out=out, in_=in_).then_inc(sem, 16)
        q['vals'][k] = prev + 16
        tok = (sem, prev + 16, None)
        self._record(tok, reads, writes)
        return tok

    def barrier(self):
        toks = [(E.sem, E.cnt, E) for E in self.engs if E.cnt]
        for q in self.dq.values():
            for sem, val in zip(q['sems'], q['vals']):
                toks.append((sem, val, None))
        for E in self.engs:
            self._wait(E, toks)


class Prog:
    def __init__(self, S, depth=DEPTH, debug=False, stages='ABC'):
        self.S, self.depth, self.debug, self.stages = S, depth, debug, stages
        self.NCH = S // 128
        nc = self.nc = bass.Bass("TRN2", target_bir_lowering=False)
        self.es = ExitStack()
        self.tk = TK(nc, self.es)
        self.dram = {}
        self.dbuf = {}

    def din(self, name, shape, dt=F32):
        self.dram[name] = self.nc.dram_tensor(name, list(shape), dt, kind="ExternalInput").ap()
        return self.dram[name]

    def dscr(self, name, shape, dt, out=False):
        kind = "ExternalOutput" if (out or self.debug) else "Internal"
        self.dram[name] = self.nc.dram_tensor(name, list(shape), dt, kind=kind).ap()
        return self.dram[name]

    def db(self, *key):
        b = self.dbuf.get(key)
        if b is None:
            b = self.dbuf[key] = Buf(None, multi=True)
        return b

    def _uid(self, name):
        self._n = getattr(self, '_n', 0) + 1
        return f'{name}_u{self._n}'

    def sb(self, st, name, shape, dt=F32):
        small = int(np.prod(shape[1:])) <= 512
        return Buf(st.enter_context(self.nc.sbuf_tensor(self._uid('s_' + name), list(shape), dt)), small=small)

    def ps(self, st, name, shape, dt=F32):
        return Buf(st.enter_context(self.nc.psum_tensor(self._uid('p_' + name), list(shape), dt)))

    def build(self):
        S, NCH, tk = self.S, self.NCH, self.tk
        nc = self.nc
        self.din('xT', [D, S])
        self.din('w_in', [self.depth, N_BLK, 128, 16, 512])
        self.din('w_o', [self.depth, 56, 128, 2048])
        self.din('cvec', [self.depth, 128, 16 + 24 * 5 + 32 * 3])
        self.din('grow', [self.depth, 128, 2048])
        self.din('fgc', [128, 16])
        self.din('rope', [128, NCH, 128])
        self.din('ctab', [128, 128 * 6 + 8 * 128 * 2 + 8])
        for l in range(self.depth):
            self.dscr(f'wb{l}', [N_BLK, 128, 16, 512], BF16)
            self.dscr(f'wob{l}', [56, 128, 2048], BF16)
            self.dscr(f'tm{l}', [S, TM_W], BF16)
            self.dscr(f'dt{l}', [S, 32], F32)
            self.dscr(f'xbcT{l}', [24, 128, S], BF16)
            self.dscr(f'gT{l}', [48, 128, S], BF16)
            self.dscr(f'y{l}', [S, 5120], BF16)
            self.dscr(f'od{l}', [3, S, 8, 129], F32)
            self.dscr(f'xn{l}', [D, S], F32)
        self.dscr('outT', [D, S], F32, out=True)
        if self.debug:
            self.dscr('dbgA', [S, 1032], F32)
            self.dscr('dbgB', [S, 1024], F32)
            self.dscr('dbgC', [S, 8], F32)

        with ExitStack() as gs:
            self.gs = gs
            self.consts(gs)
            self.cast_weights(0)
            for l in range(self.depth):
                xin = self.dram['xT'] if l == 0 else self.dram[f'xn{l - 1}']
                xkey = ('xT',) if l == 0 else ('xn', l - 1)
                if 'A' in self.stages:
                    self.stage_A(l, xin, xkey)
                if l + 1 < self.depth:
                    self.cast_weights(l + 1)
                if 'B' in self.stages or '1' in self.stages:
                    self.stage_B1(l)
                if 'B' in self.stages or '2' in self.stages:
                    self.stage_B2(l)
                if 'B' in self.stages or '3' in self.stages:
                    self.stage_B3(l)
                if 'C' in self.stages:
                    self.stage_C(l, xin, xkey)
            if 'C' in self.stages:
                self.stage_final()
            tk.barrier()
        self.es.close()
        return nc

    def consts(self, gs):
        tk, d = self.tk, self.dram
        self.c32 = self.sb(gs, 'c32', [128, 768], F32)
        self.cbf = self.sb(gs, 'cbf', [128, 768], BF16)
        tk.dma(tk.sp, out=self.c32[:], in_=d['ctab'][:, 0:768], writes=[self.c32])
        tk.op(tk.dve, lambda e: e.tensor_copy(out=self.cbf[:], in_=self.c32[:]), reads=[self.c32], writes=[self.cbf])
        self.ident_f = lambda: self.c32[:, 0:128]
        self.ones_f = lambda: self.c32[:, 128:256]
        self.U_f = lambda: self.c32[:, 256:384]
        self.Us_f = lambda: self.c32[:, 384:512]
        self.ident_b = lambda: self.cbf[:, 0:128]
        self.negcur_b = lambda: self.cbf[:, 512:640]
        self.negprev_b = lambda: self.cbf[:, 640:768]

    def cast_weights(self, l):
        tk, d = self.tk, self.dram
        if True:
            src = d['w_in'][l].rearrange("b p (q k) c -> b (p q) (k c)", q=4)
            dst = d[f'wb{l}'].rearrange("b p (q k) c -> b (p q) (k c)", q=4)
            for b in range(N_BLK):
                tk.dma(tk.pool, out=dst[b], in_=src[b], writes=[self.db('wb', l, b)])
            src = d['w_o'][l]
            dst = d[f'wob{l}']
            for b in range(56):
                tk.dma(tk.pool, out=dst[b], in_=src[b], writes=[self.db('wob', l, b)])

    def stage_A(self, l, xin, xkey):
        tk, d, S = self.tk, self.dram, self.S
        pe, act, dve, pool, sp = tk.pe, tk.act, tk.dve, tk.pool, tk.sp
        TS = 1024
        NSUP = S // TS
        with ExitStack() as st:
            hT = self.sb(st, 'hT', [128, 16, TS], BF16)
            xs = [self.sb(st, f'xs{i}', [128, 16, 128], F32) for i in range(2)]
            sq = self.sb(st, 'sq', [128, 16, 128], F32)
            rstd = self.sb(st, 'rstd', [128, 128], F32)
            wb = [self.sb(st, f'wb{i}', [128, 16, 512], BF16) for i in range(2)]
            ropet = self.sb(st, 'ropet', [128, 8, 128], F32)
            cv = self.sb(st, 'cvA', [128, 232], F32)
            halo = self.sb(st, 'halo', [128, 24, 3], F32)
            u = [self.sb(st, f'u{i}', [128, 515], F32) for i in range(2)]
            acc = [self.sb(st, f'acc{i}', [128, 512], F32) for i in range(2)]
            tsb = [self.sb(st, f'tsb{i}', [128, 512], F32) for i in range(2)]
            ra = [self.sb(st, f'ra{i}', [128, 512], F32) for i in range(2)]
            rb = [self.sb(st, f'rb{i}', [128, 512], F32) for i in range(2)]
            ob = [self.sb(st, f'ob{i}', [128, 512], BF16) for i in range(4)]
            dts = [self.sb(st, f'dts{i}', [128, 32], F32) for i in range(2)]
            psA = [self.ps(st, f'psA{i}', [128, 512], F32) for i in range(4)]
            pss = self.ps(st, 'pss', [128, 128], F32)

            tk.dma(sp, out=cv[:], in_=d['cvec'][l], writes=[cv])
            tk.op(dve, lambda e: e.memset(halo[:], 0.0), writes=[halo])
            xview = xin.rearrange("(k p) s -> p k s", p=128)
            tmd, dtd, xbd, gtd = d[f'tm{l}'], d[f'dt{l}'], d[f'xbcT{l}'], d[f'gT{l}']
            cnt = dict(ps=0, ob=0, ts=0, u=0, dt=0)

            for su in range(NSUP):
                for c8 in range(8):
                    gc = su * 8 + c8
                    x_ = xs[gc % 2]
                    tk.dma(sp, out=x_[:], in_=xview[:, :, gc * 128:(gc + 1) * 128],
                           reads=[self.db(*xkey, gc // 4)], writes=[x_])
                    tk.op(act, lambda e: e.activation(out=sq[:], in_=x_[:], func=AF.Square), reads=[x_], writes=[sq])
                    for k in range(16):
                        tk.op(pe, lambda e: e.matmul(pss[:], lhsT=self.ones_f(), rhs=sq[:, k, :], start=(k == 0), stop=(k == 15)),
                              reads=[sq, self.c32], writes=[pss], inc=(k == 15))
                    tk.op(act, lambda e: e.activation(out=rstd[:], in_=pss[:], func=AF.Sqrt, bias=EPS, scale=1.0 / D),
                          reads=[pss], writes=[rstd])
                    tk.op(dve, lambda e: e.reciprocal(out=rstd[:], in_=rstd[:]), reads=[rstd], writes=[rstd])
                    for k in range(16):
                        tk.op(dve, lambda e: e.scalar_tensor_tensor(out=hT[:, k, c8 * 128:(c8 + 1) * 128], in0=x_[:, k, :],
                                                                    scalar=cv[:, k:k + 1], in1=rstd[:], op0=ALU.mult, op1=ALU.mult),
                              reads=[x_, cv, rstd], writes=[hT])
                tk.dma(sp, out=ropet[:], in_=d['rope'][:, su * 8:(su + 1) * 8, :], writes=[ropet])

                for blk in range(N_BLK):
                    w_ = wb[blk % 2]
                    tk.dma(sp, out=w_[:], in_=d[f'wb{l}'][blk], reads=[self.db('wb', l, blk)], writes=[w_])
                    if blk <= N_TMBLK:
                        kind = 'dt' if blk == N_TMBLK else tm_block_kind(blk)
                        ncol = 32 if kind == 'dt' else 512
                        for c8 in range(8):
                            gc = su * 8 + c8
                            ps_ = psA[cnt['ps'] % 4]
                            cnt['ps'] += 1
                            for k in range(16):
                                tk.op(pe, lambda e: e.matmul(ps_[:, 0:ncol], lhsT=hT[:, k, c8 * 128:(c8 + 1) * 128], rhs=w_[:, k, 0:ncol],
                                                             start=(k == 0), stop=(k == 15)),
                                      reads=[hT, w_], writes=[ps_], inc=(k == 15))
                            rows = slice(gc * 128, (gc + 1) * 128)
                            if kind == 'dt':
                                t_ = dts[cnt['dt'] % 2]
                                cnt['dt'] += 1
                                tk.op(dve, lambda e: e.tensor_tensor(out=t_[:], in0=ps_[:, 0:32], in1=cv[:, 136:168], op=ALU.add),
                                      reads=[ps_, cv], writes=[t_])
                                tk.op(act, lambda e: e.activation(out=t_[:], in_=t_[:], func=AF.Exp), reads=[t_], writes=[t_])
                                tk.op(act, lambda e: e.activation(out=t_[:], in_=t_[:], func=AF.Ln, bias=1.0), reads=[t_], writes=[t_])
                                tk.dma(pool, out=dtd[rows, :], in_=t_[:], reads=[t_], writes=[self.db('dt', l, gc)])
                                continue
                            o_ = ob[cnt['ob'] % 4]
                            cnt['ob'] += 1
                            if kind == 'silu':
                                tk.op(act, lambda e: e.activation(out=o_[:], in_=ps_[:], func=AF.Silu), reads=[ps_], writes=[o_])
                            elif kind == 'copy':
                                tk.op(dve, lambda e: e.tensor_copy(out=o_[:], in_=ps_[:]), reads=[ps_], writes=[o_])
                            else:
                                j = cnt['ts'] % 2
                                cnt['ts'] += 1
                                t_, a_, b_ = tsb[j], ra[j], rb[j]
                                sc = 1.0 if kind == 'rope_q' else 128.0 ** -0.5
                                tk.op(act, lambda e: e.activation(out=t_[:], in_=ps_[:], func=AF.Copy, scale=sc), reads=[ps_], writes=[t_])
                                v4 = lambda b: b[:].rearrange("p (h two e) -> p h two e", h=4, two=2)
                                cosb = ropet[:, c8, 0:64].unsqueeze(1).unsqueeze(1).to_broadcast([128, 4, 2, 64])
                                sinb = ropet[:, c8, 64:128].unsqueeze(1).unsqueeze(1).to_broadcast([128, 4, 2, 64])
                                tk.op(dve, lambda e: e.tensor_tensor(out=v4(a_), in0=v4(t_), in1=cosb, op=ALU.mult), reads=[t_, ropet], writes=[a_])
                                tk.op(pool, lambda e: e.tensor_tensor(out=v4(b_), in0=v4(t_), in1=sinb, op=ALU.mult), reads=[t_, ropet], writes=[b_])
                                tk.op(dve, lambda e: e.tensor_tensor(out=v4(o_)[:, :, 0, :], in0=v4(a_)[:, :, 0, :], in1=v4(b_)[:, :, 1, :], op=ALU.subtract),
                                      reads=[a_, b_], writes=[o_])
                                tk.op(pool, lambda e: e.tensor_tensor(out=v4(o_)[:, :, 1, :], in0=v4(a_)[:, :, 1, :], in1=v4(b_)[:, :, 0, :], op=ALU.add),
                                      reads=[a_, b_], writes=[o_])
                            tk.dma(pool, out=tmd[rows, blk * 512:(blk + 1) * 512], in_=o_[:], reads=[o_], writes=[self.db('tm', l, gc)])
                    else:
                        fb = blk - N_TMBLK - 1
                        for ct4 in range(4):
                            ct = fb * 4 + ct4
                            for tt in range(2):
                                ps_ = psA[cnt['ps'] % 4]
                                cnt['ps'] += 1
                                for k in range(16):
                                    tk.op(pe, lambda e: e.matmul(ps_[:], lhsT=w_[:, k, ct4 * 128:(ct4 + 1) * 128], rhs=hT[:, k, tt * 512:(tt + 1) * 512],
                                                                 start=(k == 0), stop=(k == 15)),
                                          reads=[hT, w_], writes=[ps_], inc=(k == 15))
                                tok0 = su * TS + tt * 512
                                o_ = ob[cnt['ob'] % 4]
                                cnt['ob'] += 1
                                if ct >= 24:
                                    tk.op(act, lambda e: e.activation(out=o_[:], in_=ps_[:], func=AF.Sigmoid), reads=[ps_], writes=[o_])
                                    tk.dma(pool, out=gtd[ct - 24, :, tok0:tok0 + 512], in_=o_[:], reads=[o_], writes=[self.db('gT', l, tok0 // 512)])
                                    continue
                                u_ = u[cnt['u'] % 2]
                                a_ = acc[cnt['u'] % 2]
                                cnt['u'] += 1
                                tk.op(dve, lambda e: e.tensor_copy(out=u_[:, 0:3], in_=halo[:, ct, :]), reads=[halo], writes=[u_])
                                tk.op(act, lambda e: e.activation(out=u_[:, 3:515], in_=ps_[:], func=AF.Copy), reads=[ps_], writes=[u_])
                                tk.op(dve, lambda e: e.tensor_copy(out=halo[:, ct, :], in_=u_[:, 512:515]), reads=[u_], writes=[halo])
                                cw = lambda kk: cv[:, 16 + ct * 5 + kk:16 + ct * 5 + kk + 1]
                                tk.op(dve, lambda e: e.tensor_scalar(out=a_[:], in0=u_[:, 3:515], scalar1=cw(3), scalar2=cw(4), op0=ALU.mult, op1=ALU.add),
                                      reads=[u_, cv], writes=[a_])
                                for kk in range(3):
                                    tk.op(dve, lambda e: e.scalar_tensor_tensor(out=a_[:], in0=u_[:, kk:kk + 512], scalar=cw(kk), in1=a_[:],
                                                                                op0=ALU.mult, op1=ALU.add), reads=[u_, cv, a_], writes=[a_])
                                tk.op(act, lambda e: e.activation(out=o_[:], in_=a_[:], func=AF.Silu), reads=[a_], writes=[o_])
                                tk.dma(pool, out=xbd[ct, :, tok0:tok0 + 512], in_=o_[:], reads=[o_], writes=[self.db('xbcT', l, tok0 // 512)])
            tk.barrier()


def _blocks(w, nblk):
    return np.ascontiguousarray(w.reshape(16, 128, nblk, 512).transpose(2, 1, 0, 3))


def prep_weights(inp, depth=DEPTH):
    w_in_l, w_o_l, cvec_l, grow_l = [], [], [], []
    for l in range(depth):
        w = inp['w_in'][l]
        sl = lambda n: w[:, SPL[n][0]:SPL[n][1]]
        tm = np.concatenate([sl('z'), sl('rq'), sl('rk'), sl('rv'), sl('rg'), sl('dqkv'), sl('dg')], axis=1)
        dtp = np.zeros((D, 512), np.float32)
        dtp[:, 0:32] = sl('dt')
        fm = np.concatenate([sl('xbc'), sl('mg')], axis=1)
        w_in_l.append(np.concatenate([_blocks(tm, N_TMBLK), _blocks(dtp, 1), _blocks(fm, N_FMBLK)], axis=0))
        wo = np.concatenate([inp['w_o_ssd'][l], inp['w_o_ret'][l], inp['w_o_dil'][l], inp['w_out'][l]], axis=0)
        w_o_l.append(np.ascontiguousarray(wo.reshape(56, 128, 2048)))
        cv = np.zeros((128, 232), np.float32)
        cv[:, 0:16] = inp['norm_g'][l].reshape(16, 128).T
        cw = inp['conv_w'][l].reshape(4, 24, 128)
        cb = inp['conv_b'][l].reshape(24, 128)
        for ct in range(24):
            cv[:, 16 + ct * 5:16 + ct * 5 + 4] = cw[:, ct, :].T
            cv[:, 16 + ct * 5 + 4] = cb[ct]
        cv[:, 136:168] = inp['dt_bias'][l][None, :]
        cv[:, 168:200] = inp['a_log'][l][None, :]
        cv[:, 200:232] = inp['d_skip'][l][None, :]
        cvec_l.append(cv)
        grow_l.append(np.ascontiguousarray(np.broadcast_to(inp['ssd_norm_g'][l][None, :], (128, 2048))))
    fgc = np.ascontiguousarray(inp['final_norm_g'].reshape(16, 128).T)
    return dict(w_in=np.stack(w_in_l), w_o=np.stack(w_o_l), cvec=np.stack(cvec_l), grow=np.stack(grow_l), fgc=fgc)


def prep_consts(S):
    NCH = S // 128
    pos = np.arange(S, dtype=np.float32)
    inv = (10000.0 ** (-np.arange(0, 128, 2, dtype=np.float32) / 128.0)).astype(np.float32)
    ang = pos[:, None] * inv[None, :]
    rope = np.concatenate([np.cos(ang), np.sin(ang)], axis=1).astype(np.float32)
    rope = np.ascontiguousarray(rope.reshape(NCH, 128, 128).transpose(1, 0, 2))
    i = np.arange(128)
    ct = np.zeros((128, 768 + 2048 + 8), np.float32)
    ct[:, 0:128] = np.eye(128)
    ct[:, 128:256] = 1.0
    ct[:, 256:384] = (i[:, None] <= i[None, :])
    ct[:, 384:512] = (i[:, None] > i[None, :])
    ct[:, 512:640] = np.where(i[None, :] >= i[:, None], 0.0, -30000.0)
    ct[:, 640:768] = np.where(i[:, None] >= i[None, :], 0.0, -30000.0)
    lg = np.log(1.0 - np.exp2(-5.0 - np.arange(8, dtype=np.float64)))
    for h in range(8):
        rel = (i[None, :] - i[:, None]).astype(np.float64)
        ct[:, 768 + h * 128:768 + (h + 1) * 128] = np.where(rel >= 0, np.exp(np.maximum(rel, 0) * lg[h]), 0.0)
        ct[:, 1792 + h * 128:1792 + (h + 1) * 128] = np.exp((i[None, :] + 1.0) * lg[h])
        ct[:, 2816 + h] = np.exp((127.0 - i) * lg[h])
    return dict(rope=rope, ctab=ct)


_CACHE = {}


def kernel(**inp):
    x = np.asarray(inp['x'], np.float32)
    B, S, _ = x.shape
    if 'nc' not in _CACHE:
        _CACHE['nc'] = Prog(S).build()
    nc = _CACHE['nc']
    shared = dict(prep_weights(inp))
    shared.update(prep_consts(S))
    in_maps = []
    for b in range(B):
        m = dict(shared)
        m['xT'] = np.ascontiguousarray(x[b].T)
        in_maps.append(m)
    res = run_bass_kernel_spmd(nc, in_maps, core_ids=list(range(B)))
    out = np.stack([np.ascontiguousarray(r['outT'].T) for r in res.results], axis=0)
    return out.astype(np.float32)


def _tr(tk, ps_bf, slot, src_ap, src_bufs, P, last=True):
    tk.op(tk.pe, lambda e: e.transpose(out=ps_bf.t[:].bitcast(BF16)[:, slot * 128:(slot + 1) * 128], in_=src_ap, identity=P.ident_b()),
          reads=list(src_bufs) + [P.cbf], writes=[ps_bf], inc=last)


def stage_B1(self, l):
    tk, d, S, NCH = self.tk, self.dram, self.S, self.NCH
    pe, act, dve, pool, sp = tk.pe, tk.act, tk.dve, tk.pool, tk.sp
    with ExitStack() as st:
        xbl = self.sb(st, 'xbl', [128, 24, 512], BF16)
        x_tok_2 = [self.sb(st, f'x_tok{i}', [128, 2048], BF16) for i in range(2)]
        B_tok_2 = [self.sb(st, f'B_tok{i}', [128, 512], BF16) for i in range(2)]
        zs_2 = [self.sb(st, f'zs{i}', [128, 2048], BF16) for i in range(2)]
        xdt_2 = [self.sb(st, f'xdt{i}', [128, 32, 64], BF16) for i in range(2)]
        xdte_2 = [self.sb(st, f'xdte{i}', [128, 32, 64], BF16) for i in range(2)]
        lall = self.sb(st, 'lall', [128, 32, 128], F32)
        cbm_2 = [self.sb(st, f'cbm{i}', [128, 4, 128], F32) for i in range(2)]
        Lm = [self.sb(st, f'Lm{i}', [128, 4, 128], F32) for i in range(2)]
        M_2 = [self.sb(st, f'M{i}', [128, 32, 128], BF16) for i in range(2)]
        yt_2 = [self.sb(st, f'yt{i}', [128, 2048], F32) for i in range(2)]
        tmp = self.sb(st, 'tmp', [128, 2048], F32)
        Hs = self.sb(st, 'Hs', [128, 2048], F32)
        Hb = self.sb(st, 'Hb', [128, 2048], BF16)
        grow = self.sb(st, 'grow', [128, 2048], F32)
        ya_2 = [self.sb(st, f'ya{i}', [128, 2048], BF16) for i in range(2)]
        cv = self.sb(st, 'cvB', [128, 232], F32)
        arow = self.sb(st, 'arow', [128, 32], F32)
        dt_t_2 = [self.sb(st, f'dt_t{i}', [128, 32], F32) for i in range(2)]
        dta_2 = [self.sb(st, f'dta{i}', [128, 32], F32) for i in range(2)]
        sS_2 = [self.sb(st, f'sS{i}', [128, 64], F32) for i in range(2)]
        eacs_2 = [self.sb(st, f'eacs{i}', [128, 32], F32) for i in range(2)]
        tend_2 = [self.sb(st, f'tend{i}', [128, 32], F32) for i in range(2)]
        cdr_2 = [self.sb(st, f'cdr{i}', [128, 32], F32) for i in range(2)]
        ss_2 = [self.sb(st, f'ss{i}', [128, 1], F32) for i in range(2)]
        psT = self.ps(st, 'psT', [128, 512], F32)
        psS = self.ps(st, 'psS', [128, 64], F32)
        psC = self.ps(st, 'psC', [128, 512], F32)
        psG = [self.ps(st, f'psG{i}', [128, 512], F32) for i in range(2)]
        psD = self.ps(st, 'psD', [128, 512], F32)
        psO = self.ps(st, 'psO', [128, 512], F32)
        psN = self.ps(st, 'psN', [128, 512], F32)

        tk.dma(sp, out=cv[:], in_=d['cvec'][l], writes=[cv])
        tk.dma(sp, out=grow[:], in_=d['grow'][l], writes=[grow])
        tk.op(act, lambda e: e.activation(out=arow[:], in_=cv[:, 168:200], func=AF.Exp), reads=[cv], writes=[arow])
        tk.op(dve, lambda e: e.tensor_scalar(out=arow[:], in0=arow[:], scalar1=-1.0, scalar2=None, op0=ALU.mult), reads=[arow], writes=[arow])
        tk.op(dve, lambda e: e.memset(Hs[:], 0.0), writes=[Hs])
        tk.op(dve, lambda e: e.memset(Hb[:], 0.0), writes=[Hb])
        tmd, dtd, xbd, yd = d[f'tm{l}'], d[f'dt{l}'], d[f'xbcT{l}'], d[f'y{l}']
        bc3 = lambda ap, n: ap.unsqueeze(2).to_broadcast([128, ap.shape[1], n])
        for gc in range(NCH):
            rows = slice(gc * 128, (gc + 1) * 128)
            sub = gc % 4
            x_tok = x_tok_2[gc % 2]
            B_tok = B_tok_2[gc % 2]
            zs = zs_2[gc % 2]
            xdt = xdt_2[gc % 2]
            xdte = xdte_2[gc % 2]
            cbm = cbm_2[gc % 2]
            M = M_2[gc % 2]
            yt = yt_2[gc % 2]
            ya = ya_2[gc % 2]
            dt_t = dt_t_2[gc % 2]
            dta = dta_2[gc % 2]
            sS = sS_2[gc % 2]
            eacs = eacs_2[gc % 2]
            tend = tend_2[gc % 2]
            cdr = cdr_2[gc % 2]
            ss = ss_2[gc % 2]
            ts_ = slice(sub * 128, (sub + 1) * 128)
            if sub == 0:
                tk.dma(sp, out=xbl[:], in_=xbd[:, :, gc * 128:gc * 128 + 512].rearrange("t p s -> p t s"),
                       reads=[self.db('xbcT', l, gc // 4)], writes=[xbl])
            tk.dma(sp, out=dt_t[:], in_=dtd[rows, :], reads=[self.db('dt', l, gc)], writes=[dt_t])
            tk.dma(sp, out=zs[:], in_=tmd[rows, TM_Z:TM_Z + 2048], reads=[self.db('tm', l, gc)], writes=[zs])
            for half in range(2):
                for i in range(8):
                    _tr(tk, psT, i, xbl[:, half * 8 + i, ts_], [xbl], self, last=(i == 7))
                tk.op(act, lambda e: e.activation(out=x_tok[:, half * 1024:(half + 1) * 1024], in_=psT.t[:].bitcast(BF16), func=AF.Copy),
                      reads=[psT], writes=[x_tok])
            for i in range(4):
                _tr(tk, psT, i, xbl[:, 16 + i, ts_], [xbl], self, last=(i == 3))
            tk.op(act, lambda e: e.activation(out=B_tok[:], in_=psT.t[:].bitcast(BF16)[:, 0:512], func=AF.Copy), reads=[psT], writes=[B_tok])
            tk.op(dve, lambda e: e.tensor_tensor(out=dta[:], in0=dt_t[:], in1=arow[:], op=ALU.mult), reads=[dt_t, arow], writes=[dta])
            tk.op(pe, lambda e: e.matmul(psS[:, 0:32], lhsT=self.U_f(), rhs=dta[:], start=True, stop=True), reads=[dta, self.c32], writes=[psS], inc=False)
            tk.op(pe, lambda e: e.matmul(psS[:, 32:64], lhsT=self.ones_f(), rhs=dta[:], start=True, stop=True), reads=[dta, self.c32], writes=[psS])
            tk.op(act, lambda e: e.activation(out=sS[:], in_=psS[:], func=AF.Copy), reads=[psS], writes=[sS])
            tk.op(act, lambda e: e.activation(out=eacs[:], in_=sS[:, 0:32], func=AF.Exp), reads=[sS], writes=[eacs])
            tk.op(dve, lambda e: e.tensor_tensor(out=tend[:], in0=sS[:, 32:64], in1=sS[:, 0:32], op=ALU.subtract), reads=[sS], writes=[tend])
            tk.op(act, lambda e: e.activation(out=tend[:], in_=tend[:], func=AF.Exp), reads=[tend], writes=[tend])
            tk.op(act, lambda e: e.activation(out=cdr[:], in_=sS[:, 32:64], func=AF.Exp), reads=[sS], writes=[cdr])
            xv = x_tok[:].rearrange("p (h e) -> p h e", h=32)
            tk.op(dve, lambda e: e.tensor_tensor(out=xdt[:], in0=xv, in1=bc3(dt_t[:], 64), op=ALU.mult), reads=[x_tok, dt_t], writes=[xdt])
            tk.op(pool, lambda e: e.tensor_tensor(out=xdte[:], in0=xdt[:], in1=bc3(tend[:], 64), op=ALU.mult), reads=[xdt, tend], writes=[xdte])
            usb = self.Us_f().unsqueeze(1).to_broadcast([128, 16, 128])
            tk.op(dve, lambda e: e.tensor_tensor(out=lall[:, 0:16, :], in0=bc3(dta[:, 0:16], 128), in1=usb, op=ALU.mult), reads=[dta, self.c32], writes=[lall])
            tk.op(pool, lambda e: e.tensor_tensor(out=lall[:, 16:32, :], in0=bc3(dta[:, 16:32], 128), in1=usb, op=ALU.mult), reads=[dta, self.c32], writes=[lall])
            for g in range(4):
                tk.op(pe, lambda e: e.matmul(psC[:, g * 128:(g + 1) * 128], lhsT=xbl[:, 16 + g, ts_], rhs=xbl[:, 20 + g, ts_], start=True, stop=True),
                      reads=[xbl], writes=[psC], inc=(g == 3))
            ub = self.U_f().unsqueeze(1).to_broadcast([128, 4, 128])
            tk.op(dve, lambda e: e.tensor_tensor(out=cbm[:], in0=psC[:].rearrange("p (g e) -> p g e", g=4), in1=ub, op=ALU.mult), reads=[psC, self.c32], writes=[cbm])
            for hq in range(8):
                pg, lm = psG[hq % 2], Lm[hq % 2]
                for i in range(4):
                    tk.op(pe, lambda e: e.matmul(pg[:, i * 128:(i + 1) * 128], lhsT=lall[:, hq * 4 + i, :], rhs=self.U_f(), start=True, stop=True),
                          reads=[lall, self.c32], writes=[pg], inc=(i == 3))
                tk.op(act, lambda e: e.activation(out=lm[:], in_=pg[:].rearrange("p (g e) -> p g e", g=4), func=AF.Exp), reads=[pg], writes=[lm])
                cb_b = cbm[:, hq // 2, :].unsqueeze(1).to_broadcast([128, 4, 128])
                tk.op(dve, lambda e: e.tensor_tensor(out=M[:, hq * 4:(hq + 1) * 4, :], in0=lm[:], in1=cb_b, op=ALU.mult), reads=[lm, cbm], writes=[M])
            for g in range(4):
                gs_ = slice(g * 512, (g + 1) * 512)
                for i in range(8):
                    h = g * 8 + i
                    tk.op(pe, lambda e: e.matmul(psD[:, i * 64:(i + 1) * 64], lhsT=M[:, h, :], rhs=xdt[:, h, :], start=True, stop=True),
                          reads=[M, xdt], writes=[psD], inc=(i == 7))
                tk.op(pe, lambda e: e.matmul(psO[:], lhsT=xbl[:, 20 + g, ts_], rhs=Hb[:, gs_], start=True, stop=True), reads=[xbl, Hb], writes=[psO])
                tk.op(pe, lambda e: e.matmul(psN[:], lhsT=B_tok[:, g * 128:(g + 1) * 128], rhs=xdte[:, g * 8:(g + 1) * 8, :], start=True, stop=True),
                      reads=[B_tok, xdte], writes=[psN])
                v8 = lambda ap: ap.rearrange("p (h e) -> p h e", h=8)
                tk.op(dve, lambda e: e.tensor_tensor(out=v8(yt[:, gs_]), in0=v8(psO[:]), in1=bc3(eacs[:, g * 8:(g + 1) * 8], 64), op=ALU.mult),
                      reads=[psO, eacs], writes=[yt])
                tk.op(dve, lambda e: e.tensor_tensor(out=yt[:, gs_], in0=psD[:], in1=yt[:, gs_], op=ALU.add), reads=[psD, yt], writes=[yt])
                tk.op(pool, lambda e: e.tensor_tensor(out=v8(Hs[:, gs_]), in0=v8(Hs[:, gs_]), in1=bc3(cdr[:, g * 8:(g + 1) * 8], 64), op=ALU.mult),
                      reads=[Hs, cdr], writes=[Hs])
                tk.op(dve, lambda e: e.tensor_tensor(out=Hs[:, gs_], in0=psN[:], in1=Hs[:, gs_], op=ALU.add), reads=[psN, Hs], writes=[Hs])
                tk.op(act, lambda e: e.activation(out=Hb[:, gs_], in_=Hs[:, gs_], func=AF.Copy), reads=[Hs], writes=[Hb])
            tk.op(pool, lambda e: e.tensor_tensor(out=tmp[:].rearrange("p (h e) -> p h e", h=32), in0=xv, in1=bc3(cv[:, 200:232], 64), op=ALU.mult),
                  reads=[x_tok, cv], writes=[tmp])
            tk.op(pool, lambda e: e.tensor_tensor(out=yt[:], in0=yt[:], in1=tmp[:], op=ALU.add), reads=[yt, tmp], writes=[yt])
            tk.op(pool, lambda e: e.tensor_tensor(out=yt[:], in0=yt[:], in1=zs[:], op=ALU.mult), reads=[yt, zs], writes=[yt])
            tk.op(act, lambda e: e.activation(out=tmp[:], in_=yt[:], func=AF.Square, accum_out=ss[:]), reads=[yt], writes=[tmp, ss])
            tk.op(act, lambda e: e.activation(out=ss[:], in_=ss[:], func=AF.Sqrt, bias=EPS, scale=1.0 / 2048), reads=[ss], writes=[ss])
            tk.op(dve, lambda e: e.reciprocal(out=ss[:], in_=ss[:]), reads=[ss], writes=[ss])
            tk.op(dve, lambda e: e.scalar_tensor_tensor(out=ya[:], in0=yt[:], scalar=ss[:, 0:1], in1=grow[:], op0=ALU.mult, op1=ALU.mult),
                  reads=[yt, ss, grow], writes=[ya])
            tk.dma(pool, out=yd[rows, 0:2048], in_=ya[:], reads=[ya], writes=[self.db('y', l, gc)])
        tk.barrier()


Prog.stage_B1 = stage_B1


def gen_B2(self, l, st):
    tk, d, S, NCH = self.tk, self.dram, self.S, self.NCH
    pe, act, dve, pool, sp = tk.pe, tk.act, tk.dve, tk.pool, tk.sp
    cdh = [float(np.exp(128.0 * np.log(1.0 - 2.0 ** (-5.0 - h)))) for h in range(8)]
    if True:
        rt = [self.sb(st, f'rt{i}', [128, 6144], BF16) for i in range(2)]
        tab = self.sb(st, 'rtab', [128, 2056], F32)
        qT_2 = [self.sb(st, f'qT{i}', [128, 8, 128], BF16) for i in range(2)]
        qTd_2 = [self.sb(st, f'qTd{i}', [128, 8, 128], BF16) for i in range(2)]
        kT_2 = [self.sb(st, f'kT{i}', [128, 8, 128], BF16) for i in range(2)]
        kdk_2 = [self.sb(st, f'kdk{i}', [128, 8, 128], BF16) for i in range(2)]
        ST = [self.sb(st, f'ST{i}', [128, 4, 128], BF16) for i in range(2)]
        osb_2 = [self.sb(st, 'osbS', [128, 2048], F32)] * 2
        junk = self.sb(st, 'junk', [128, 256], F32)
        ss8_2 = [self.sb(st, f'ss8{i}', [128, 8], F32) for i in range(2)]
        Rs = self.sb(st, 'Rs', [128, 2048], F32)
        Rb = self.sb(st, 'Rb', [128, 2048], BF16)
        yb_2 = [self.sb(st, f'yb{i}', [128, 2048], BF16) for i in range(2)]
        psT = self.ps(st, 'psT2', [128, 512], F32)
        psSc = [self.ps(st, 'psScS', [128, 512], F32)] * 2
        psOb = self.ps(st, 'psO2b', [128, 512], F32)
        psKb = self.ps(st, 'psK2b', [128, 512], F32)
        psO = [psOb, psOb]
        psK = [psKb, psKb]
        tk.dma(sp, out=tab[:], in_=d['ctab'][:, 768:768 + 2056], writes=[tab])
        tk.op(dve, lambda e: e.memset(Rs[:], 0.0), writes=[Rs])
        tk.op(dve, lambda e: e.memset(Rb[:], 0.0), writes=[Rb])
        tmd, yd = d[f'tm{l}'], d[f'y{l}']
        psTb = lambda: psT.t[:].bitcast(BF16)
        for gc in range(NCH):
            rows = slice(gc * 128, (gc + 1) * 128)
            r_ = rt[gc % 2]
            qT = qT_2[gc % 2]
            qTd = qTd_2[gc % 2]
            kT = kT_2[gc % 2]
            kdk = kdk_2[gc % 2]
            osb = osb_2[gc % 2]
            ss8 = ss8_2[gc % 2]
            yb = yb_2[gc % 2]
            tk.dma(sp, out=r_[:], in_=tmd[rows, TM_RQ:TM_DQKV], reads=[self.db('tm', l, gc)], writes=[r_])
            q_tok = lambda h: r_[:, h * 128:(h + 1) * 128]
            k_tok = lambda h: r_[:, 1024 + h * 128:1024 + (h + 1) * 128]
            v_tok = lambda h: r_[:, 2048 + h * 256:2048 + (h + 1) * 256]
            rgs = lambda h: r_[:, 4096 + h * 256:4096 + (h + 1) * 256]
            for h in range(8):
                _tr(tk, psT, h, q_tok(h), [r_], self, last=(h == 7))
            tk.op(act, lambda e: e.activation(out=qT[:].rearrange("p h e -> p (h e)"), in_=psTb(), func=AF.Copy), reads=[psT], writes=[qT])
            tk.op(dve, lambda e: e.tensor_tensor(out=qTd[:].rearrange("p h e -> p (h e)"), in0=qT[:].rearrange("p h e -> p (h e)"), in1=tab[:, 1024:2048], op=ALU.mult),
                  reads=[qT, tab], writes=[qTd])
            for h in range(8):
                _tr(tk, psT, h, k_tok(h), [r_], self, last=(h == 7))
            tk.op(act, lambda e: e.activation(out=kT[:].rearrange("p h e -> p (h e)"), in_=psTb(), func=AF.Copy), reads=[psT], writes=[kT])
            tk.op(dve, lambda e: e.tensor_tensor(out=kdk[:], in0=r_[:, 1024:2048].rearrange("p (h e) -> p h e", h=8),
                                                  in1=tab[:, 2048:2056].unsqueeze(2).to_broadcast([128, 8, 128]), op=ALU.mult),
                  reads=[r_, tab], writes=[kdk])
            yield
            for hq in range(2):
                pc, st_ = psSc[hq], ST[hq]
                for i in range(4):
                    h = hq * 4 + i
                    tk.op(pe, lambda e: e.matmul(pc[:, i * 128:(i + 1) * 128], lhsT=kT[:, h, :], rhs=qT[:, h, :], start=True, stop=True),
                          reads=[kT, qT], writes=[pc], inc=(i == 3))
                tk.op(dve, lambda e: e.tensor_tensor(out=st_[:].rearrange("p h e -> p (h e)"), in0=pc[:], in1=tab[:, hq * 512:(hq + 1) * 512], op=ALU.mult),
                      reads=[pc, tab], writes=[st_])
            yield
            for h in range(8):
                po, pk = psO[h % 2], psK[h % 2]
                hs = slice(h * 256, (h + 1) * 256)
                tk.op(pe, lambda e: e.matmul(po[:, 0:256], lhsT=ST[h // 4][:, h % 4, :], rhs=v_tok(h), start=True, stop=False),
                      reads=[ST[h // 4], r_], writes=[po], inc=False)
                tk.op(pe, lambda e: e.matmul(po[:, 0:256], lhsT=qTd[:, h, :], rhs=Rb[:, hs], start=False, stop=True), reads=[qTd, Rb], writes=[po])
                tk.op(pe, lambda e: e.matmul(pk[:, 0:256], lhsT=kdk[:, h, :], rhs=v_tok(h), start=True, stop=True), reads=[kdk, r_], writes=[pk])
                tk.op(dve, lambda e: e.tensor_copy(out=osb[:, hs], in_=po[:, 0:256]), reads=[po], writes=[osb])
                tk.op(act, lambda e: e.activation(out=junk[:], in_=osb[:, hs], func=AF.Square, accum_out=ss8[:, h:h + 1]), reads=[osb], writes=[junk, ss8])
                tk.op(dve, lambda e: e.tensor_scalar(out=Rs[:, hs], in0=Rs[:, hs], scalar1=cdh[h], scalar2=None, op0=ALU.mult), reads=[Rs], writes=[Rs])
                tk.op(dve, lambda e: e.tensor_tensor(out=Rs[:, hs], in0=pk[:, 0:256], in1=Rs[:, hs], op=ALU.add), reads=[Rs, pk], writes=[Rs])
                tk.op(act, lambda e: e.activation(out=Rb[:, hs], in_=Rs[:, hs], func=AF.Copy), reads=[Rs], writes=[Rb])
                yield
            tk.op(act, lambda e: e.activation(out=ss8[:], in_=ss8[:], func=AF.Sqrt, bias=EPS, scale=1.0 / 256), reads=[ss8], writes=[ss8])
            tk.op(dve, lambda e: e.reciprocal(out=ss8[:], in_=ss8[:]), reads=[ss8], writes=[ss8])
            for h in range(8):
                hs = slice(h * 256, (h + 1) * 256)
                tk.op(dve, lambda e: e.scalar_tensor_tensor(out=yb[:, hs], in0=osb[:, hs], scalar=ss8[:, h:h + 1], in1=rgs(h), op0=ALU.mult, op1=ALU.mult),
                      reads=[osb, ss8, r_], writes=[yb])
            tk.dma(pool, out=yd[rows, 2048:4096], in_=yb[:], reads=[yb], writes=[self.db('y', l, gc)])
            yield


def gen_B3(self, l, st):
    tk, d, S, NCH = self.tk, self.dram, self.S, self.NCH
    pe, act, dve, pool, sp = tk.pe, tk.act, tk.dve, tk.pool, tk.sp
    DIL = (1, 4, 16)
    if True:
        qtok_2 = [self.sb(st, 'qtokS', [128, NCH, 128], BF16)] * 2
        ktok_2 = [self.sb(st, 'ktokS', [128, NCH, 128], BF16)] * 2
        qT_2 = [self.sb(st, f'dqT{i}', [128, S], BF16) for i in range(2)]
        kT_2 = [self.sb(st, f'dkT{i}', [128, S], BF16) for i in range(2)]
        vaug = [self.sb(st, f'vaug{i}', [128, NCH, 130], BF16) for i in range(2)]
        pT = [self.sb(st, f'pT{i}', [128, 256], BF16) for i in range(3)]
        osb = [self.sb(st, 'dosbS', [128, NCH, 129], F32)] * 2
        psTb = self.ps(st, 'psT3', [128, 512], F32)
        psT = [psTb, psTb]
        psSb = self.ps(st, 'psS3', [128, 512], F32)
        psS = [psSb, psSb]
        psOb = self.ps(st, 'psO3', [128, 512], F32)
        psO = [psOb, psOb, psOb]
        for i in range(2):
            tk.op(dve, lambda e: e.memset(vaug[i][:], 1.0), writes=[vaug[i]])
        tmd, odd = d[f'tm{l}'], d[f'od{l}']
        alltm = [self.db('tm', l, gc) for gc in range(NCH)]
        tmv = tmd.rearrange("(c p) w -> p c w", p=128)
        it = 0
        nb = 0
        for g in range(3):
            Dl = DIL[g]
            nsp = S // (128 * Dl)
            tmr = tmd.rearrange("(m i r) w -> i m r w", i=128, r=Dl)
            for h in range(8):
                cq = TM_DQKV + g * 3072 + h * 128
                va, ob_ = vaug[it % 2], osb[it % 2]
                qtok, ktok, qT, kT = qtok_2[it % 2], ktok_2[it % 2], qT_2[it % 2], kT_2[it % 2]
                it += 1
                tk.dma(sp, out=qtok[:], in_=tmv[:, :, cq:cq + 128], reads=alltm, writes=[qtok])
                tk.dma(sp, out=ktok[:], in_=tmv[:, :, cq + 1024:cq + 1152], reads=alltm, writes=[ktok])
                rn = min(Dl, 8)
                for m in range(nsp):
                    for r0 in range(0, Dl, rn):
                        b0 = m * Dl + r0
                        tk.dma(sp, out=va[:, b0:b0 + rn, 0:128], in_=tmr[:, m, r0:r0 + rn, cq + 2048:cq + 2176], reads=alltm, writes=[va])
                for src, dst in ((qtok, qT), (ktok, kT)):
                    for c8 in range(NCH // 8):
                        p_ = psT[c8 % 2]
                        for i in range(8):
                            _tr(tk, p_, i, src[:, c8 * 8 + i, :], [src], self, last=(i == 7))
                        eng = act
                        if eng is act:
                            tk.op(act, lambda e: e.activation(out=dst[:, c8 * 1024:(c8 + 1) * 1024], in_=p_.t[:].bitcast(BF16), func=AF.Copy),
                                  reads=[p_], writes=[dst])
                        else:
                            tk.op(dve, lambda e: e.tensor_copy(out=dst[:, c8 * 1024:(c8 + 1) * 1024], in_=p_.t[:].bitcast(BF16)), reads=[p_], writes=[dst])
                yield
                for m in range(nsp):
                    for r in range(Dl):
                        bi = m * Dl + r
                        cur = slice(m * 128 * Dl + r, m * 128 * Dl + r + 127 * Dl + 1, Dl)
                        ps_, p_, po = psS[nb % 2], pT[nb % 3], psO[nb % 3]
                        nb += 1
                        lo = 0 if m > 0 else 128
                        if m > 0:
                            prv = slice((m - 1) * 128 * Dl + r, (m - 1) * 128 * Dl + r + 127 * Dl + 1, Dl)
                            tk.op(pe, lambda e: e.matmul(ps_[:, 0:128], lhsT=kT[:, prv], rhs=qT[:, cur], start=True, stop=False),
                                  reads=[kT, qT], writes=[ps_], inc=False)
                            tk.op(pe, lambda e: e.matmul(ps_[:, 0:128], lhsT=self.ident_b(), rhs=self.negprev_b(), start=False, stop=True),
                                  reads=[self.cbf], writes=[ps_], inc=False)
                        tk.op(pe, lambda e: e.matmul(ps_[:, 128:256], lhsT=kT[:, cur], rhs=qT[:, cur], start=True, stop=False),
                              reads=[kT, qT], writes=[ps_], inc=False)
                        tk.op(pe, lambda e: e.matmul(ps_[:, 128:256], lhsT=self.ident_b(), rhs=self.negcur_b(), start=False, stop=True),
                              reads=[self.cbf], writes=[ps_])
                        tk.op(act, lambda e: e.activation(out=p_[:, lo:256], in_=ps_[:, lo:256], func=AF.Exp), reads=[ps_], writes=[p_])
                        if m > 0:
                            tk.op(pe, lambda e: e.matmul(po[:, 0:129], lhsT=p_[:, 0:128], rhs=va[:, bi - Dl, 0:129], start=True, stop=False),
                                  reads=[p_, va], writes=[po], inc=False)
                        tk.op(pe, lambda e: e.matmul(po[:, 0:129], lhsT=p_[:, 128:256], rhs=va[:, bi, 0:129], start=(m == 0), stop=True),
                              reads=[p_, va], writes=[po])
                        tk.op(dve, lambda e: e.tensor_copy(out=ob_[:, bi, :], in_=po[:, 0:129]), reads=[po], writes=[ob_])
                        yield
                odr = odd[g].rearrange("(m i r) h e -> i m r h e", i=128, r=Dl)
                for m in range(nsp):
                    for r0 in range(0, Dl, rn):
                        b0 = m * Dl + r0
                        tk.dma(pool, out=odr[:, m, r0:r0 + rn, h, :], in_=ob_[:, b0:b0 + rn, :], reads=[ob_], writes=[self.db('od', l)])


def stage_B3comb(self, l):
    tk, d, S, NCH = self.tk, self.dram, self.S, self.NCH
    pe, act, dve, pool, sp = tk.pe, tk.act, tk.dve, tk.pool, tk.sp
    tmd, odd = d[f'tm{l}'], d[f'od{l}']
    if True:
        tk.barrier()
        with ExitStack() as st2:
            o3 = [self.sb(st2, f'o3{i}', [128, 3, 8 * 129], F32) for i in range(2)]
            dgs = [self.sb(st2, f'dgs{i}', [128, 1024], BF16) for i in range(2)]
            rden = self.sb(st2, 'rden', [128, 8], F32)
            sm = self.sb(st2, 'sm3', [128, 8 * 129], F32)
            yn = self.sb(st2, 'yn3', [128, 8, 128], F32)
            yc = [self.sb(st2, f'yc{i}', [128, 1024], BF16) for i in range(2)]
            yd = d[f'y{l}']
            for gc in range(NCH):
                rows = slice(gc * 128, (gc + 1) * 128)
                o_, g_, y_ = o3[gc % 2], dgs[gc % 2], yc[gc % 2]
                tk.dma(sp, out=o_[:], in_=odd[:, rows, :, :].rearrange("g p h e -> p g (h e)"), reads=[self.db('od', l)], writes=[o_])
                tk.dma(sp, out=g_[:], in_=tmd[rows, TM_DG:TM_DG + 1024], reads=[self.db('tm', l, gc)], writes=[g_])
                tk.op(dve, lambda e: e.tensor_tensor(out=sm[:], in0=o_[:, 0, :], in1=o_[:, 1, :], op=ALU.add), reads=[o_], writes=[sm])
                tk.op(dve, lambda e: e.tensor_tensor(out=sm[:], in0=sm[:], in1=o_[:, 2, :], op=ALU.add), reads=[o_, sm], writes=[sm])
                sv = sm[:].rearrange("p (h e) -> p h e", h=8)
                tk.op(dve, lambda e: e.tensor_copy(out=rden[:].unsqueeze(2), in_=sv[:, :, 128:129]), reads=[sm], writes=[rden])
                tk.op(dve, lambda e: e.reciprocal(out=rden[:], in_=rden[:]), reads=[rden], writes=[rden])
                tk.op(dve, lambda e: e.tensor_tensor(out=yn[:], in0=sv[:, :, 0:128], in1=rden[:].unsqueeze(2).to_broadcast([128, 8, 128]), op=ALU.mult),
                      reads=[sm, rden], writes=[yn])
                if self.debug:
                    tk.dma(pool, out=d['dbgA'][rows, :], in_=sm[:], reads=[sm], writes=[self.db('dbg')])
                    tk.dma(pool, out=d['dbgB'][rows, :], in_=yn[:].rearrange("p h e -> p (h e)"), reads=[yn], writes=[self.db('dbg')])
                tk.op(pool, lambda e: e.tensor_tensor(out=y_[:].rearrange("p (h e) -> p h e", h=8), in0=yn[:],
                                                      in1=g_[:].rearrange("p (h e) -> p h e", h=8), op=ALU.mult), reads=[yn, g_], writes=[y_])
                tk.dma(pool, out=yd[rows, 4096:5120], in_=y_[:], reads=[y_], writes=[self.db('y', l, gc)])
            tk.barrier()


def stage_B23(self, l):
    with ExitStack() as st:
        gens = [self.gen_B2(l, st), self.gen_B3(l, st), None]
        gens[2] = gens[1]
        alive = list(gens)
        while alive:
            for g in list(alive):
                try:
                    next(g)
                except StopIteration:
                    alive = [x for x in alive if x is not g]
    self.stage_B3comb(l)


def _noop(self, l):
    pass

Prog.gen_B2 = gen_B2
Prog.gen_B3 = gen_B3
Prog.stage_B3comb = stage_B3comb
Prog.stage_B2 = stage_B23
Prog.stage_B3 = _noop


def stage_C(self, l, xin, xkey):
    tk, d, S, NCH = self.tk, self.dram, self.S, self.NCH
    pe, act, dve, pool, sp = tk.pe, tk.act, tk.dve, tk.pool, tk.sp
    NT = S // 512
    with ExitStack() as st:
        ytok = [self.sb(st, f'ytok{i}', [128, 5120], BF16) for i in range(2)]
        yT = self.sb(st, 'yT', [128, 40, 512], BF16)
        wa = [self.sb(st, f'wa{i}', [128, 40, 128], BF16) for i in range(2)]
        wo = [self.sb(st, f'wo{i}', [128, 16, 128], BF16) for i in range(2)]
        gt = [self.sb(st, f'gt{i}', [128, 3, 512], BF16) for i in range(2)]
        mT = self.sb(st, 'mT', [128, 16, 512], BF16)
        mf = [self.sb(st, f'mf{i}', [128, 512], F32) for i in range(2)]
        mg = [self.sb(st, f'mg{i}', [128, 512], F32) for i in range(2)]
        xt = [self.sb(st, f'xt{i}', [128, 512], F32) for i in range(2)]
        psT = [self.ps(st, f'psTc{i}', [128, 512], F32) for i in range(2)]
        psP = [self.ps(st, f'psP{i}', [128, 512], F32) for i in range(3)]
        psX = [self.ps(st, f'psX{i}', [128, 512], F32) for i in range(2)]
        yd, gtd, xnd = d[f'y{l}'], d[f'gT{l}'], d[f'xn{l}']
        wob = d[f'wob{l}'].rearrange("k p (j c) -> j p k c", c=128)
        gview = gtd.rearrange("(b j) p s -> j p b s", b=3)
        allwob = [self.db('wob', l, b) for b in range(56)]
        nt = 0
        for tt in range(NT):
            tok0 = tt * 512
            for c4 in range(4):
                gc = tt * 4 + c4
                y_ = ytok[gc % 2]
                tk.dma(sp, out=y_[:], in_=yd[gc * 128:(gc + 1) * 128, :], reads=[self.db('y', l, gc)], writes=[y_])
                for t8 in range(5):
                    p_ = psT[nt % 2]
                    nt += 1
                    for i in range(8):
                        _tr(tk, p_, i, y_[:, (t8 * 8 + i) * 128:(t8 * 8 + i + 1) * 128], [y_], self, last=(i == 7))
                    tk.op(act, lambda e: e.activation(out=yT[:, t8 * 8:(t8 + 1) * 8, c4 * 128:(c4 + 1) * 128],
                                                      in_=p_.t[:].bitcast(BF16).rearrange("p (t e) -> p t e", t=8), func=AF.Copy), reads=[p_], writes=[yT])
            for jj in range(16):
                w_, g_ = wa[jj % 2], gt[jj % 2]
                tk.dma(sp, out=w_[:], in_=wob[jj, :, 0:40, :], reads=allwob, writes=[w_])
                tk.dma(sp, out=g_[:], in_=gview[jj, :, :, tok0:tok0 + 512], reads=[self.db('gT', l, tt)], writes=[g_])
                for br, (k0, nk) in enumerate(((0, 16), (16, 16), (32, 8))):
                    for k in range(nk):
                        tk.op(pe, lambda e: e.matmul(psP[br][:], lhsT=w_[:, k0 + k, :], rhs=yT[:, k0 + k, :], start=(k == 0), stop=(k == nk - 1)),
                              reads=[w_, yT], writes=[psP[br]], inc=(k == nk - 1))
                f_, h_ = mf[jj % 2], mg[jj % 2]
                tk.op(dve, lambda e: e.tensor_tensor(out=f_[:], in0=psP[0][:], in1=g_[:, 0, :], op=ALU.mult), reads=[psP[0], g_], writes=[f_])
                tk.op(dve, lambda e: e.tensor_tensor(out=h_[:], in0=psP[1][:], in1=g_[:, 1, :], op=ALU.mult), reads=[psP[1], g_], writes=[h_])
                tk.op(pool, lambda e: e.tensor_tensor(out=f_[:], in0=f_[:], in1=h_[:], op=ALU.add), reads=[f_, h_], writes=[f_])
                tk.op(dve, lambda e: e.tensor_tensor(out=h_[:], in0=psP[2][:], in1=g_[:, 2, :], op=ALU.mult), reads=[psP[2], g_], writes=[h_])
                tk.op(pool, lambda e: e.tensor_tensor(out=mT[:, jj, :], in0=f_[:], in1=h_[:], op=ALU.add), reads=[f_, h_], writes=[mT])
            for jj in range(16):
                w_, x_, px = wo[jj % 2], xt[jj % 2], psX[jj % 2]
                tk.dma(sp, out=w_[:], in_=wob[jj, :, 40:56, :], reads=allwob, writes=[w_])
                tk.dma(sp, out=x_[:], in_=xin[jj * 128:(jj + 1) * 128, tok0:tok0 + 512], reads=[self.db(*xkey, tt)], writes=[x_])
                for k in range(16):
                    tk.op(pe, lambda e: e.matmul(px[:], lhsT=w_[:, k, :], rhs=mT[:, k, :], start=(k == 0), stop=(k == 15)),
                          reads=[w_, mT], writes=[px], inc=(k == 15))
                tk.op(dve, lambda e: e.tensor_tensor(out=x_[:], in0=px[:], in1=x_[:], op=ALU.add), reads=[px, x_], writes=[x_])
                tk.dma(pool, out=xnd[jj * 128:(jj + 1) * 128, tok0:tok0 + 512], in_=x_[:], reads=[x_], writes=[self.db('xn', l, tt)])
        tk.barrier()


def stage_final(self):
    tk, d, S, NCH = self.tk, self.dram, self.S, self.NCH
    pe, act, dve, pool, sp = tk.pe, tk.act, tk.dve, tk.pool, tk.sp
    l = self.depth - 1
    with ExitStack() as st:
        xs = [self.sb(st, f'fx{i}', [128, 16, 128], F32) for i in range(2)]
        sq = self.sb(st, 'fsq', [128, 16, 128], F32)
        rstd = self.sb(st, 'frstd', [128, 128], F32)
        gc_ = self.sb(st, 'fg', [128, 16], F32)
        pss = self.ps(st, 'fpss', [128, 128], F32)
        tk.dma(sp, out=gc_[:], in_=d['fgc'], writes=[gc_])
        xview = d[f'xn{l}'].rearrange("(k p) s -> p k s", p=128)
        oview = d['outT'].rearrange("(k p) s -> p k s", p=128)
        for gc in range(NCH):
            x_ = xs[gc % 2]
            cs = slice(gc * 128, (gc + 1) * 128)
            tk.dma(sp, out=x_[:], in_=xview[:, :, cs], reads=[self.db('xn', l, gc // 4)], writes=[x_])
            tk.op(act, lambda e: e.activation(out=sq[:], in_=x_[:], func=AF.Square), reads=[x_], writes=[sq])
            for k in range(16):
                tk.op(pe, lambda e: e.matmul(pss[:], lhsT=self.ones_f(), rhs=sq[:, k, :], start=(k == 0), stop=(k == 15)),
                      reads=[sq, self.c32], writes=[pss], inc=(k == 15))
            tk.op(act, lambda e: e.activation(out=rstd[:], in_=pss[:], func=AF.Sqrt, bias=EPS, scale=1.0 / D), reads=[pss], writes=[rstd])
            tk.op(dve, lambda e: e.reciprocal(out=rstd[:], in_=rstd[:]), reads=[rstd], writes=[rstd])
            for k in range(16):
                tk.op(dve, lambda e: e.scalar_tensor_tensor(out=x_[:, k, :], in0=x_[:, k, :], scalar=gc_[:, k:k + 1], in1=rstd[:],
                                                            op0=ALU.mult, op1=ALU.mult), reads=[x_, gc_, rstd], writes=[x_])
            tk.dma(pool, out=oview[:, :, cs], in_=x_[:], reads=[x_], writes=[self.db('out')])
        tk.barrier()


Prog.stage_C = stage_C
Prog.stage_final = stage_final
```

```python
import math
from contextlib import ExitStack

import numpy as np
import concourse.bass as bass
import concourse.mybir as mybir
from concourse.bass_utils import run_bass_kernel_spmd

F32 = mybir.dt.float32
BF16 = mybir.dt.bfloat16
AF = mybir.ActivationFunctionType
ALU = mybir.AluOpType

D = 2048
DEPTH = 2
EPS = 1e-6
NRING = 12
SAME_ENG_SYNC = False

SPL = dict(z=(0, 2048), xbc=(2048, 5120), dt=(5120, 5152), rq=(5152, 6176), rk=(6176, 7200),
           rv=(7200, 9248), rg=(9248, 11296), dqkv=(11296, 20512), dg=(20512, 21536), mg=(21536, 27680))
TM_Z, TM_RQ, TM_RK, TM_RV, TM_RG, TM_DQKV, TM_DG = 0, 2048, 3072, 4096, 6144, 8192, 17408
TM_W = 18432
N_TMBLK = 36
N_FMBLK = 18
N_BLK = N_TMBLK + 1 + N_FMBLK


def tm_block_kind(b):
    c = b * 512
    if c < TM_RQ:
        return 'silu'
    if c < TM_RK:
        return 'rope_q'
    if c < TM_RV:
        return 'rope_k'
    if c < TM_RG:
        return 'copy'
    if c < TM_DQKV:
        return 'silu'
    if c < TM_DG:
        j = ((c - TM_DQKV) // 1024) % 3
        return ('rope_q', 'rope_k', 'copy')[j]
    return 'silu'


class Buf:
    __slots__ = ('t', 'w', 'r', 'multi', 'small')

    def __init__(self, t=None, multi=False, small=False):
        self.t = t
        self.w = {}
        self.r = {}
        self.multi = multi
        self.small = small

    def __getitem__(self, idx):
        return self.t[idx]


class Eng:
    def __init__(self, name, e, sem):
        self.name, self.e, self.sem = name, e, sem
        self.cnt = 0
        self.seen = {}


class TK:
    def __init__(self, nc, es):
        self.nc = nc
        mk = lambda n: es.enter_context(nc.semaphore(n))
        self.pe = Eng('pe', nc.tensor, mk('s_pe'))
        self.act = Eng('act', nc.scalar, mk('s_act'))
        self.dve = Eng('dve', nc.vector, mk('s_dve'))
        self.pool = Eng('pool', nc.gpsimd, mk('s_pool'))
        self.sp = Eng('sp', nc.sync, mk('s_sp'))
        self.engs = [self.pe, self.act, self.dve, self.pool, self.sp]
        self.dq = {}
        for E in (self.sp, self.pool, self.act):
            self.dq[E.name] = dict(sems=[mk(f'd_{E.name}{i}') for i in range(NRING)], vals=[0] * NRING, i=0)

    def _wait(self, E, toks):
        for tk_ in toks:
            sem, val, owner = tk_[0], tk_[1], tk_[2]
            small = len(tk_) > 3 and tk_[3]
            if val <= 0:
                continue
            if owner is E and (E is self.pe or not (SAME_ENG_SYNC or small)):
                continue
            k = id(sem)
            if E.seen.get(k, 0) >= val:
                continue
            E.e.wait_ge(sem, val)
            E.seen[k] = val

    @staticmethod
    def _deps(reads, writes):
        toks = []
        for b in reads:
            toks += [t + (True,) for t in b.w.values()]
        for b in writes:
            toks += [t + (True,) for t in b.w.values()]
            toks += list(b.r.values())
        return toks

    @staticmethod
    def _record(tok, reads, writes):
        k = id(tok[0])
        for b in reads:
            o = b.r.get(k)
            if o is None or o[1] < tok[1]:
                b.r[k] = tok
        for b in writes:
            if b.multi:
                o = b.w.get(k)
                if o is None or o[1] < tok[1]:
                    b.w[k] = tok
            else:
                b.w = {k: tok}
                b.r = {}

    def op(self, E, fn, reads=(), writes=(), inc=True):
        self._wait(E, self._deps(reads, writes))
        ins = fn(E.e)
        if inc:
            E.cnt += 1
            ins.then_inc(E.sem, 1)
            tok = (E.sem, E.cnt, E)
        else:
            tok = (E.sem, E.cnt + 1, E)
        self._record(tok, reads, writes)
        return tok

    def dma(self, E, out, in_, reads=(), writes=()):
        q = self.dq[E.name]
        k = q['i'] % NRING
        q['i'] += 1
        sem = q['sems'][k]
        prev = q['vals'][k]
        toks = self._deps(reads, writes)
        toks.append((sem, prev, None))
        self._wait(E, toks)
        E.e.dma_start(out=out, in_=in_).then_inc(sem, 16)
        q['vals'][k] = prev + 16
        tok = (sem, prev + 16, None)
        self._record(tok, reads, writes)
        return tok

    def barrier(self):
        toks = [(E.sem, E.cnt, E) for E in self.engs if E.cnt]
        for q in self.dq.values():
            for sem, val in zip(q['sems'], q['vals']):
                toks.append((sem, val, None))
        for E in self.engs:
            self._wait(E, toks)


class Prog:
    def __init__(self, S, depth=DEPTH, debug=False, stages='ABC'):
        self.S, self.depth, self.debug, self.stages = S, depth, debug, stages
        self.NCH = S // 128
        nc = self.nc = bass.Bass("TRN2", target_bir_lowering=False)
        self.es = ExitStack()
        self.tk = TK(nc, self.es)
        self.dram = {}
        self.dbuf = {}

    def din(self, name, shape, dt=F32):
        self.dram[name] = self.nc.dram_tensor(name, list(shape), dt, kind="ExternalInput").ap()
        return self.dram[name]

    def dscr(self, name, shape, dt, out=False):
        kind = "ExternalOutput" if (out or self.debug) else "Internal"
        self.dram[name] = self.nc.dram_tensor(name, list(shape), dt, kind=kind).ap()
        return self.dram[name]

    def db(self, *key):
        b = self.dbuf.get(key)
        if b is None:
            b = self.dbuf[key] = Buf(None, multi=True)
        return b

    def _uid(self, name):
        self._n = getattr(self, '_n', 0) + 1
        return f'{name}_u{self._n}'

    def sb(self, st, name, shape, dt=F32):
        small = int(np.prod(shape[1:])) <= 512
        return Buf(st.enter_context(self.nc.sbuf_tensor(self._uid('s_' + name), list(shape), dt)), small=small)

    def ps(self, st, name, shape, dt=F32):
        return Buf(st.enter_context(self.nc.psum_tensor(self._uid('p_' + name), list(shape), dt)))

    def build(self):
        S, NCH, tk = self.S, self.NCH, self.tk
        nc = self.nc
        self.din('xT', [D, S])
        self.din('w_in', [self.depth, N_BLK, 128, 16, 512])
        self.din('w_o', [self.depth, 56, 128, 2048])
        self.din('cvec', [self.depth, 128, 16 + 24 * 5 + 32 * 3])
        self.din('grow', [self.depth, 128, 2048])
        self.din('fgc', [128, 16])
        self.din('rope', [128, NCH, 128])
        self.din('ctab', [128, 128 * 6 + 8 * 128 * 2 + 8])
        for l in range(self.depth):
            self.dscr(f'wb{l}', [N_BLK, 128, 16, 512], BF16)
            self.dscr(f'wob{l}', [56, 128, 2048], BF16)
            self.dscr(f'tm{l}', [S, TM_W], BF16)
            self.dscr(f'dt{l}', [S, 32], F32)
            self.dscr(f'xbcT{l}', [24, 128, S], BF16)
            self.dscr(f'gT{l}', [48, 128, S], BF16)
            self.dscr(f'y{l}', [S, 5120], BF16)
            self.dscr(f'od{l}', [3, S, 8, 129], F32)
            self.dscr(f'xn{l}', [D, S], F32)
        self.dscr('outT', [D, S], F32, out=True)
        if self.debug:
            self.dscr('dbgA', [S, 1032], F32)
            self.dscr('dbgB', [S, 1024], F32)
            self.dscr('dbgC', [S, 8], F32)

        with ExitStack() as gs:
            self.gs = gs
            self.consts(gs)
            self.cast_weights(0)
            for l in range(self.depth):
                xin = self.dram['xT'] if l == 0 else self.dram[f'xn{l - 1}']
                xkey = ('xT',) if l == 0 else ('xn', l - 1)
                if 'A' in self.stages:
                    self.stage_A(l, xin, xkey)
                if l + 1 < self.depth:
                    self.cast_weights(l + 1)
                if 'B' in self.stages or '1' in self.stages:
                    self.stage_B1(l)
                if 'B' in self.stages or '2' in self.stages:
                    self.stage_B2(l)
                if 'B' in self.stages or '3' in self.stages:
                    self.stage_B3(l)
                if 'C' in self.stages:
                    self.stage_C(l, xin, xkey)
            if 'C' in self.stages:
                self.stage_final()
            tk.barrier()
        self.es.close()
        return nc

    def consts(self, gs):
        tk, d = self.tk, self.dram
        self.c32 = self.sb(gs, 'c32', [128, 768], F32)
        self.cbf = self.sb(gs, 'cbf', [128, 768], BF16)
        tk.dma(tk.sp, out=self.c32[:], in_=d['ctab'][:, 0:768], writes=[self.c32])
        tk.op(tk.dve, lambda e: e.tensor_copy(out=self.cbf[:], in_=self.c32[:]), reads=[self.c32], writes=[self.cbf])
        self.ident_f = lambda: self.c32[:, 0:128]
        self.ones_f = lambda: self.c32[:, 128:256]
        self.U_f = lambda: self.c32[:, 256:384]
        self.Us_f = lambda: self.c32[:, 384:512]
        self.ident_b = lambda: self.cbf[:, 0:128]
        self.negcur_b = lambda: self.cbf[:, 512:640]
        self.negprev_b = lambda: self.cbf[:, 640:768]

    def cast_weights(self, l):
        tk, d = self.tk, self.dram
        if True:
            src = d['w_in'][l].rearrange("b p (q k) c -> b (p q) (k c)", q=4)
            dst = d[f'wb{l}'].rearrange("b p (q k) c -> b (p q) (k c)", q=4)
            for b in range(N_BLK):
                tk.dma(tk.pool, out=dst[b], in_=src[b], writes=[self.db('wb', l, b)])
            src = d['w_o'][l]
            dst = d[f'wob{l}']
            for b in range(56):
                tk.dma(tk.pool, out=dst[b], in_=src[b], writes=[self.db('wob', l, b)])

    def stage_A(self, l, xin, xkey):
        tk, d, S = self.tk, self.dram, self.S
        pe, act, dve, pool, sp = tk.pe, tk.act, tk.dve, tk.pool, tk.sp
        TS = 1024
        NSUP = S // TS
        with ExitStack() as st:
            hT = self.sb(st, 'hT', [128, 16, TS], BF16)
            xs = [self.sb(st, f'xs{i}', [128, 16, 128], F32) for i in range(2)]
            sq = self.sb(st, 'sq', [128, 16, 128], F32)
            rstd = self.sb(st, 'rstd', [128, 128], F32)
            wb = [self.sb(st, f'wb{i}', [128, 16, 512], BF16) for i in range(2)]
            ropet = self.sb(st, 'ropet', [128, 8, 128], F32)
            cv = self.sb(st, 'cvA', [128, 232], F32)
            halo = self.sb(st, 'halo', [128, 24, 3], F32)
            u = [self.sb(st, f'u{i}', [128, 515], F32) for i in range(2)]
            acc = [self.sb(st, f'acc{i}', [128, 512], F32) for i in range(2)]
            tsb = [self.sb(st, f'tsb{i}', [128, 512], F32) for i in range(2)]
            ra = [self.sb(st, f'ra{i}', [128, 512], F32) for i in range(2)]
            rb = [self.sb(st, f'rb{i}', [128, 512], F32) for i in range(2)]
            ob = [self.sb(st, f'ob{i}', [128, 512], BF16) for i in range(4)]
            dts = [self.sb(st, f'dts{i}', [128, 32], F32) for i in range(2)]
            psA = [self.ps(st, f'psA{i}', [128, 512], F32) for i in range(4)]
            pss = self.ps(st, 'pss', [128, 128], F32)

            tk.dma(sp, out=cv[:], in_=d['cvec'][l], writes=[cv])
            tk.op(dve, lambda e: e.memset(halo[:], 0.0), writes=[halo])
            xview = xin.rearrange("(k p) s -> p k s", p=128)
            tmd, dtd, xbd, gtd = d[f'tm{l}'], d[f'dt{l}'], d[f'xbcT{l}'], d[f'gT{l}']
            cnt = dict(ps=0, ob=0, ts=0, u=0, dt=0)

            for su in range(NSUP):
                for c8 in range(8):
                    gc = su * 8 + c8
                    x_ = xs[gc % 2]
                    tk.dma(sp, out=x_[:], in_=xview[:, :, gc * 128:(gc + 1) * 128],
                           reads=[self.db(*xkey, gc // 4)], writes=[x_])
                    tk.op(act, lambda e: e.activation(out=sq[:], in_=x_[:], func=AF.Square), reads=[x_], writes=[sq])
                    for k in range(16):
                        tk.op(pe, lambda e: e.matmul(pss[:], lhsT=self.ones_f(), rhs=sq[:, k, :], start=(k == 0), stop=(k == 15)),
                              reads=[sq, self.c32], writes=[pss], inc=(k == 15))
                    tk.op(act, lambda e: e.activation(out=rstd[:], in_=pss[:], func=AF.Sqrt, bias=EPS, scale=1.0 / D),
                          reads=[pss], writes=[rstd])
                    tk.op(dve, lambda e: e.reciprocal(out=rstd[:], in_=rstd[:]), reads=[rstd], writes=[rstd])
                    for k in range(16):
                        tk.op(dve, lambda e: e.scalar_tensor_tensor(out=hT[:, k, c8 * 128:(c8 + 1) * 128], in0=x_[:, k, :],
                                                                    scalar=cv[:, k:k + 1], in1=rstd[:], op0=ALU.mult, op1=ALU.mult),
                              reads=[x_, cv, rstd], writes=[hT])
                tk.dma(sp, out=ropet[:], in_=d['rope'][:, su * 8:(su + 1) * 8, :], writes=[ropet])

                for blk in range(N_BLK):
                    w_ = wb[blk % 2]
                    tk.dma(sp, out=w_[:], in_=d[f'wb{l}'][blk], reads=[self.db('wb', l, blk)], writes=[w_])
                    if blk <= N_TMBLK:
                        kind = 'dt' if blk == N_TMBLK else tm_block_kind(blk)
                        ncol = 32 if kind == 'dt' else 512
                        for c8 in range(8):
                            gc = su * 8 + c8
                            ps_ = psA[cnt['ps'] % 4]
                            cnt['ps'] += 1
                            for k in range(16):
                                tk.op(pe, lambda e: e.matmul(ps_[:, 0:ncol], lhsT=hT[:, k, c8 * 128:(c8 + 1) * 128], rhs=w_[:, k, 0:ncol],
                                                             start=(k == 0), stop=(k == 15)),
                                      reads=[hT, w_], writes=[ps_], inc=(k == 15))
                            rows = slice(gc * 128, (gc + 1) * 128)
                            if kind == 'dt':
                                t_ = dts[cnt['dt'] % 2]
                                cnt['dt'] += 1
                                tk.op(dve, lambda e: e.tensor_tensor(out=t_[:], in0=ps_[:, 0:32], in1=cv[:, 136:168], op=ALU.add),
                                      reads=[ps_, cv], writes=[t_])
                                tk.op(act, lambda e: e.activation(out=t_[:], in_=t_[:], func=AF.Exp), reads=[t_], writes=[t_])
                                tk.op(act, lambda e: e.activation(out=t_[:], in_=t_[:], func=AF.Ln, bias=1.0), reads=[t_], writes=[t_])
                                tk.dma(pool, out=dtd[rows, :], in_=t_[:], reads=[t_], writes=[self.db('dt', l, gc)])
                                continue
                            o_ = ob[cnt['ob'] % 4]
                            cnt['ob'] += 1
                            if kind == 'silu':
                                tk.op(act, lambda e: e.activation(out=o_[:], in_=ps_[:], func=AF.Silu), reads=[ps_], writes=[o_])
                            elif kind == 'copy':
                                tk.op(dve, lambda e: e.tensor_copy(out=o_[:], in_=ps_[:]), reads=[ps_], writes=[o_])
                            else:
                                j = cnt['ts'] % 2
                                cnt['ts'] += 1
                                t_, a_, b_ = tsb[j], ra[j], rb[j]
                                sc = 1.0 if kind == 'rope_q' else 128.0 ** -0.5
                                tk.op(act, lambda e: e.activation(out=t_[:], in_=ps_[:], func=AF.Copy, scale=sc), reads=[ps_], writes=[t_])
                                v4 = lambda b: b[:].rearrange("p (h two e) -> p h two e", h=4, two=2)
                                cosb = ropet[:, c8, 0:64].unsqueeze(1).unsqueeze(1).to_broadcast([128, 4, 2, 64])
                                sinb = ropet[:, c8, 64:128].unsqueeze(1).unsqueeze(1).to_broadcast([128, 4, 2, 64])
                                tk.op(dve, lambda e: e.tensor_tensor(out=v4(a_), in0=v4(t_), in1=cosb, op=ALU.mult), reads=[t_, ropet], writes=[a_])
                                tk.op(pool, lambda e: e.tensor_tensor(out=v4(b_), in0=v4(t_), in1=sinb, op=ALU.mult), reads=[t_, ropet], writes=[b_])
                                tk.op(dve, lambda e: e.tensor_tensor(out=v4(o_)[:, :, 0, :], in0=v4(a_)[:, :, 0, :], in1=v4(b_)[:, :, 1, :], op=ALU.subtract),
                                      reads=[a_, b_], writes=[o_])
                                tk.op(pool, lambda e: e.tensor_tensor(out=v4(o_)[:, :, 1, :], in0=v4(a_)[:, :, 1, :], in1=v4(b_)[:, :, 0, :], op=ALU.add),
                                      reads=[a_, b_], writes=[o_])
                            tk.dma(pool, out=tmd[rows, blk * 512:(blk + 1) * 512], in_=o_[:], reads=[o_], writes=[self.db('tm', l, gc)])
                    else:
                        fb = blk - N_TMBLK - 1
                        for ct4 in range(4):
                            ct = fb * 4 + ct4
                            for tt in range(2):
                                ps_ = psA[cnt['ps'] % 4]
                                cnt['ps'] += 1
                                for k in range(16):
                                    tk.op(pe, lambda e: e.matmul(ps_[:], lhsT=w_[:, k, ct4 * 128:(ct4 + 1) * 128], rhs=hT[:, k, tt * 512:(tt + 1) * 512],
                                                                 start=(k == 0), stop=(k == 15)),
                                          reads=[hT, w_], writes=[ps_], inc=(k == 15))
                                tok0 = su * TS + tt * 512
                                o_ = ob[cnt['ob'] % 4]
                                cnt['ob'] += 1
                                if ct >= 24:
                                    tk.op(act, lambda e: e.activation(out=o_[:], in_=ps_[:], func=AF.Sigmoid), reads=[ps_], writes=[o_])
                                    tk.dma(pool, out=gtd[ct - 24, :, tok0:tok0 + 512], in_=o_[:], reads=[o_], writes=[self.db('gT', l, tok0 // 512)])
                                    continue
                                u_ = u[cnt['u'] % 2]
                                a_ = acc[cnt['u'] % 2]
                                cnt['u'] += 1
                                tk.op(dve, lambda e: e.tensor_copy(out=u_[:, 0:3], in_=halo[:, ct, :]), reads=[halo], writes=[u_])
                                tk.op(act, lambda e: e.activation(out=u_[:, 3:515], in_=ps_[:], func=AF.Copy), reads=[ps_], writes=[u_])
                                tk.op(dve, lambda e: e.tensor_copy(out=halo[:, ct, :], in_=u_[:, 512:515]), reads=[u_], writes=[halo])
                                cw = lambda kk: cv[:, 16 + ct * 5 + kk:16 + ct * 5 + kk + 1]
                                tk.op(dve, lambda e: e.tensor_scalar(out=a_[:], in0=u_[:, 3:515], scalar1=cw(3), scalar2=cw(4), op0=ALU.mult, op1=ALU.add),
                                      reads=[u_, cv], writes=[a_])
                                for kk in range(3):
                                    tk.op(dve, lambda e: e.scalar_tensor_tensor(out=a_[:], in0=u_[:, kk:kk + 512], scalar=cw(kk), in1=a_[:],
                                                                                op0=ALU.mult, op1=ALU.add), reads=[u_, cv, a_], writes=[a_])
                                tk.op(act, lambda e: e.activation(out=o_[:], in_=a_[:], func=AF.Silu), reads=[a_], writes=[o_])
                                tk.dma(pool, out=xbd[ct, :, tok0:tok0 + 512], in_=o_[:], reads=[o_], writes=[self.db('xbcT', l, tok0 // 512)])
            tk.barrier()


def _blocks(w, nblk):
    return np.ascontiguousarray(w.reshape(16, 128, nblk, 512).transpose(2, 1, 0, 3))


def prep_weights(inp, depth=DEPTH):
    w_in_l, w_o_l, cvec_l, grow_l = [], [], [], []
    for l in range(depth):
        w = inp['w_in'][l]
        sl = lambda n: w[:, SPL[n][0]:SPL[n][1]]
        tm = np.concatenate([sl('z'), sl('rq'), sl('rk'), sl('rv'), sl('rg'), sl('dqkv'), sl('dg')], axis=1)
        dtp = np.zeros((D, 512), np.float32)
        dtp[:, 0:32] = sl('dt')
        fm = np.concatenate([sl('xbc'), sl('mg')], axis=1)
        w_in_l.append(np.concatenate([_blocks(tm, N_TMBLK), _blocks(dtp, 1), _blocks(fm, N_FMBLK)], axis=0))
        wo = np.concatenate([inp['w_o_ssd'][l], inp['w_o_ret'][l], inp['w_o_dil'][l], inp['w_out'][l]], axis=0)
        w_o_l.append(np.ascontiguousarray(wo.reshape(56, 128, 2048)))
        cv = np.zeros((128, 232), np.float32)
        cv[:, 0:16] = inp['norm_g'][l].reshape(16, 128).T
        cw = inp['conv_w'][l].reshape(4, 24, 128)
        cb = inp['conv_b'][l].reshape(24, 128)
        for ct in range(24):
            cv[:, 16 + ct * 5:16 + ct * 5 + 4] = cw[:, ct, :].T
            cv[:, 16 + ct * 5 + 4] = cb[ct]
        cv[:, 136:168] = inp['dt_bias'][l][None, :]
        cv[:, 168:200] = inp['a_log'][l][None, :]
        cv[:, 200:232] = inp['d_skip'][l][None, :]
        cvec_l.append(cv)
        grow_l.append(np.ascontiguousarray(np.broadcast_to(inp['ssd_norm_g'][l][None, :], (128, 2048))))
    fgc = np.ascontiguousarray(inp['final_norm_g'].reshape(16, 128).T)
    return dict(w_in=np.stack(w_in_l), w_o=np.stack(w_o_l), cvec=np.stack(cvec_l), grow=np.stack(grow_l), fgc=fgc)


def prep_consts(S):
    NCH = S // 128
    pos = np.arange(S, dtype=np.float32)
    inv = (10000.0 ** (-np.arange(0, 128, 2, dtype=np.float32) / 128.0)).astype(np.float32)
    ang = pos[:, None] * inv[None, :]
    rope = np.concatenate([np.cos(ang), np.sin(ang)], axis=1).astype(np.float32)
    rope = np.ascontiguousarray(rope.reshape(NCH, 128, 128).transpose(1, 0, 2))
    i = np.arange(128)
    ct = np.zeros((128, 768 + 2048 + 8), np.float32)
    ct[:, 0:128] = np.eye(128)
    ct[:, 128:256] = 1.0
    ct[:, 256:384] = (i[:, None] <= i[None, :])
    ct[:, 384:512] = (i[:, None] > i[None, :])
    ct[:, 512:640] = np.where(i[None, :] >= i[:, None], 0.0, -30000.0)
    ct[:, 640:768] = np.where(i[:, None] >= i[None, :], 0.0, -30000.0)
    lg = np.log(1.0 - np.exp2(-5.0 - np.arange(8, dtype=np.float64)))
    for h in range(8):
        rel = (i[None, :] - i[:, None]).astype(np.float64)
        ct[:, 768 + h * 128:768 + (h + 1) * 128] = np.where(rel >= 0, np.exp(np.maximum(rel, 0) * lg[h]), 0.0)
        ct[:, 1792 + h * 128:1792 + (h + 1) * 128] = np.exp((i[None, :] + 1.0) * lg[h])
        ct[:, 2816 + h] = np.exp((127.0 - i) * lg[h])
    return dict(rope=rope, ctab=ct)


_CACHE = {}


def kernel(**inp):
    x = np.asarray(inp['x'], np.float32)
    B, S, _ = x.shape
    if 'nc' not in _CACHE:
        _CACHE['nc'] = Prog(S).build()
    nc = _CACHE['nc']
    shared = dict(prep_weights(inp))
    shared.update(prep_consts(S))
    in_maps = []
    for b in range(B):
        m = dict(shared)
        m['xT'] = np.ascontiguousarray(x[b].T)
        in_maps.append(m)
    res = run_bass_kernel_spmd(nc, in_maps, core_ids=list(range(B)))
    out = np.stack([np.ascontiguousarray(r['outT'].T) for r in res.results], axis=0)
    return out.astype(np.float32)


def _tr(tk, ps_bf, slot, src_ap, src_bufs, P, last=True):
    tk.op(tk.pe, lambda e: e.transpose(out=ps_bf.t[:].bitcast(BF16)[:, slot * 128:(slot + 1) * 128], in_=src_ap, identity=P.ident_b()),
          reads=list(src_bufs) + [P.cbf], writes=[ps_bf], inc=last)


def stage_B1(self, l):
    tk, d, S, NCH = self.tk, self.dram, self.S, self.NCH
    pe, act, dve, pool, sp = tk.pe, tk.act, tk.dve, tk.pool, tk.sp
    with ExitStack() as st:
        xbl = self.sb(st, 'xbl', [128, 24, 512], BF16)
        x_tok_2 = [self.sb(st, f'x_tok{i}', [128, 2048], BF16) for i in range(2)]
        B_tok_2 = [self.sb(st, f'B_tok{i}', [128, 512], BF16) for i in range(2)]
        zs_2 = [self.sb(st, f'zs{i}', [128, 2048], BF16) for i in range(2)]
        xdt_2 = [self.sb(st, f'xdt{i}', [128, 32, 64], BF16) for i in range(2)]
        xdte_2 = [self.sb(st, f'xdte{i}', [128, 32, 64], BF16) for i in range(2)]
        lall = self.sb(st, 'lall', [128, 32, 128], F32)
        cbm_2 = [self.sb(st, f'cbm{i}', [128, 4, 128], F32) for i in range(2)]
        Lm = [self.sb(st, f'Lm{i}', [128, 4, 128], F32) for i in range(2)]
        M_2 = [self.sb(st, f'M{i}', [128, 32, 128], BF16) for i in range(2)]
        yt_2 = [self.sb(st, f'yt{i}', [128, 2048], F32) for i in range(2)]
        tmp = self.sb(st, 'tmp', [128, 2048], F32)
        Hs = self.sb(st, 'Hs', [128, 2048], F32)
        Hb = self.sb(st, 'Hb', [128, 2048], BF16)
        grow = self.sb(st, 'grow', [128, 2048], F32)
        ya_2 = [self.sb(st, f'ya{i}', [128, 2048], BF16) for i in range(2)]
        cv = self.sb(st, 'cvB', [128, 232], F32)
        arow = self.sb(st, 'arow', [128, 32], F32)
        dt_t_2 = [self.sb(st, f'dt_t{i}', [128, 32], F32) for i in range(2)]
        dta_2 = [self.sb(st, f'dta{i}', [128, 32], F32) for i in range(2)]
        sS_2 = [self.sb(st, f'sS{i}', [128, 64], F32) for i in range(2)]
        eacs_2 = [self.sb(st, f'eacs{i}', [128, 32], F32) for i in range(2)]
        tend_2 = [self.sb(st, f'tend{i}', [128, 32], F32) for i in range(2)]
        cdr_2 = [self.sb(st, f'cdr{i}', [128, 32], F32) for i in range(2)]
        ss_2 = [self.sb(st, f'ss{i}', [128, 1], F32) for i in range(2)]
        psT = self.ps(st, 'psT', [128, 512], F32)
        psS = self.ps(st, 'psS', [128, 64], F32)
        psC = self.ps(st, 'psC', [128, 512], F32)
        psG = [self.ps(st, f'psG{i}', [128, 512], F32) for i in range(2)]
        psD = self.ps(st, 'psD', [128, 512], F32)
        psO = self.ps(st, 'psO', [128, 512], F32)
        psN = self.ps(st, 'psN', [128, 512], F32)

        tk.dma(sp, out=cv[:], in_=d['cvec'][l], writes=[cv])
        tk.dma(sp, out=grow[:], in_=d['grow'][l], writes=[grow])
        tk.op(act, lambda e: e.activation(out=arow[:], in_=cv[:, 168:200], func=AF.Exp), reads=[cv], writes=[arow])
        tk.op(dve, lambda e: e.tensor_scalar(out=arow[:], in0=arow[:], scalar1=-1.0, scalar2=None, op0=ALU.mult), reads=[arow], writes=[arow])
        tk.op(dve, lambda e: e.memset(Hs[:], 0.0), writes=[Hs])
        tk.op(dve, lambda e: e.memset(Hb[:], 0.0), writes=[Hb])
        tmd, dtd, xbd, yd = d[f'tm{l}'], d[f'dt{l}'], d[f'xbcT{l}'], d[f'y{l}']
        bc3 = lambda ap, n: ap.unsqueeze(2).to_broadcast([128, ap.shape[1], n])
        for gc in range(NCH):
            rows = slice(gc * 128, (gc + 1) * 128)
            sub = gc % 4
            x_tok = x_tok_2[gc % 2]
            B_tok = B_tok_2[gc % 2]
            zs = zs_2[gc % 2]
            xdt = xdt_2[gc % 2]
            xdte = xdte_2[gc % 2]
            cbm = cbm_2[gc % 2]
            M = M_2[gc % 2]
            yt = yt_2[gc % 2]
            ya = ya_2[gc % 2]
            dt_t = dt_t_2[gc % 2]
            dta = dta_2[gc % 2]
            sS = sS_2[gc % 2]
            eacs = eacs_2[gc % 2]
            tend = tend_2[gc % 2]
            cdr = cdr_2[gc % 2]
            ss = ss_2[gc % 2]
            ts_ = slice(sub * 128, (sub + 1) * 128)
            if sub == 0:
                tk.dma(sp, out=xbl[:], in_=xbd[:, :, gc * 128:gc * 128 + 512].rearrange("t p s -> p t s"),
                       reads=[self.db('xbcT', l, gc // 4)], writes=[xbl])
            tk.dma(sp, out=dt_t[:], in_=dtd[rows, :], reads=[self.db('dt', l, gc)], writes=[dt_t])
            tk.dma(sp, out=zs[:], in_=tmd[rows, TM_Z:TM_Z + 2048], reads=[self.db('tm', l, gc)], writes=[zs])
            for half in range(2):
                for i in range(8):
                    _tr(tk, psT, i, xbl[:, half * 8 + i, ts_], [xbl], self, last=(i == 7))
                tk.op(act, lambda e: e.activation(out=x_tok[:, half * 1024:(half + 1) * 1024], in_=psT.t[:].bitcast(BF16), func=AF.Copy),
                      reads=[psT], writes=[x_tok])
            for i in range(4):
                _tr(tk, psT, i, xbl[:, 16 + i, ts_], [xbl], self, last=(i == 3))
            tk.op(act, lambda e: e.activation(out=B_tok[:], in_=psT.t[:].bitcast(BF16)[:, 0:512], func=AF.Copy), reads=[psT], writes=[B_tok])
            tk.op(dve, lambda e: e.tensor_tensor(out=dta[:], in0=dt_t[:], in1=arow[:], op=ALU.mult), reads=[dt_t, arow], writes=[dta])
            tk.op(pe, lambda e: e.matmul(psS[:, 0:32], lhsT=self.U_f(), rhs=dta[:], start=True, stop=True), reads=[dta, self.c32], writes=[psS], inc=False)
            tk.op(pe, lambda e: e.matmul(psS[:, 32:64], lhsT=self.ones_f(), rhs=dta[:], start=True, stop=True), reads=[dta, self.c32], writes=[psS])
            tk.op(act, lambda e: e.activation(out=sS[:], in_=psS[:], func=AF.Copy), reads=[psS], writes=[sS])
            tk.op(act, lambda e: e.activation(out=eacs[:], in_=sS[:, 0:32], func=AF.Exp), reads=[sS], writes=[eacs])
            tk.op(dve, lambda e: e.tensor_tensor(out=tend[:], in0=sS[:, 32:64], in1=sS[:, 0:32], op=ALU.subtract), reads=[sS], writes=[tend])
            tk.op(act, lambda e: e.activation(out=tend[:], in_=tend[:], func=AF.Exp), reads=[tend], writes=[tend])
            tk.op(act, lambda e: e.activation(out=cdr[:], in_=sS[:, 32:64], func=AF.Exp), reads=[sS], writes=[cdr])
            xv = x_tok[:].rearrange("p (h e) -> p h e", h=32)
            tk.op(dve, lambda e: e.tensor_tensor(out=xdt[:], in0=xv, in1=bc3(dt_t[:], 64), op=ALU.mult), reads=[x_tok, dt_t], writes=[xdt])
            tk.op(pool, lambda e: e.tensor_tensor(out=xdte[:], in0=xdt[:], in1=bc3(tend[:], 64), op=ALU.mult), reads=[xdt, tend], writes=[xdte])
            usb = self.Us_f().unsqueeze(1).to_broadcast([128, 16, 128])
            tk.op(dve, lambda e: e.tensor_tensor(out=lall[:, 0:16, :], in0=bc3(dta[:, 0:16], 128), in1=usb, op=ALU.mult), reads=[dta, self.c32], writes=[lall])
            tk.op(pool, lambda e: e.tensor_tensor(out=lall[:, 16:32, :], in0=bc3(dta[:, 16:32], 128), in1=usb, op=ALU.mult), reads=[dta, self.c32], writes=[lall])
            for g in range(4):
                tk.op(pe, lambda e: e.matmul(psC[:, g * 128:(g + 1) * 128], lhsT=xbl[:, 16 + g, ts_], rhs=xbl[:, 20 + g, ts_], start=True, stop=True),
                      reads=[xbl], writes=[psC], inc=(g == 3))
            ub = self.U_f().unsqueeze(1).to_broadcast([128, 4, 128])
            tk.op(dve, lambda e: e.tensor_tensor(out=cbm[:], in0=psC[:].rearrange("p (g e) -> p g e", g=4), in1=ub, op=ALU.mult), reads=[psC, self.c32], writes=[cbm])
            for hq in range(8):
                pg, lm = psG[hq % 2], Lm[hq % 2]
                for i in range(4):
                    tk.op(pe, lambda e: e.matmul(pg[:, i * 128:(i + 1) * 128], lhsT=lall[:, hq * 4 + i, :], rhs=self.U_f(), start=True, stop=True),
                          reads=[lall, self.c32], writes=[pg], inc=(i == 3))
                tk.op(act, lambda e: e.activation(out=lm[:], in_=pg[:].rearrange("p (g e) -> p g e", g=4), func=AF.Exp), reads=[pg], writes=[lm])
                cb_b = cbm[:, hq // 2, :].unsqueeze(1).to_broadcast([128, 4, 128])
                tk.op(dve, lambda e: e.tensor_tensor(out=M[:, hq * 4:(hq + 1) * 4, :], in0=lm[:], in1=cb_b, op=ALU.mult), reads=[lm, cbm], writes=[M])
            for g in range(4):
                gs_ = slice(g * 512, (g + 1) * 512)
                for i in range(8):
                    h = g * 8 + i
                    tk.op(pe, lambda e: e.matmul(psD[:, i * 64:(i + 1) * 64], lhsT=M[:, h, :], rhs=xdt[:, h, :], start=True, stop=True),
                          reads=[M, xdt], writes=[psD], inc=(i == 7))
                tk.op(pe, lambda e: e.matmul(psO[:], lhsT=xbl[:, 20 + g, ts_], rhs=Hb[:, gs_], start=True, stop=True), reads=[xbl, Hb], writes=[psO])
                tk.op(pe, lambda e: e.matmul(psN[:], lhsT=B_tok[:, g * 128:(g + 1) * 128], rhs=xdte[:, g * 8:(g + 1) * 8, :], start=True, stop=True),
                      reads=[B_tok, xdte], writes=[psN])
                v8 = lambda ap: ap.rearrange("p (h e) -> p h e", h=8)
                tk.op(dve, lambda e: e.tensor_tensor(out=v8(yt[:, gs_]), in0=v8(psO[:]), in1=bc3(eacs[:, g * 8:(g + 1) * 8], 64), op=ALU.mult),
                      reads=[psO, eacs], writes=[yt])
                tk.op(dve, lambda e: e.tensor_tensor(out=yt[:, gs_], in0=psD[:], in1=yt[:, gs_], op=ALU.add), reads=[psD, yt], writes=[yt])
                tk.op(pool, lambda e: e.tensor_tensor(out=v8(Hs[:, gs_]), in0=v8(Hs[:, gs_]), in1=bc3(cdr[:, g * 8:(g + 1) * 8], 64), op=ALU.mult),
                      reads=[Hs, cdr], writes=[Hs])
                tk.op(dve, lambda e: e.tensor_tensor(out=Hs[:, gs_], in0=psN[:], in1=Hs[:, gs_], op=ALU.add), reads=[psN, Hs], writes=[Hs])
                tk.op(act, lambda e: e.activation(out=Hb[:, gs_], in_=Hs[:, gs_], func=AF.Copy), reads=[Hs], writes=[Hb])
            tk.op(pool, lambda e: e.tensor_tensor(out=tmp[:].rearrange("p (h e) -> p h e", h=32), in0=xv, in1=bc3(cv[:, 200:232], 64), op=ALU.mult),
                  reads=[x_tok, cv], writes=[tmp])
            tk.op(pool, lambda e: e.tensor_tensor(out=yt[:], in0=yt[:], in1=tmp[:], op=ALU.add), reads=[yt, tmp], writes=[yt])
            tk.op(pool, lambda e: e.tensor_tensor(out=yt[:], in0=yt[:], in1=zs[:], op=ALU.mult), reads=[yt, zs], writes=[yt])
            tk.op(act, lambda e: e.activation(out=tmp[:], in_=yt[:], func=AF.Square, accum_out=ss[:]), reads=[yt], writes=[tmp, ss])
            tk.op(act, lambda e: e.activation(out=ss[:], in_=ss[:], func=AF.Sqrt, bias=EPS, scale=1.0 / 2048), reads=[ss], writes=[ss])
            tk.op(dve, lambda e: e.reciprocal(out=ss[:], in_=ss[:]), reads=[ss], writes=[ss])
            tk.op(dve, lambda e: e.scalar_tensor_tensor(out=ya[:], in0=yt[:], scalar=ss[:, 0:1], in1=grow[:], op0=ALU.mult, op1=ALU.mult),
                  reads=[yt, ss, grow], writes=[ya])
            tk.dma(pool, out=yd[rows, 0:2048], in_=ya[:], reads=[ya], writes=[self.db('y', l, gc)])
        tk.barrier()


Prog.stage_B1 = stage_B1


def stage_B2(self, l):
    tk, d, S, NCH = self.tk, self.dram, self.S, self.NCH
    pe, act, dve, pool, sp = tk.pe, tk.act, tk.dve, tk.pool, tk.sp
    cdh = [float(np.exp(128.0 * np.log(1.0 - 2.0 ** (-5.0 - h)))) for h in range(8)]
    with ExitStack() as st:
        rt = [self.sb(st, f'rt{i}', [128, 6144], BF16) for i in range(2)]
        tab = self.sb(st, 'rtab', [128, 2056], F32)
        qT_2 = [self.sb(st, f'qT{i}', [128, 8, 128], BF16) for i in range(2)]
        qTd_2 = [self.sb(st, f'qTd{i}', [128, 8, 128], BF16) for i in range(2)]
        kT_2 = [self.sb(st, f'kT{i}', [128, 8, 128], BF16) for i in range(2)]
        kdk_2 = [self.sb(st, f'kdk{i}', [128, 8, 128], BF16) for i in range(2)]
        ST = [self.sb(st, f'ST{i}', [128, 4, 128], BF16) for i in range(2)]
        osb_2 = [self.sb(st, f'osb{i}', [128, 2048], F32) for i in range(2)]
        junk = self.sb(st, 'junk', [128, 256], F32)
        ss8_2 = [self.sb(st, f'ss8{i}', [128, 8], F32) for i in range(2)]
        Rs = self.sb(st, 'Rs', [128, 2048], F32)
        Rb = self.sb(st, 'Rb', [128, 2048], BF16)
        yb_2 = [self.sb(st, f'yb{i}', [128, 2048], BF16) for i in range(2)]
        psT = self.ps(st, 'psT2', [128, 512], F32)
        psSc = [self.ps(st, f'psSc{i}', [128, 512], F32) for i in range(2)]
        psO = [self.ps(st, f'psO2{i}', [128, 256], F32) for i in range(2)]
        psK = [self.ps(st, f'psK{i}', [128, 256], F32) for i in range(2)]
        tk.dma(sp, out=tab[:], in_=d['ctab'][:, 768:768 + 2056], writes=[tab])
        tk.op(dve, lambda e: e.memset(Rs[:], 0.0), writes=[Rs])
        tk.op(dve, lambda e: e.memset(Rb[:], 0.0), writes=[Rb])
        tmd, yd = d[f'tm{l}'], d[f'y{l}']
        psTb = lambda: psT.t[:].bitcast(BF16)
        for gc in range(NCH):
            rows = slice(gc * 128, (gc + 1) * 128)
            r_ = rt[gc % 2]
            qT = qT_2[gc % 2]
            qTd = qTd_2[gc % 2]
            kT = kT_2[gc % 2]
            kdk = kdk_2[gc % 2]
            osb = osb_2[gc % 2]
            ss8 = ss8_2[gc % 2]
            yb = yb_2[gc % 2]
            tk.dma(sp, out=r_[:], in_=tmd[rows, TM_RQ:TM_DQKV], reads=[self.db('tm', l, gc)], writes=[r_])
            q_tok = lambda h: r_[:, h * 128:(h + 1) * 128]
            k_tok = lambda h: r_[:, 1024 + h * 128:1024 + (h + 1) * 128]
            v_tok = lambda h: r_[:, 2048 + h * 256:2048 + (h + 1) * 256]
            rgs = lambda h: r_[:, 4096 + h * 256:4096 + (h + 1) * 256]
            for h in range(8):
                _tr(tk, psT, h, q_tok(h), [r_], self, last=(h == 7))
            tk.op(act, lambda e: e.activation(out=qT[:].rearrange("p h e -> p (h e)"), in_=psTb(), func=AF.Copy), reads=[psT], writes=[qT])
            tk.op(dve, lambda e: e.tensor_tensor(out=qTd[:].rearrange("p h e -> p (h e)"), in0=qT[:].rearrange("p h e -> p (h e)"), in1=tab[:, 1024:2048], op=ALU.mult),
                  reads=[qT, tab], writes=[qTd])
            for h in range(8):
                _tr(tk, psT, h, k_tok(h), [r_], self, last=(h == 7))
            tk.op(act, lambda e: e.activation(out=kT[:].rearrange("p h e -> p (h e)"), in_=psTb(), func=AF.Copy), reads=[psT], writes=[kT])
            tk.op(dve, lambda e: e.tensor_tensor(out=kdk[:], in0=r_[:, 1024:2048].rearrange("p (h e) -> p h e", h=8),
                                                  in1=tab[:, 2048:2056].unsqueeze(2).to_broadcast([128, 8, 128]), op=ALU.mult),
                  reads=[r_, tab], writes=[kdk])
            for hq in range(2):
                pc, st_ = psSc[hq], ST[hq]
                for i in range(4):
                    h = hq * 4 + i
                    tk.op(pe, lambda e: e.matmul(pc[:, i * 128:(i + 1) * 128], lhsT=kT[:, h, :], rhs=qT[:, h, :], start=True, stop=True),
                          reads=[kT, qT], writes=[pc], inc=(i == 3))
                tk.op(dve, lambda e: e.tensor_tensor(out=st_[:].rearrange("p h e -> p (h e)"), in0=pc[:], in1=tab[:, hq * 512:(hq + 1) * 512], op=ALU.mult),
                      reads=[pc, tab], writes=[st_])
            for h in range(8):
                po, pk = psO[h % 2], psK[h % 2]
                hs = slice(h * 256, (h + 1) * 256)
                tk.op(pe, lambda e: e.matmul(po[:], lhsT=ST[h // 4][:, h % 4, :], rhs=v_tok(h), start=True, stop=False),
                      reads=[ST[h // 4], r_], writes=[po], inc=False)
                tk.op(pe, lambda e: e.matmul(po[:], lhsT=qTd[:, h, :], rhs=Rb[:, hs], start=False, stop=True), reads=[qTd, Rb], writes=[po])
                tk.op(pe, lambda e: e.matmul(pk[:], lhsT=kdk[:, h, :], rhs=v_tok(h), start=True, stop=True), reads=[kdk, r_], writes=[pk])
                tk.op(dve, lambda e: e.tensor_copy(out=osb[:, hs], in_=po[:]), reads=[po], writes=[osb])
                tk.op(act, lambda e: e.activation(out=junk[:], in_=osb[:, hs], func=AF.Square, accum_out=ss8[:, h:h + 1]), reads=[osb], writes=[junk, ss8])
                tk.op(dve, lambda e: e.tensor_scalar(out=Rs[:, hs], in0=Rs[:, hs], scalar1=cdh[h], scalar2=None, op0=ALU.mult), reads=[Rs], writes=[Rs])
                tk.op(dve, lambda e: e.tensor_tensor(out=Rs[:, hs], in0=pk[:], in1=Rs[:, hs], op=ALU.add), reads=[Rs, pk], writes=[Rs])
                tk.op(act, lambda e: e.activation(out=Rb[:, hs], in_=Rs[:, hs], func=AF.Copy), reads=[Rs], writes=[Rb])
            tk.op(act, lambda e: e.activation(out=ss8[:], in_=ss8[:], func=AF.Sqrt, bias=EPS, scale=1.0 / 256), reads=[ss8], writes=[ss8])
            tk.op(dve, lambda e: e.reciprocal(out=ss8[:], in_=ss8[:]), reads=[ss8], writes=[ss8])
            for h in range(8):
                hs = slice(h * 256, (h + 1) * 256)
                tk.op(dve, lambda e: e.scalar_tensor_tensor(out=yb[:, hs], in0=osb[:, hs], scalar=ss8[:, h:h + 1], in1=rgs(h), op0=ALU.mult, op1=ALU.mult),
                      reads=[osb, ss8, r_], writes=[yb])
            tk.dma(pool, out=yd[rows, 2048:4096], in_=yb[:], reads=[yb], writes=[self.db('y', l, gc)])
        tk.barrier()


def stage_B3(self, l):
    tk, d, S, NCH = self.tk, self.dram, self.S, self.NCH
    pe, act, dve, pool, sp = tk.pe, tk.act, tk.dve, tk.pool, tk.sp
    DIL = (1, 4, 16)
    with ExitStack() as st:
        qtok_2 = [self.sb(st, f'qtok{i}', [128, NCH, 128], BF16) for i in range(2)]
        ktok_2 = [self.sb(st, f'ktok{i}', [128, NCH, 128], BF16) for i in range(2)]
        qT_2 = [self.sb(st, f'dqT{i}', [128, S], BF16) for i in range(2)]
        kT_2 = [self.sb(st, f'dkT{i}', [128, S], BF16) for i in range(2)]
        vaug = [self.sb(st, f'vaug{i}', [128, NCH, 130], BF16) for i in range(2)]
        pT = [self.sb(st, f'pT{i}', [128, 256], BF16) for i in range(3)]
        osb = [self.sb(st, f'dosb{i}', [128, NCH, 129], F32) for i in range(2)]
        psT = [self.ps(st, f'psT3{i}', [128, 512], F32) for i in range(2)]
        psS = [self.ps(st, f'psS3{i}', [128, 512], F32) for i in range(3)]
        psO = [self.ps(st, f'psO3{i}', [128, 512], F32) for i in range(3)]
        for i in range(2):
            tk.op(dve, lambda e: e.memset(vaug[i][:], 1.0), writes=[vaug[i]])
        tmd, odd = d[f'tm{l}'], d[f'od{l}']
        alltm = [self.db('tm', l, gc) for gc in range(NCH)]
        tmv = tmd.rearrange("(c p) w -> p c w", p=128)
        it = 0
        nb = 0
        for g in range(3):
            Dl = DIL[g]
            nsp = S // (128 * Dl)
            tmr = tmd.rearrange("(m i r) w -> i m r w", i=128, r=Dl)
            for h in range(8):
                cq = TM_DQKV + g * 3072 + h * 128
                va, ob_ = vaug[it % 2], osb[it % 2]
                qtok, ktok, qT, kT = qtok_2[it % 2], ktok_2[it % 2], qT_2[it % 2], kT_2[it % 2]
                it += 1
                tk.dma(sp, out=qtok[:], in_=tmv[:, :, cq:cq + 128], reads=alltm, writes=[qtok])
                tk.dma(sp, out=ktok[:], in_=tmv[:, :, cq + 1024:cq + 1152], reads=alltm, writes=[ktok])
                rn = min(Dl, 8)
                for m in range(nsp):
                    for r0 in range(0, Dl, rn):
                        b0 = m * Dl + r0
                        tk.dma(sp, out=va[:, b0:b0 + rn, 0:128], in_=tmr[:, m, r0:r0 + rn, cq + 2048:cq + 2176], reads=alltm, writes=[va])
                for src, dst in ((qtok, qT), (ktok, kT)):
                    for c8 in range(NCH // 8):
                        p_ = psT[c8 % 2]
                        for i in range(8):
                            _tr(tk, p_, i, src[:, c8 * 8 + i, :], [src], self, last=(i == 7))
                        eng = act
                        if eng is act:
                            tk.op(act, lambda e: e.activation(out=dst[:, c8 * 1024:(c8 + 1) * 1024], in_=p_.t[:].bitcast(BF16), func=AF.Copy),
                                  reads=[p_], writes=[dst])
                        else:
                            tk.op(dve, lambda e: e.tensor_copy(out=dst[:, c8 * 1024:(c8 + 1) * 1024], in_=p_.t[:].bitcast(BF16)), reads=[p_], writes=[dst])
                for m in range(nsp):
                    for r in range(Dl):
                        bi = m * Dl + r
                        cur = slice(m * 128 * Dl + r, m * 128 * Dl + r + 127 * Dl + 1, Dl)
                        ps_, p_, po = psS[nb % 3], pT[nb % 3], psO[nb % 3]
                        nb += 1
                        lo = 0 if m > 0 else 128
                        if m > 0:
                            prv = slice((m - 1) * 128 * Dl + r, (m - 1) * 128 * Dl + r + 127 * Dl + 1, Dl)
                            tk.op(pe, lambda e: e.matmul(ps_[:, 0:128], lhsT=kT[:, prv], rhs=qT[:, cur], start=True, stop=False),
                                  reads=[kT, qT], writes=[ps_], inc=False)
                            tk.op(pe, lambda e: e.matmul(ps_[:, 0:128], lhsT=self.ident_b(), rhs=self.negprev_b(), start=False, stop=True),
                                  reads=[self.cbf], writes=[ps_], inc=False)
                        tk.op(pe, lambda e: e.matmul(ps_[:, 128:256], lhsT=kT[:, cur], rhs=qT[:, cur], start=True, stop=False),
                              reads=[kT, qT], writes=[ps_], inc=False)
                        tk.op(pe, lambda e: e.matmul(ps_[:, 128:256], lhsT=self.ident_b(), rhs=self.negcur_b(), start=False, stop=True),
                              reads=[self.cbf], writes=[ps_])
                        tk.op(act, lambda e: e.activation(out=p_[:, lo:256], in_=ps_[:, lo:256], func=AF.Exp), reads=[ps_], writes=[p_])
                        if m > 0:
                            tk.op(pe, lambda e: e.matmul(po[:, 0:129], lhsT=p_[:, 0:128], rhs=va[:, bi - Dl, 0:129], start=True, stop=False),
                                  reads=[p_, va], writes=[po], inc=False)
                        tk.op(pe, lambda e: e.matmul(po[:, 0:129], lhsT=p_[:, 128:256], rhs=va[:, bi, 0:129], start=(m == 0), stop=True),
                              reads=[p_, va], writes=[po])
                        tk.op(dve, lambda e: e.tensor_copy(out=ob_[:, bi, :], in_=po[:, 0:129]), reads=[po], writes=[ob_])
                odr = odd[g].rearrange("(m i r) h e -> i m r h e", i=128, r=Dl)
                for m in range(nsp):
                    for r0 in range(0, Dl, rn):
                        b0 = m * Dl + r0
                        tk.dma(pool, out=odr[:, m, r0:r0 + rn, h, :], in_=ob_[:, b0:b0 + rn, :], reads=[ob_], writes=[self.db('od', l)])
        tk.barrier()
        with ExitStack() as st2:
            o3 = [self.sb(st2, f'o3{i}', [128, 3, 8 * 129], F32) for i in range(2)]
            dgs = [self.sb(st2, f'dgs{i}', [128, 1024], BF16) for i in range(2)]
            rden = self.sb(st2, 'rden', [128, 8], F32)
            sm = self.sb(st2, 'sm3', [128, 8 * 129], F32)
            yn = self.sb(st2, 'yn3', [128, 8, 128], F32)
            yc = [self.sb(st2, f'yc{i}', [128, 1024], BF16) for i in range(2)]
            yd = d[f'y{l}']
            for gc in range(NCH):
                rows = slice(gc * 128, (gc + 1) * 128)
                o_, g_, y_ = o3[gc % 2], dgs[gc % 2], yc[gc % 2]
                tk.dma(sp, out=o_[:], in_=odd[:, rows, :, :].rearrange("g p h e -> p g (h e)"), reads=[self.db('od', l)], writes=[o_])
                tk.dma(sp, out=g_[:], in_=tmd[rows, TM_DG:TM_DG + 1024], reads=[self.db('tm', l, gc)], writes=[g_])
                tk.op(dve, lambda e: e.tensor_tensor(out=sm[:], in0=o_[:, 0, :], in1=o_[:, 1, :], op=ALU.add), reads=[o_], writes=[sm])
                tk.op(dve, lambda e: e.tensor_tensor(out=sm[:], in0=sm[:], in1=o_[:, 2, :], op=ALU.add), reads=[o_, sm], writes=[sm])
                sv = sm[:].rearrange("p (h e) -> p h e", h=8)
                tk.op(dve, lambda e: e.tensor_copy(out=rden[:].unsqueeze(2), in_=sv[:, :, 128:129]), reads=[sm], writes=[rden])
                tk.op(dve, lambda e: e.reciprocal(out=rden[:], in_=rden[:]), reads=[rden], writes=[rden])
                tk.op(dve, lambda e: e.tensor_tensor(out=yn[:], in0=sv[:, :, 0:128], in1=rden[:].unsqueeze(2).to_broadcast([128, 8, 128]), op=ALU.mult),
                      reads=[sm, rden], writes=[yn])
                if self.debug:
                    tk.dma(pool, out=d['dbgA'][rows, :], in_=sm[:], reads=[sm], writes=[self.db('dbg')])
                    tk.dma(pool, out=d['dbgB'][rows, :], in_=yn[:].rearrange("p h e -> p (h e)"), reads=[yn], writes=[self.db('dbg')])
                tk.op(pool, lambda e: e.tensor_tensor(out=y_[:].rearrange("p (h e) -> p h e", h=8), in0=yn[:],
                                                      in1=g_[:].rearrange("p (h e) -> p h e", h=8), op=ALU.mult), reads=[yn, g_], writes=[y_])
                tk.dma(pool, out=yd[rows, 4096:5120], in_=y_[:], reads=[y_], writes=[self.db('y', l, gc)])
            tk.barrier()


Prog.stage_B2 = stage_B2
Prog.stage_B3 = stage_B3


def stage_C(self, l, xin, xkey):
    tk, d, S, NCH = self.tk, self.dram, self.S, self.NCH
    pe, act, dve, pool, sp = tk.pe, tk.act, tk.dve, tk.pool, tk.sp
    NT = S // 512
    with ExitStack() as st:
        ytok = [self.sb(st, f'ytok{i}', [128, 5120], BF16) for i in range(2)]
        yT = self.sb(st, 'yT', [128, 40, 512], BF16)
        wa = [self.sb(st, f'wa{i}', [128, 40, 128], BF16) for i in range(2)]
        wo = [self.sb(st, f'wo{i}', [128, 16, 128], BF16) for i in range(2)]
        gt = [self.sb(st, f'gt{i}', [128, 3, 512], BF16) for i in range(2)]
        mT = self.sb(st, 'mT', [128, 16, 512], BF16)
        mf = [self.sb(st, f'mf{i}', [128, 512], F32) for i in range(2)]
        mg = [self.sb(st, f'mg{i}', [128, 512], F32) for i in range(2)]
        xt = [self.sb(st, f'xt{i}', [128, 512], F32) for i in range(2)]
        psT = [self.ps(st, f'psTc{i}', [128, 512], F32) for i in range(2)]
        psP = [self.ps(st, f'psP{i}', [128, 512], F32) for i in range(3)]
        psX = [self.ps(st, f'psX{i}', [128, 512], F32) for i in range(2)]
        yd, gtd, xnd = d[f'y{l}'], d[f'gT{l}'], d[f'xn{l}']
        wob = d[f'wob{l}'].rearrange("k p (j c) -> j p k c", c=128)
        gview = gtd.rearrange("(b j) p s -> j p b s", b=3)
        allwob = [self.db('wob', l, b) for b in range(56)]
        nt = 0
        for tt in range(NT):
            tok0 = tt * 512
            for c4 in range(4):
                gc = tt * 4 + c4
                y_ = ytok[gc % 2]
                tk.dma(sp, out=y_[:], in_=yd[gc * 128:(gc + 1) * 128, :], reads=[self.db('y', l, gc)], writes=[y_])
                for t8 in range(5):
                    p_ = psT[nt % 2]
                    nt += 1
                    for i in range(8):
                        _tr(tk, p_, i, y_[:, (t8 * 8 + i) * 128:(t8 * 8 + i + 1) * 128], [y_], self, last=(i == 7))
                    tk.op(act, lambda e: e.activation(out=yT[:, t8 * 8:(t8 + 1) * 8, c4 * 128:(c4 + 1) * 128],
                                                      in_=p_.t[:].bitcast(BF16).rearrange("p (t e) -> p t e", t=8), func=AF.Copy), reads=[p_], writes=[yT])
            for jj in range(16):
                w_, g_ = wa[jj % 2], gt[jj % 2]
                tk.dma(sp, out=w_[:], in_=wob[jj, :, 0:40, :], reads=allwob, writes=[w_])
                tk.dma(sp, out=g_[:], in_=gview[jj, :, :, tok0:tok0 + 512], reads=[self.db('gT', l, tt)], writes=[g_])
                for br, (k0, nk) in enumerate(((0, 16), (16, 16), (32, 8))):
                    for k in range(nk):
                        tk.op(pe, lambda e: e.matmul(psP[br][:], lhsT=w_[:, k0 + k, :], rhs=yT[:, k0 + k, :], start=(k == 0), stop=(k == nk - 1)),
                              reads=[w_, yT], writes=[psP[br]], inc=(k == nk - 1))
                f_, h_ = mf[jj % 2], mg[jj % 2]
                tk.op(dve, lambda e: e.tensor_tensor(out=f_[:], in0=psP[0][:], in1=g_[:, 0, :], op=ALU.mult), reads=[psP[0], g_], writes=[f_])
                tk.op(dve, lambda e: e.tensor_tensor(out=h_[:], in0=psP[1][:], in1=g_[:, 1, :], op=ALU.mult), reads=[psP[1], g_], writes=[h_])
                tk.op(pool, lambda e: e.tensor_tensor(out=f_[:], in0=f_[:], in1=h_[:], op=ALU.add), reads=[f_, h_], writes=[f_])
                tk.op(dve, lambda e: e.tensor_tensor(out=h_[:], in0=psP[2][:], in1=g_[:, 2, :], op=ALU.mult), reads=[psP[2], g_], writes=[h_])
                tk.op(pool, lambda e: e.tensor_tensor(out=mT[:, jj, :], in0=f_[:], in1=h_[:], op=ALU.add), reads=[f_, h_], writes=[mT])
            for jj in range(16):
                w_, x_, px = wo[jj % 2], xt[jj % 2], psX[jj % 2]
                tk.dma(sp, out=w_[:], in_=wob[jj, :, 40:56, :], reads=allwob, writes=[w_])
                tk.dma(sp, out=x_[:], in_=xin[jj * 128:(jj + 1) * 128, tok0:tok0 + 512], reads=[self.db(*xkey, tt)], writes=[x_])
                for k in range(16):
                    tk.op(pe, lambda e: e.matmul(px[:], lhsT=w_[:, k, :], rhs=mT[:, k, :], start=(k == 0), stop=(k == 15)),
                          reads=[w_, mT], writes=[px], inc=(k == 15))
                tk.op(dve, lambda e: e.tensor_tensor(out=x_[:], in0=px[:], in1=x_[:], op=ALU.add), reads=[px, x_], writes=[x_])
                tk.dma(pool, out=xnd[jj * 128:(jj + 1) * 128, tok0:tok0 + 512], in_=x_[:], reads=[x_], writes=[self.db('xn', l, tt)])
        tk.barrier()


def stage_final(self):
    tk, d, S, NCH = self.tk, self.dram, self.S, self.NCH
    pe, act, dve, pool, sp = tk.pe, tk.act, tk.dve, tk.pool, tk.sp
    l = self.depth - 1
    with ExitStack() as st:
        xs = [self.sb(st, f'fx{i}', [128, 16, 128], F32) for i in range(2)]
        sq = self.sb(st, 'fsq', [128, 16, 128], F32)
        rstd = self.sb(st, 'frstd', [128, 128], F32)
        gc_ = self.sb(st, 'fg', [128, 16], F32)
        pss = self.ps(st, 'fpss', [128, 128], F32)
        tk.dma(sp, out=gc_[:], in_=d['fgc'], writes=[gc_])
        xview = d[f'xn{l}'].rearrange("(k p) s -> p k s", p=128)
        oview = d['outT'].rearrange("(k p) s -> p k s", p=128)
        for gc in range(NCH):
            x_ = xs[gc % 2]
            cs = slice(gc * 128, (gc + 1) * 128)
            tk.dma(sp, out=x_[:], in_=xview[:, :, cs], reads=[self.db('xn', l, gc // 4)], writes=[x_])
            tk.op(act, lambda e: e.activation(out=sq[:], in_=x_[:], func=AF.Square), reads=[x_], writes=[sq])
            for k in range(16):
                tk.op(pe, lambda e: e.matmul(pss[:], lhsT=self.ones_f(), rhs=sq[:, k, :], start=(k == 0), stop=(k == 15)),
                      reads=[sq, self.c32], writes=[pss], inc=(k == 15))
            tk.op(act, lambda e: e.activation(out=rstd[:], in_=pss[:], func=AF.Sqrt, bias=EPS, scale=1.0 / D), reads=[pss], writes=[rstd])
            tk.op(dve, lambda e: e.reciprocal(out=rstd[:], in_=rstd[:]), reads=[rstd], writes=[rstd])
            for k in range(16):
                tk.op(dve, lambda e: e.scalar_tensor_tensor(out=x_[:, k, :], in0=x_[:, k, :], scalar=gc_[:, k:k + 1], in1=rstd[:],
                                                            op0=ALU.mult, op1=ALU.mult), reads=[x_, gc_, rstd], writes=[x_])
            tk.dma(pool, out=oview[:, :, cs], in_=x_[:], reads=[x_], writes=[self.db('out')])
        tk.barrier()


Prog.stage_C = stage_C
Prog.stage_final = stage_final
```

```python
import math
from contextlib import ExitStack

import numpy as np
import concourse.bass as bass
import concourse.mybir as mybir
from concourse.bass_utils import run_bass_kernel_spmd

F32 = mybir.dt.float32
BF16 = mybir.dt.bfloat16
AF = mybir.ActivationFunctionType
ALU = mybir.AluOpType

D = 2048
DEPTH = 2
EPS = 1e-6
NRING = 12
SAME_ENG_SYNC = False

SPL = dict(z=(0, 2048), xbc=(2048, 5120), dt=(5120, 5152), rq=(5152, 6176), rk=(6176, 7200),
           rv=(7200, 9248), rg=(9248, 11296), dqkv=(11296, 20512), dg=(20512, 21536), mg=(21536, 27680))
TM_Z, TM_RQ, TM_RK, TM_RV, TM_RG, TM_DQKV, TM_DG = 0, 2048, 3072, 4096, 6144, 8192, 17408
TM_W = 18432
N_TMBLK = 36
N_FMBLK = 18
N_BLK = N_TMBLK + 1 + N_FMBLK


def tm_block_kind(b):
    c = b * 512
    if c < TM_RQ:
        return 'silu'
    if c < TM_RK:
        return 'rope_q'
    if c < TM_RV:
        return 'rope_k'
    if c < TM_RG:
        return 'copy'
    if c < TM_DQKV:
        return 'silu'
    if c < TM_DG:
        j = ((c - TM_DQKV) // 1024) % 3
        return ('rope_q', 'rope_k', 'copy')[j]
    return 'silu'


class Buf:
    __slots__ = ('t', 'w', 'r', 'multi', 'small')

    def __init__(self, t=None, multi=False, small=False):
        self.t = t
        self.w = {}
        self.r = {}
        self.multi = multi
        self.small = small

    def __getitem__(self, idx):
        return self.t[idx]


class Eng:
    def __init__(self, name, e, sem):
        self.name, self.e, self.sem = name, e, sem
        self.cnt = 0
        self.seen = {}


class TK:
    def __init__(self, nc, es):
        self.nc = nc
        mk = lambda n: es.enter_context(nc.semaphore(n))
        self.pe = Eng('pe', nc.tensor, mk('s_pe'))
        self.act = Eng('act', nc.scalar, mk('s_act'))
        self.dve = Eng('dve', nc.vector, mk('s_dve'))
        self.pool = Eng('pool', nc.gpsimd, mk('s_pool'))
        self.sp = Eng('sp', nc.sync, mk('s_sp'))
        self.engs = [self.pe, self.act, self.dve, self.pool, self.sp]
        self.dq = {}
        for E in (self.sp, self.pool, self.act):
            self.dq[E.name] = dict(sems=[mk(f'd_{E.name}{i}') for i in range(NRING)], vals=[0] * NRING, i=0)

    def _wait(self, E, toks):
        for tk_ in toks:
            sem, val, owner = tk_[0], tk_[1], tk_[2]
            small = len(tk_) > 3 and tk_[3]
            if val <= 0:
                continue
            if owner is E and (E is self.pe or not (SAME_ENG_SYNC or small)):
                continue
            k = id(sem)
            if E.seen.get(k, 0) >= val:
                continue
            E.e.wait_ge(sem, val)
            E.seen[k] = val

    @staticmethod
    def _deps(reads, writes):
        toks = []
        for b in reads:
            toks += [t + (True,) for t in b.w.values()]
        for b in writes:
            toks += [t + (True,) for t in b.w.values()]
            toks += list(b.r.values())
        return toks

    @staticmethod
    def _record(tok, reads, writes):
        k = id(tok[0])
        for b in reads:
            o = b.r.get(k)
            if o is None or o[1] < tok[1]:
                b.r[k] = tok
        for b in writes:
            if b.multi:
                o = b.w.get(k)
                if o is None or o[1] < tok[1]:
                    b.w[k] = tok
            else:
                b.w = {k: tok}
                b.r = {}

    def op(self, E, fn, reads=(), writes=(), inc=True):
        self._wait(E, self._deps(reads, writes))
        ins = fn(E.e)
        if inc:
            E.cnt += 1
            ins.then_inc(E.sem, 1)
            tok = (E.sem, E.cnt, E)
        else:
            tok = (E.sem, E.cnt + 1, E)
        self._record(tok, reads, writes)
        return tok

    def dma(self, E, out, in_, reads=(), writes=()):
        q = self.dq[E.name]
        k = q['i'] % NRING
        q['i'] += 1
        sem = q['sems'][k]
        prev = q['vals'][k]
        toks = self._deps(reads, writes)
        toks.append((sem, prev, None))
        self._wait(E, toks)
        E.e.dma_start(out=out, in_=in_).then_inc(sem, 16)
        q['vals'][k] = prev + 16
        tok = (sem, prev + 16, None)
        self._record(tok, reads, writes)
        return tok

    def barrier(self):
        toks = [(E.sem, E.cnt, E) for E in self.engs if E.cnt]
        for q in self.dq.values():
            for sem, val in zip(q['sems'], q['vals']):
                toks.append((sem, val, None))
        for E in self.engs:
            self._wait(E, toks)


class Prog:
    def __init__(self, S, depth=DEPTH, debug=False, stages='ABC'):
        self.S, self.depth, self.debug, self.stages = S, depth, debug, stages
        self.NCH = S // 128
        nc = self.nc = bass.Bass("TRN2", target_bir_lowering=False)
        self.es = ExitStack()
        self.tk = TK(nc, self.es)
        self.dram = {}
        self.dbuf = {}

    def din(self, name, shape, dt=F32):
        self.dram[name] = self.nc.dram_tensor(name, list(shape), dt, kind="ExternalInput").ap()
        return self.dram[name]

    def dscr(self, name, shape, dt, out=False):
        kind = "ExternalOutput" if (out or self.debug) else "Internal"
        self.dram[name] = self.nc.dram_tensor(name, list(shape), dt, kind=kind).ap()
        return self.dram[name]

    def db(self, *key):
        b = self.dbuf.get(key)
        if b is None:
            b = self.dbuf[key] = Buf(None, multi=True)
        return b

    def _uid(self, name):
        self._n = getattr(self, '_n', 0) + 1
        return f'{name}_u{self._n}'

    def sb(self, st, name, shape, dt=F32):
        small = int(np.prod(shape[1:])) <= 512
        return Buf(st.enter_context(self.nc.sbuf_tensor(self._uid('s_' + name), list(shape), dt)), small=small)

    def ps(self, st, name, shape, dt=F32):
        return Buf(st.enter_context(self.nc.psum_tensor(self._uid('p_' + name), list(shape), dt)))

    def build(self):
        S, NCH, tk = self.S, self.NCH, self.tk
        nc = self.nc
        self.din('xT', [D, S])
        self.din('w_in', [self.depth, N_BLK, 128, 16, 512])
        self.din('w_o', [self.depth, 56, 128, 2048])
        self.din('cvec', [self.depth, 128, 16 + 24 * 5 + 32 * 3])
        self.din('grow', [self.depth, 128, 2048])
        self.din('fgc', [128, 16])
        self.din('rope', [128, NCH, 128])
        self.din('ctab', [128, 128 * 6 + 8 * 128 * 2 + 8])
        for l in range(self.depth):
            self.dscr(f'wb{l}', [N_BLK, 128, 16, 512], BF16)
            self.dscr(f'wob{l}', [56, 128, 2048], BF16)
            self.dscr(f'tm{l}', [S, TM_W], BF16)
            self.dscr(f'dt{l}', [S, 32], F32)
            self.dscr(f'xbcT{l}', [24, 128, S], BF16)
            self.dscr(f'gT{l}', [48, 128, S], BF16)
            self.dscr(f'y{l}', [S, 5120], BF16)
            self.dscr(f'od{l}', [3, S, 8, 129], F32)
            self.dscr(f'xn{l}', [D, S], F32)
        self.dscr('outT', [D, S], F32, out=True)
        if self.debug:
            self.dscr('dbgA', [S, 1032], F32)
            self.dscr('dbgB', [S, 1024], F32)
            self.dscr('dbgC', [S, 8], F32)

        with ExitStack() as gs:
            self.gs = gs
            self.consts(gs)
            self.cast_weights(0)
            for l in range(self.depth):
                xin = self.dram['xT'] if l == 0 else self.dram[f'xn{l - 1}']
                xkey = ('xT',) if l == 0 else ('xn', l - 1)
                if 'A' in self.stages:
                    self.stage_A(l, xin, xkey)
                if l + 1 < self.depth:
                    self.cast_weights(l + 1)
                if 'B' in self.stages or '1' in self.stages:
                    self.stage_B1(l)
                if 'B' in self.stages or '2' in self.stages:
                    self.stage_B2(l)
                if 'B' in self.stages or '3' in self.stages:
                    self.stage_B3(l)
                if 'C' in self.stages:
                    self.stage_C(l, xin, xkey)
            if 'C' in self.stages:
                self.stage_final()
            tk.barrier()
        self.es.close()
        return nc

    def consts(self, gs):
        tk, d = self.tk, self.dram
        self.c32 = self.sb(gs, 'c32', [128, 768], F32)
        self.cbf = self.sb(gs, 'cbf', [128, 768], BF16)
        tk.dma(tk.sp, out=self.c32[:], in_=d['ctab'][:, 0:768], writes=[self.c32])
        tk.op(tk.dve, lambda e: e.tensor_copy(out=self.cbf[:], in_=self.c32[:]), reads=[self.c32], writes=[self.cbf])
        self.ident_f = lambda: self.c32[:, 0:128]
        self.ones_f = lambda: self.c32[:, 128:256]
        self.U_f = lambda: self.c32[:, 256:384]
        self.Us_f = lambda: self.c32[:, 384:512]
        self.ident_b = lambda: self.cbf[:, 0:128]
        self.negcur_b = lambda: self.cbf[:, 512:640]
        self.negprev_b = lambda: self.cbf[:, 640:768]

    def cast_weights(self, l):
        tk, d = self.tk, self.dram
        if True:
            src = d['w_in'][l].rearrange("b p (q k) c -> b (p q) (k c)", q=4)
            dst = d[f'wb{l}'].rearrange("b p (q k) c -> b (p q) (k c)", q=4)
            for b in range(N_BLK):
                tk.dma(tk.pool, out=dst[b], in_=src[b], writes=[self.db('wb', l, b)])
            src = d['w_o'][l]
            dst = d[f'wob{l}']
            for b in range(56):
                tk.dma(tk.pool, out=dst[b], in_=src[b], writes=[self.db('wob', l, b)])

    def stage_A(self, l, xin, xkey):
        tk, d, S = self.tk, self.dram, self.S
        pe, act, dve, pool, sp = tk.pe, tk.act, tk.dve, tk.pool, tk.sp
        TS = 1024
        NSUP = S // TS
        with ExitStack() as st:
            hT = self.sb(st, 'hT', [128, 16, TS], BF16)
            xs = [self.sb(st, f'xs{i}', [128, 16, 128], F32) for i in range(2)]
            sq = self.sb(st, 'sq', [128, 16, 128], F32)
            rstd = self.sb(st, 'rstd', [128, 128], F32)
            wb = [self.sb(st, f'wb{i}', [128, 16, 512], BF16) for i in range(2)]
            ropet = self.sb(st, 'ropet', [128, 8, 128], F32)
            cv = self.sb(st, 'cvA', [128, 232], F32)
            halo = self.sb(st, 'halo', [128, 24, 3], F32)
            u = [self.sb(st, f'u{i}', [128, 515], F32) for i in range(2)]
            acc = [self.sb(st, f'acc{i}', [128, 512], F32) for i in range(2)]
            tsb = [self.sb(st, f'tsb{i}', [128, 512], F32) for i in range(2)]
            ra = [self.sb(st, f'ra{i}', [128, 512], F32) for i in range(2)]
            rb = [self.sb(st, f'rb{i}', [128, 512], F32) for i in range(2)]
            ob = [self.sb(st, f'ob{i}', [128, 512], BF16) for i in range(4)]
            dts = [self.sb(st, f'dts{i}', [128, 32], F32) for i in range(2)]
            psA = [self.ps(st, f'psA{i}', [128, 512], F32) for i in range(4)]
            pss = self.ps(st, 'pss', [128, 128], F32)

            tk.dma(sp, out=cv[:], in_=d['cvec'][l], writes=[cv])
            tk.op(dve, lambda e: e.memset(halo[:], 0.0), writes=[halo])
            xview = xin.rearrange("(k p) s -> p k s", p=128)
            tmd, dtd, xbd, gtd = d[f'tm{l}'], d[f'dt{l}'], d[f'xbcT{l}'], d[f'gT{l}']
            cnt = dict(ps=0, ob=0, ts=0, u=0, dt=0)

            for su in range(NSUP):
                for c8 in range(8):
                    gc = su * 8 + c8
                    x_ = xs[gc % 2]
                    tk.dma(sp, out=x_[:], in_=xview[:, :, gc * 128:(gc + 1) * 128],
                           reads=[self.db(*xkey, gc // 4)], writes=[x_])
                    tk.op(act, lambda e: e.activation(out=sq[:], in_=x_[:], func=AF.Square), reads=[x_], writes=[sq])
                    for k in range(16):
                        tk.op(pe, lambda e: e.matmul(pss[:], lhsT=self.ones_f(), rhs=sq[:, k, :], start=(k == 0), stop=(k == 15)),
                              reads=[sq, self.c32], writes=[pss], inc=(k == 15))
                    tk.op(act, lambda e: e.activation(out=rstd[:], in_=pss[:], func=AF.Sqrt, bias=EPS, scale=1.0 / D),
                          reads=[pss], writes=[rstd])
                    tk.op(dve, lambda e: e.reciprocal(out=rstd[:], in_=rstd[:]), reads=[rstd], writes=[rstd])
                    for k in range(16):
                        tk.op(dve, lambda e: e.scalar_tensor_tensor(out=hT[:, k, c8 * 128:(c8 + 1) * 128], in0=x_[:, k, :],
                                                                    scalar=cv[:, k:k + 1], in1=rstd[:], op0=ALU.mult, op1=ALU.mult),
                              reads=[x_, cv, rstd], writes=[hT])
                tk.dma(sp, out=ropet[:], in_=d['rope'][:, su * 8:(su + 1) * 8, :], writes=[ropet])

                for blk in range(N_BLK):
                    w_ = wb[blk % 2]
                    tk.dma(sp, out=w_[:], in_=d[f'wb{l}'][blk], reads=[self.db('wb', l, blk)], writes=[w_])
                    if blk <= N_TMBLK:
                        kind = 'dt' if blk == N_TMBLK else tm_block_kind(blk)
                        ncol = 32 if kind == 'dt' else 512
                        for c8 in range(8):
                            gc = su * 8 + c8
                            ps_ = psA[cnt['ps'] % 4]
                            cnt['ps'] += 1
                            for k in range(16):
                                tk.op(pe, lambda e: e.matmul(ps_[:, 0:ncol], lhsT=hT[:, k, c8 * 128:(c8 + 1) * 128], rhs=w_[:, k, 0:ncol],
                                                             start=(k == 0), stop=(k == 15)),
                                      reads=[hT, w_], writes=[ps_], inc=(k == 15))
                            rows = slice(gc * 128, (gc + 1) * 128)
                            if kind == 'dt':
                                t_ = dts[cnt['dt'] % 2]
                                cnt['dt'] += 1
                                tk.op(dve, lambda e: e.tensor_tensor(out=t_[:], in0=ps_[:, 0:32], in1=cv[:, 136:168], op=ALU.add),
                                      reads=[ps_, cv], writes=[t_])
                                tk.op(act, lambda e: e.activation(out=t_[:], in_=t_[:], func=AF.Exp), reads=[t_], writes=[t_])
                                tk.op(act, lambda e: e.activation(out=t_[:], in_=t_[:], func=AF.Ln, bias=1.0), reads=[t_], writes=[t_])
                                tk.dma(pool, out=dtd[rows, :], in_=t_[:], reads=[t_], writes=[self.db('dt', l, gc)])
                                continue
                            o_ = ob[cnt['ob'] % 4]
                            cnt['ob'] += 1
                            if kind == 'silu':
                                tk.op(act, lambda e: e.activation(out=o_[:], in_=ps_[:], func=AF.Silu), reads=[ps_], writes=[o_])
                            elif kind == 'copy':
                                tk.op(dve, lambda e: e.tensor_copy(out=o_[:], in_=ps_[:]), reads=[ps_], writes=[o_])
                            else:
                                j = cnt['ts'] % 2
                                cnt['ts'] += 1
                                t_, a_, b_ = tsb[j], ra[j], rb[j]
                                sc = 1.0 if kind == 'rope_q' else 128.0 ** -0.5
                                tk.op(act, lambda e: e.activation(out=t_[:], in_=ps_[:], func=AF.Copy, scale=sc), reads=[ps_], writes=[t_])
                                v4 = lambda b: b[:].rearrange("p (h two e) -> p h two e", h=4, two=2)
                                cosb = ropet[:, c8, 0:64].unsqueeze(1).unsqueeze(1).to_broadcast([128, 4, 2, 64])
                                sinb = ropet[:, c8, 64:128].unsqueeze(1).unsqueeze(1).to_broadcast([128, 4, 2, 64])
                                tk.op(dve, lambda e: e.tensor_tensor(out=v4(a_), in0=v4(t_), in1=cosb, op=ALU.mult), reads=[t_, ropet], writes=[a_])
                                tk.op(dve, lambda e: e.tensor_tensor(out=v4(b_), in0=v4(t_), in1=sinb, op=ALU.mult), reads=[t_, ropet], writes=[b_])
                                tk.op(dve, lambda e: e.tensor_tensor(out=v4(o_)[:, :, 0, :], in0=v4(a_)[:, :, 0, :], in1=v4(b_)[:, :, 1, :], op=ALU.subtract),
                                      reads=[a_, b_], writes=[o_])
                                tk.op(dve, lambda e: e.tensor_tensor(out=v4(o_)[:, :, 1, :], in0=v4(a_)[:, :, 1, :], in1=v4(b_)[:, :, 0, :], op=ALU.add),
                                      reads=[a_, b_], writes=[o_])
                            tk.dma(pool, out=tmd[rows, blk * 512:(blk + 1) * 512], in_=o_[:], reads=[o_], writes=[self.db('tm', l, gc)])
                    else:
                        fb = blk - N_TMBLK - 1
                        for ct4 in range(4):
                            ct = fb * 4 + ct4
                            for tt in range(2):
                                ps_ = psA[cnt['ps'] % 4]
                                cnt['ps'] += 1
                                for k in range(16):
                                    tk.op(pe, lambda e: e.matmul(ps_[:], lhsT=w_[:, k, ct4 * 128:(ct4 + 1) * 128], rhs=hT[:, k, tt * 512:(tt + 1) * 512],
                                                                 start=(k == 0), stop=(k == 15)),
                                          reads=[hT, w_], writes=[ps_], inc=(k == 15))
                                tok0 = su * TS + tt * 512
                                o_ = ob[cnt['ob'] % 4]
                                cnt['ob'] += 1
                                if ct >= 24:
                                    tk.op(act, lambda e: e.activation(out=o_[:], in_=ps_[:], func=AF.Sigmoid), reads=[ps_], writes=[o_])
                                    tk.dma(pool, out=gtd[ct - 24, :, tok0:tok0 + 512], in_=o_[:], reads=[o_], writes=[self.db('gT', l, tok0 // 512)])
                                    continue
                                u_ = u[cnt['u'] % 2]
                                a_ = acc[cnt['u'] % 2]
                                cnt['u'] += 1
                                tk.op(dve, lambda e: e.tensor_copy(out=u_[:, 0:3], in_=halo[:, ct, :]), reads=[halo], writes=[u_])
                                tk.op(act, lambda e: e.activation(out=u_[:, 3:515], in_=ps_[:], func=AF.Copy), reads=[ps_], writes=[u_])
                                tk.op(dve, lambda e: e.tensor_copy(out=halo[:, ct, :], in_=u_[:, 512:515]), reads=[u_], writes=[halo])
                                cw = lambda kk: cv[:, 16 + ct * 5 + kk:16 + ct * 5 + kk + 1]
                                tk.op(dve, lambda e: e.tensor_scalar(out=a_[:], in0=u_[:, 3:515], scalar1=cw(3), scalar2=cw(4), op0=ALU.mult, op1=ALU.add),
                                      reads=[u_, cv], writes=[a_])
                                for kk in range(3):
                                    tk.op(dve, lambda e: e.scalar_tensor_tensor(out=a_[:], in0=u_[:, kk:kk + 512], scalar=cw(kk), in1=a_[:],
                                                                                op0=ALU.mult, op1=ALU.add), reads=[u_, cv, a_], writes=[a_])
                                tk.op(act, lambda e: e.activation(out=o_[:], in_=a_[:], func=AF.Silu), reads=[a_], writes=[o_])
                                tk.dma(pool, out=xbd[ct, :, tok0:tok0 + 512], in_=o_[:], reads=[o_], writes=[self.db('xbcT', l, tok0 // 512)])
            tk.barrier()


def _blocks(w, nblk):
    return np.ascontiguousarray(w.reshape(16, 128, nblk, 512).transpose(2, 1, 0, 3))


def prep_weights(inp, depth=DEPTH):
    w_in_l, w_o_l, cvec_l, grow_l = [], [], [], []
    for l in range(depth):
        w = inp['w_in'][l]
        sl = lambda n: w[:, SPL[n][0]:SPL[n][1]]
        tm = np.concatenate([sl('z'), sl('rq'), sl('rk'), sl('rv'), sl('rg'), sl('dqkv'), sl('dg')], axis=1)
        dtp = np.zeros((D, 512), np.float32)
        dtp[:, 0:32] = sl('dt')
        fm = np.concatenate([sl('xbc'), sl('mg')], axis=1)
        w_in_l.append(np.concatenate([_blocks(tm, N_TMBLK), _blocks(dtp, 1), _blocks(fm, N_FMBLK)], axis=0))
        wo = np.concatenate([inp['w_o_ssd'][l], inp['w_o_ret'][l], inp['w_o_dil'][l], inp['w_out'][l]], axis=0)
        w_o_l.append(np.ascontiguousarray(wo.reshape(56, 128, 2048)))
        cv = np.zeros((128, 232), np.float32)
        cv[:, 0:16] = inp['norm_g'][l].reshape(16, 128).T
        cw = inp['conv_w'][l].reshape(4, 24, 128)
        cb = inp['conv_b'][l].reshape(24, 128)
        for ct in range(24):
            cv[:, 16 + ct * 5:16 + ct * 5 + 4] = cw[:, ct, :].T
            cv[:, 16 + ct * 5 + 4] = cb[ct]
        cv[:, 136:168] = inp['dt_bias'][l][None, :]
        cv[:, 168:200] = inp['a_log'][l][None, :]
        cv[:, 200:232] = inp['d_skip'][l][None, :]
        cvec_l.append(cv)
        grow_l.append(np.ascontiguousarray(np.broadcast_to(inp['ssd_norm_g'][l][None, :], (128, 2048))))
    fgc = np.ascontiguousarray(inp['final_norm_g'].reshape(16, 128).T)
    return dict(w_in=np.stack(w_in_l), w_o=np.stack(w_o_l), cvec=np.stack(cvec_l), grow=np.stack(grow_l), fgc=fgc)


def prep_consts(S):
    NCH = S // 128
    pos = np.arange(S, dtype=np.float32)
    inv = (10000.0 ** (-np.arange(0, 128, 2, dtype=np.float32) / 128.0)).astype(np.float32)
    ang = pos[:, None] * inv[None, :]
    rope = np.concatenate([np.cos(ang), np.sin(ang)], axis=1).astype(np.float32)
    rope = np.ascontiguousarray(rope.reshape(NCH, 128, 128).transpose(1, 0, 2))
    i = np.arange(128)
    ct = np.zeros((128, 768 + 2048 + 8), np.float32)
    ct[:, 0:128] = np.eye(128)
    ct[:, 128:256] = 1.0
    ct[:, 256:384] = (i[:, None] <= i[None, :])
    ct[:, 384:512] = (i[:, None] > i[None, :])
    ct[:, 512:640] = np.where(i[None, :] >= i[:, None], 0.0, -30000.0)
    ct[:, 640:768] = np.where(i[:, None] >= i[None, :], 0.0, -30000.0)
    lg = np.log(1.0 - np.exp2(-5.0 - np.arange(8, dtype=np.float64)))
    for h in range(8):
        rel = (i[None, :] - i[:, None]).astype(np.float64)
        ct[:, 768 + h * 128:768 + (h + 1) * 128] = np.where(rel >= 0, np.exp(np.maximum(rel, 0) * lg[h]), 0.0)
        ct[:, 1792 + h * 128:1792 + (h + 1) * 128] = np.exp((i[None, :] + 1.0) * lg[h])
        ct[:, 2816 + h] = np.exp((127.0 - i) * lg[h])
    return dict(rope=rope, ctab=ct)


_CACHE = {}


def kernel(**inp):
    x = np.asarray(inp['x'], np.float32)
    B, S, _ = x.shape
    if 'nc' not in _CACHE:
        _CACHE['nc'] = Prog(S).build()
    nc = _CACHE['nc']
    shared = dict(prep_weights(inp))
    shared.update(prep_consts(S))
    in_maps = []
    for b in range(B):
        m = dict(shared)
        m['xT'] = np.ascontiguousarray(x[b].T)
        in_maps.append(m)
    res = run_bass_kernel_spmd(nc, in_maps, core_ids=list(range(B)))
    out = np.stack([np.ascontiguousarray(r['outT'].T) for r in res.results], axis=0)
    return out.astype(np.float32)


def _tr(tk, ps_bf, slot, src_ap, src_bufs, P, last=True):
    tk.op(tk.pe, lambda e: e.transpose(out=ps_bf.t[:].bitcast(BF16)[:, slot * 128:(slot + 1) * 128], in_=src_ap, identity=P.ident_b()),
          reads=list(src_bufs) + [P.cbf], writes=[ps_bf], inc=last)


def stage_B1(self, l):
    tk, d, S, NCH = self.tk, self.dram, self.S, self.NCH
    pe, act, dve, pool, sp = tk.pe, tk.act, tk.dve, tk.pool, tk.sp
    with ExitStack() as st:
        xbl = self.sb(st, 'xbl', [128, 24, 512], BF16)
        x_tok_2 = [self.sb(st, f'x_tok{i}', [128, 2048], BF16) for i in range(2)]
        B_tok_2 = [self.sb(st, f'B_tok{i}', [128, 512], BF16) for i in range(2)]
        zs_2 = [self.sb(st, f'zs{i}', [128, 2048], BF16) for i in range(2)]
        xdt_2 = [self.sb(st, f'xdt{i}', [128, 32, 64], BF16) for i in range(2)]
        xdte_2 = [self.sb(st, f'xdte{i}', [128, 32, 64], BF16) for i in range(2)]
        lall = self.sb(st, 'lall', [128, 32, 128], F32)
        cbm_2 = [self.sb(st, f'cbm{i}', [128, 4, 128], F32) for i in range(2)]
        Lm = [self.sb(st, f'Lm{i}', [128, 4, 128], F32) for i in range(2)]
        M_2 = [self.sb(st, f'M{i}', [128, 32, 128], BF16) for i in range(2)]
        yt_2 = [self.sb(st, f'yt{i}', [128, 2048], F32) for i in range(2)]
        tmp = self.sb(st, 'tmp', [128, 2048], F32)
        Hs = self.sb(st, 'Hs', [128, 2048], F32)
        Hb = self.sb(st, 'Hb', [128, 2048], BF16)
        grow = self.sb(st, 'grow', [128, 2048], F32)
        ya_2 = [self.sb(st, f'ya{i}', [128, 2048], BF16) for i in range(2)]
        cv = self.sb(st, 'cvB', [128, 232], F32)
        arow = self.sb(st, 'arow', [128, 32], F32)
        dt_t_2 = [self.sb(st, f'dt_t{i}', [128, 32], F32) for i in range(2)]
        dta_2 = [self.sb(st, f'dta{i}', [128, 32], F32) for i in range(2)]
        sS_2 = [self.sb(st, f'sS{i}', [128, 64], F32) for i in range(2)]
        eacs_2 = [self.sb(st, f'eacs{i}', [128, 32], F32) for i in range(2)]
        tend_2 = [self.sb(st, f'tend{i}', [128, 32], F32) for i in range(2)]
        cdr_2 = [self.sb(st, f'cdr{i}', [128, 32], F32) for i in range(2)]
        ss_2 = [self.sb(st, f'ss{i}', [128, 1], F32) for i in range(2)]
        psT = self.ps(st, 'psT', [128, 512], F32)
        psS = self.ps(st, 'psS', [128, 64], F32)
        psC = self.ps(st, 'psC', [128, 512], F32)
        psG = [self.ps(st, f'psG{i}', [128, 512], F32) for i in range(2)]
        psD = self.ps(st, 'psD', [128, 512], F32)
        psO = self.ps(st, 'psO', [128, 512], F32)
        psN = self.ps(st, 'psN', [128, 512], F32)

        tk.dma(sp, out=cv[:], in_=d['cvec'][l], writes=[cv])
        tk.dma(sp, out=grow[:], in_=d['grow'][l], writes=[grow])
        tk.op(act, lambda e: e.activation(out=arow[:], in_=cv[:, 168:200], func=AF.Exp), reads=[cv], writes=[arow])
        tk.op(dve, lambda e: e.tensor_scalar(out=arow[:], in0=arow[:], scalar1=-1.0, scalar2=None, op0=ALU.mult), reads=[arow], writes=[arow])
        tk.op(dve, lambda e: e.memset(Hs[:], 0.0), writes=[Hs])
        tk.op(dve, lambda e: e.memset(Hb[:], 0.0), writes=[Hb])
        tmd, dtd, xbd, yd = d[f'tm{l}'], d[f'dt{l}'], d[f'xbcT{l}'], d[f'y{l}']
        bc3 = lambda ap, n: ap.unsqueeze(2).to_broadcast([128, ap.shape[1], n])
        for gc in range(NCH):
            rows = slice(gc * 128, (gc + 1) * 128)
            sub = gc % 4
            x_tok = x_tok_2[gc % 2]
            B_tok = B_tok_2[gc % 2]
            zs = zs_2[gc % 2]
            xdt = xdt_2[gc % 2]
            xdte = xdte_2[gc % 2]
            cbm = cbm_2[gc % 2]
            M = M_2[gc % 2]
            yt = yt_2[gc % 2]
            ya = ya_2[gc % 2]
            dt_t = dt_t_2[gc % 2]
            dta = dta_2[gc % 2]
            sS = sS_2[gc % 2]
            eacs = eacs_2[gc % 2]
            tend = tend_2[gc % 2]
            cdr = cdr_2[gc % 2]
            ss = ss_2[gc % 2]
            ts_ = slice(sub * 128, (sub + 1) * 128)
            if sub == 0:
                tk.dma(sp, out=xbl[:], in_=xbd[:, :, gc * 128:gc * 128 + 512].rearrange("t p s -> p t s"),
                       reads=[self.db('xbcT', l, gc // 4)], writes=[xbl])
            tk.dma(sp, out=dt_t[:], in_=dtd[rows, :], reads=[self.db('dt', l, gc)], writes=[dt_t])
            tk.dma(sp, out=zs[:], in_=tmd[rows, TM_Z:TM_Z + 2048], reads=[self.db('tm', l, gc)], writes=[zs])
            for half in range(2):
                for i in range(8):
                    _tr(tk, psT, i, xbl[:, half * 8 + i, ts_], [xbl], self, last=(i == 7))
                tk.op(act, lambda e: e.activation(out=x_tok[:, half * 1024:(half + 1) * 1024], in_=psT.t[:].bitcast(BF16), func=AF.Copy),
                      reads=[psT], writes=[x_tok])
            for i in range(4):
                _tr(tk, psT, i, xbl[:, 16 + i, ts_], [xbl], self, last=(i == 3))
            tk.op(act, lambda e: e.activation(out=B_tok[:], in_=psT.t[:].bitcast(BF16)[:, 0:512], func=AF.Copy), reads=[psT], writes=[B_tok])
            tk.op(dve, lambda e: e.tensor_tensor(out=dta[:], in0=dt_t[:], in1=arow[:], op=ALU.mult), reads=[dt_t, arow], writes=[dta])
            tk.op(pe, lambda e: e.matmul(psS[:, 0:32], lhsT=self.U_f(), rhs=dta[:], start=True, stop=True), reads=[dta, self.c32], writes=[psS], inc=False)
            tk.op(pe, lambda e: e.matmul(psS[:, 32:64], lhsT=self.ones_f(), rhs=dta[:], start=True, stop=True), reads=[dta, self.c32], writes=[psS])
            tk.op(act, lambda e: e.activation(out=sS[:], in_=psS[:], func=AF.Copy), reads=[psS], writes=[sS])
            tk.op(act, lambda e: e.activation(out=eacs[:], in_=sS[:, 0:32], func=AF.Exp), reads=[sS], writes=[eacs])
            tk.op(dve, lambda e: e.tensor_tensor(out=tend[:], in0=sS[:, 32:64], in1=sS[:, 0:32], op=ALU.subtract), reads=[sS], writes=[tend])
            tk.op(act, lambda e: e.activation(out=tend[:], in_=tend[:], func=AF.Exp), reads=[tend], writes=[tend])
            tk.op(act, lambda e: e.activation(out=cdr[:], in_=sS[:, 32:64], func=AF.Exp), reads=[sS], writes=[cdr])
            xv = x_tok[:].rearrange("p (h e) -> p h e", h=32)
            tk.op(dve, lambda e: e.tensor_tensor(out=xdt[:], in0=xv, in1=bc3(dt_t[:], 64), op=ALU.mult), reads=[x_tok, dt_t], writes=[xdt])
            tk.op(dve, lambda e: e.tensor_tensor(out=xdte[:], in0=xdt[:], in1=bc3(tend[:], 64), op=ALU.mult), reads=[xdt, tend], writes=[xdte])
            usb = self.Us_f().unsqueeze(1).to_broadcast([128, 16, 128])
            tk.op(dve, lambda e: e.tensor_tensor(out=lall[:, 0:16, :], in0=bc3(dta[:, 0:16], 128), in1=usb, op=ALU.mult), reads=[dta, self.c32], writes=[lall])
            tk.op(dve, lambda e: e.tensor_tensor(out=lall[:, 16:32, :], in0=bc3(dta[:, 16:32], 128), in1=usb, op=ALU.mult), reads=[dta, self.c32], writes=[lall])
            for g in range(4):
                tk.op(pe, lambda e: e.matmul(psC[:, g * 128:(g + 1) * 128], lhsT=xbl[:, 16 + g, ts_], rhs=xbl[:, 20 + g, ts_], start=True, stop=True),
                      reads=[xbl], writes=[psC], inc=(g == 3))
            ub = self.U_f().unsqueeze(1).to_broadcast([128, 4, 128])
            tk.op(dve, lambda e: e.tensor_tensor(out=cbm[:], in0=psC[:].rearrange("p (g e) -> p g e", g=4), in1=ub, op=ALU.mult), reads=[psC, self.c32], writes=[cbm])
            for hq in range(8):
                pg, lm = psG[hq % 2], Lm[hq % 2]
                for i in range(4):
                    tk.op(pe, lambda e: e.matmul(pg[:, i * 128:(i + 1) * 128], lhsT=lall[:, hq * 4 + i, :], rhs=self.U_f(), start=True, stop=True),
                          reads=[lall, self.c32], writes=[pg], inc=(i == 3))
                tk.op(act, lambda e: e.activation(out=lm[:], in_=pg[:].rearrange("p (g e) -> p g e", g=4), func=AF.Exp), reads=[pg], writes=[lm])
                cb_b = cbm[:, hq // 2, :].unsqueeze(1).to_broadcast([128, 4, 128])
                tk.op(dve, lambda e: e.tensor_tensor(out=M[:, hq * 4:(hq + 1) * 4, :], in0=lm[:], in1=cb_b, op=ALU.mult), reads=[lm, cbm], writes=[M])
            for g in range(4):
                gs_ = slice(g * 512, (g + 1) * 512)
                for i in range(8):
                    h = g * 8 + i
                    tk.op(pe, lambda e: e.matmul(psD[:, i * 64:(i + 1) * 64], lhsT=M[:, h, :], rhs=xdt[:, h, :], start=True, stop=True),
                          reads=[M, xdt], writes=[psD], inc=(i == 7))
                tk.op(pe, lambda e: e.matmul(psO[:], lhsT=xbl[:, 20 + g, ts_], rhs=Hb[:, gs_], start=True, stop=True), reads=[xbl, Hb], writes=[psO])
                tk.op(pe, lambda e: e.matmul(psN[:], lhsT=B_tok[:, g * 128:(g + 1) * 128], rhs=xdte[:, g * 8:(g + 1) * 8, :], start=True, stop=True),
                      reads=[B_tok, xdte], writes=[psN])
                v8 = lambda ap: ap.rearrange("p (h e) -> p h e", h=8)
                tk.op(dve, lambda e: e.tensor_tensor(out=v8(yt[:, gs_]), in0=v8(psO[:]), in1=bc3(eacs[:, g * 8:(g + 1) * 8], 64), op=ALU.mult),
                      reads=[psO, eacs], writes=[yt])
                tk.op(dve, lambda e: e.tensor_tensor(out=yt[:, gs_], in0=psD[:], in1=yt[:, gs_], op=ALU.add), reads=[psD, yt], writes=[yt])
                tk.op(dve, lambda e: e.tensor_tensor(out=v8(Hs[:, gs_]), in0=v8(Hs[:, gs_]), in1=bc3(cdr[:, g * 8:(g + 1) * 8], 64), op=ALU.mult),
                      reads=[Hs, cdr], writes=[Hs])
                tk.op(dve, lambda e: e.tensor_tensor(out=Hs[:, gs_], in0=psN[:], in1=Hs[:, gs_], op=ALU.add), reads=[psN, Hs], writes=[Hs])
                tk.op(act, lambda e: e.activation(out=Hb[:, gs_], in_=Hs[:, gs_], func=AF.Copy), reads=[Hs], writes=[Hb])
            tk.op(dve, lambda e: e.tensor_tensor(out=tmp[:].rearrange("p (h e) -> p h e", h=32), in0=xv, in1=bc3(cv[:, 200:232], 64), op=ALU.mult),
                  reads=[x_tok, cv], writes=[tmp])
            tk.op(dve, lambda e: e.tensor_tensor(out=yt[:], in0=yt[:], in1=tmp[:], op=ALU.add), reads=[yt, tmp], writes=[yt])
            tk.op(dve, lambda e: e.tensor_tensor(out=yt[:], in0=yt[:], in1=zs[:], op=ALU.mult), reads=[yt, zs], writes=[yt])
            tk.op(act, lambda e: e.activation(out=tmp[:], in_=yt[:], func=AF.Square, accum_out=ss[:]), reads=[yt], writes=[tmp, ss])
            tk.op(act, lambda e: e.activation(out=ss[:], in_=ss[:], func=AF.Sqrt, bias=EPS, scale=1.0 / 2048), reads=[ss], writes=[ss])
            tk.op(dve, lambda e: e.reciprocal(out=ss[:], in_=ss[:]), reads=[ss], writes=[ss])
            tk.op(dve, lambda e: e.scalar_tensor_tensor(out=ya[:], in0=yt[:], scalar=ss[:, 0:1], in1=grow[:], op0=ALU.mult, op1=ALU.mult),
                  reads=[yt, ss, grow], writes=[ya])
            tk.dma(pool, out=yd[rows, 0:2048], in_=ya[:], reads=[ya], writes=[self.db('y', l, gc)])
        tk.barrier()


Prog.stage_B1 = stage_B1


def stage_B2(self, l):
    tk, d, S, NCH = self.tk, self.dram, self.S, self.NCH
    pe, act, dve, pool, sp = tk.pe, tk.act, tk.dve, tk.pool, tk.sp
    cdh = [float(np.exp(128.0 * np.log(1.0 - 2.0 ** (-5.0 - h)))) for h in range(8)]
    with ExitStack() as st:
        rt = [self.sb(st, f'rt{i}', [128, 6144], BF16) for i in range(2)]
        tab = self.sb(st, 'rtab', [128, 2056], F32)
        qT_2 = [self.sb(st, f'qT{i}', [128, 8, 128], BF16) for i in range(2)]
        qTd_2 = [self.sb(st, f'qTd{i}', [128, 8, 128], BF16) for i in range(2)]
        kT_2 = [self.sb(st, f'kT{i}', [128, 8, 128], BF16) for i in range(2)]
        kdk_2 = [self.sb(st, f'kdk{i}', [128, 8, 128], BF16) for i in range(2)]
        ST = [self.sb(st, f'ST{i}', [128, 4, 128], BF16) for i in range(2)]
        osb_2 = [self.sb(st, f'osb{i}', [128, 2048], F32) for i in range(2)]
        junk = self.sb(st, 'junk', [128, 256], F32)
        ss8_2 = [self.sb(st, f'ss8{i}', [128, 8], F32) for i in range(2)]
        Rs = self.sb(st, 'Rs', [128, 2048], F32)
        Rb = self.sb(st, 'Rb', [128, 2048], BF16)
        yb_2 = [self.sb(st, f'yb{i}', [128, 2048], BF16) for i in range(2)]
        psT = self.ps(st, 'psT2', [128, 512], F32)
        psSc = [self.ps(st, f'psSc{i}', [128, 512], F32) for i in range(2)]
        psO = [self.ps(st, f'psO2{i}', [128, 256], F32) for i in range(2)]
        psK = [self.ps(st, f'psK{i}', [128, 256], F32) for i in range(2)]
        tk.dma(sp, out=tab[:], in_=d['ctab'][:, 768:768 + 2056], writes=[tab])
        tk.op(dve, lambda e: e.memset(Rs[:], 0.0), writes=[Rs])
        tk.op(dve, lambda e: e.memset(Rb[:], 0.0), writes=[Rb])
        tmd, yd = d[f'tm{l}'], d[f'y{l}']
        psTb = lambda: psT.t[:].bitcast(BF16)
        for gc in range(NCH):
            rows = slice(gc * 128, (gc + 1) * 128)
            r_ = rt[gc % 2]
            qT = qT_2[gc % 2]
            qTd = qTd_2[gc % 2]
            kT = kT_2[gc % 2]
            kdk = kdk_2[gc % 2]
            osb = osb_2[gc % 2]
            ss8 = ss8_2[gc % 2]
            yb = yb_2[gc % 2]
            tk.dma(sp, out=r_[:], in_=tmd[rows, TM_RQ:TM_DQKV], reads=[self.db('tm', l, gc)], writes=[r_])
            q_tok = lambda h: r_[:, h * 128:(h + 1) * 128]
            k_tok = lambda h: r_[:, 1024 + h * 128:1024 + (h + 1) * 128]
            v_tok = lambda h: r_[:, 2048 + h * 256:2048 + (h + 1) * 256]
            rgs = lambda h: r_[:, 4096 + h * 256:4096 + (h + 1) * 256]
            for h in range(8):
                _tr(tk, psT, h, q_tok(h), [r_], self, last=(h == 7))
            tk.op(act, lambda e: e.activation(out=qT[:].rearrange("p h e -> p (h e)"), in_=psTb(), func=AF.Copy), reads=[psT], writes=[qT])
            tk.op(dve, lambda e: e.tensor_tensor(out=qTd[:].rearrange("p h e -> p (h e)"), in0=qT[:].rearrange("p h e -> p (h e)"), in1=tab[:, 1024:2048], op=ALU.mult),
                  reads=[qT, tab], writes=[qTd])
            for h in range(8):
                _tr(tk, psT, h, k_tok(h), [r_], self, last=(h == 7))
            tk.op(act, lambda e: e.activation(out=kT[:].rearrange("p h e -> p (h e)"), in_=psTb(), func=AF.Copy), reads=[psT], writes=[kT])
            tk.op(dve, lambda e: e.tensor_tensor(out=kdk[:], in0=r_[:, 1024:2048].rearrange("p (h e) -> p h e", h=8),
                                                  in1=tab[:, 2048:2056].unsqueeze(2).to_broadcast([128, 8, 128]), op=ALU.mult),
                  reads=[r_, tab], writes=[kdk])
            for hq in range(2):
                pc, st_ = psSc[hq], ST[hq]
                for i in range(4):
                    h = hq * 4 + i
                    tk.op(pe, lambda e: e.matmul(pc[:, i * 128:(i + 1) * 128], lhsT=kT[:, h, :], rhs=qT[:, h, :], start=True, stop=True),
                          reads=[kT, qT], writes=[pc], inc=(i == 3))
                tk.op(dve, lambda e: e.tensor_tensor(out=st_[:].rearrange("p h e -> p (h e)"), in0=pc[:], in1=tab[:, hq * 512:(hq + 1) * 512], op=ALU.mult),
                      reads=[pc, tab], writes=[st_])
            for h in range(8):
                po, pk = psO[h % 2], psK[h % 2]
                hs = slice(h * 256, (h + 1) * 256)
                tk.op(pe, lambda e: e.matmul(po[:], lhsT=ST[h // 4][:, h % 4, :], rhs=v_tok(h), start=True, stop=False),
                      reads=[ST[h // 4], r_], writes=[po], inc=False)
                tk.op(pe, lambda e: e.matmul(po[:], lhsT=qTd[:, h, :], rhs=Rb[:, hs], start=False, stop=True), reads=[qTd, Rb], writes=[po])
                tk.op(pe, lambda e: e.matmul(pk[:], lhsT=kdk[:, h, :], rhs=v_tok(h), start=True, stop=True), reads=[kdk, r_], writes=[pk])
                tk.op(dve, lambda e: e.tensor_copy(out=osb[:, hs], in_=po[:]), reads=[po], writes=[osb])
                tk.op(act, lambda e: e.activation(out=junk[:], in_=osb[:, hs], func=AF.Square, accum_out=ss8[:, h:h + 1]), reads=[osb], writes=[junk, ss8])
                tk.op(dve, lambda e: e.tensor_scalar(out=Rs[:, hs], in0=Rs[:, hs], scalar1=cdh[h], scalar2=None, op0=ALU.mult), reads=[Rs], writes=[Rs])
                tk.op(dve, lambda e: e.tensor_tensor(out=Rs[:, hs], in0=pk[:], in1=Rs[:, hs], op=ALU.add), reads=[Rs, pk], writes=[Rs])
                tk.op(act, lambda e: e.activation(out=Rb[:, hs], in_=Rs[:, hs], func=AF.Copy), reads=[Rs], writes=[Rb])
            tk.op(act, lambda e: e.activation(out=ss8[:], in_=ss8[:], func=AF.Sqrt, bias=EPS, scale=1.0 / 256), reads=[ss8], writes=[ss8])
            tk.op(dve, lambda e: e.reciprocal(out=ss8[:], in_=ss8[:]), reads=[ss8], writes=[ss8])
            for h in range(8):
                hs = slice(h * 256, (h + 1) * 256)
                tk.op(dve, lambda e: e.scalar_tensor_tensor(out=yb[:, hs], in0=osb[:, hs], scalar=ss8[:, h:h + 1], in1=rgs(h), op0=ALU.mult, op1=ALU.mult),
                      reads=[osb, ss8, r_], writes=[yb])
            tk.dma(pool, out=yd[rows, 2048:4096], in_=yb[:], reads=[yb], writes=[self.db('y', l, gc)])
        tk.barrier()


def stage_B3(self, l):
    tk, d, S, NCH = self.tk, self.dram, self.S, self.NCH
    pe, act, dve, pool, sp = tk.pe, tk.act, tk.dve, tk.pool, tk.sp
    DIL = (1, 4, 16)
    with ExitStack() as st:
        qtok_2 = [self.sb(st, f'qtok{i}', [128, NCH, 128], BF16) for i in range(2)]
        ktok_2 = [self.sb(st, f'ktok{i}', [128, NCH, 128], BF16) for i in range(2)]
        qT_2 = [self.sb(st, f'dqT{i}', [128, S], BF16) for i in range(2)]
        kT_2 = [self.sb(st, f'dkT{i}', [128, S], BF16) for i in range(2)]
        vaug = [self.sb(st, f'vaug{i}', [128, NCH, 130], BF16) for i in range(2)]
        pT = [self.sb(st, f'pT{i}', [128, 256], BF16) for i in range(3)]
        osb = [self.sb(st, f'dosb{i}', [128, NCH, 129], F32) for i in range(2)]
        psT = [self.ps(st, f'psT3{i}', [128, 512], F32) for i in range(2)]
        psS = [self.ps(st, f'psS3{i}', [128, 512], F32) for i in range(3)]
        psO = [self.ps(st, f'psO3{i}', [128, 512], F32) for i in range(3)]
        for i in range(2):
            tk.op(dve, lambda e: e.memset(vaug[i][:], 1.0), writes=[vaug[i]])
        tmd, odd = d[f'tm{l}'], d[f'od{l}']
        alltm = [self.db('tm', l, gc) for gc in range(NCH)]
        tmv = tmd.rearrange("(c p) w -> p c w", p=128)
        it = 0
        nb = 0
        for g in range(3):
            Dl = DIL[g]
            nsp = S // (128 * Dl)
            tmr = tmd.rearrange("(m i r) w -> i m r w", i=128, r=Dl)
            for h in range(8):
                cq = TM_DQKV + g * 3072 + h * 128
                va, ob_ = vaug[it % 2], osb[it % 2]
                qtok, ktok, qT, kT = qtok_2[it % 2], ktok_2[it % 2], qT_2[it % 2], kT_2[it % 2]
                it += 1
                tk.dma(sp, out=qtok[:], in_=tmv[:, :, cq:cq + 128], reads=alltm, writes=[qtok])
                tk.dma(sp, out=ktok[:], in_=tmv[:, :, cq + 1024:cq + 1152], reads=alltm, writes=[ktok])
                rn = min(Dl, 8)
                for m in range(nsp):
                    for r0 in range(0, Dl, rn):
                        b0 = m * Dl + r0
                        tk.dma(sp, out=va[:, b0:b0 + rn, 0:128], in_=tmr[:, m, r0:r0 + rn, cq + 2048:cq + 2176], reads=alltm, writes=[va])
                for src, dst in ((qtok, qT), (ktok, kT)):
                    for c8 in range(NCH // 8):
                        p_ = psT[c8 % 2]
                        for i in range(8):
                            _tr(tk, p_, i, src[:, c8 * 8 + i, :], [src], self, last=(i == 7))
                        eng = act
                        if eng is act:
                            tk.op(act, lambda e: e.activation(out=dst[:, c8 * 1024:(c8 + 1) * 1024], in_=p_.t[:].bitcast(BF16), func=AF.Copy),
                                  reads=[p_], writes=[dst])
                        else:
                            tk.op(dve, lambda e: e.tensor_copy(out=dst[:, c8 * 1024:(c8 + 1) * 1024], in_=p_.t[:].bitcast(BF16)), reads=[p_], writes=[dst])
                for m in range(nsp):
                    for r in range(Dl):
                        bi = m * Dl + r
                        cur = slice(m * 128 * Dl + r, m * 128 * Dl + r + 127 * Dl + 1, Dl)
                        ps_, p_, po = psS[nb % 3], pT[nb % 3], psO[nb % 3]
                        nb += 1
                        lo = 0 if m > 0 else 128
                        if m > 0:
                            prv = slice((m - 1) * 128 * Dl + r, (m - 1) * 128 * Dl + r + 127 * Dl + 1, Dl)
                            tk.op(pe, lambda e: e.matmul(ps_[:, 0:128], lhsT=kT[:, prv], rhs=qT[:, cur], start=True, stop=False),
                                  reads=[kT, qT], writes=[ps_], inc=False)
                            tk.op(pe, lambda e: e.matmul(ps_[:, 0:128], lhsT=self.ident_b(), rhs=self.negprev_b(), start=False, stop=True),
                                  reads=[self.cbf], writes=[ps_], inc=False)
                        tk.op(pe, lambda e: e.matmul(ps_[:, 128:256], lhsT=kT[:, cur], rhs=qT[:, cur], start=True, stop=False),
                              reads=[kT, qT], writes=[ps_], inc=False)
                        tk.op(pe, lambda e: e.matmul(ps_[:, 128:256], lhsT=self.ident_b(), rhs=self.negcur_b(), start=False, stop=True),
                              reads=[self.cbf], writes=[ps_])
                        tk.op(act, lambda e: e.activation(out=p_[:, lo:256], in_=ps_[:, lo:256], func=AF.Exp), reads=[ps_], writes=[p_])
                        if m > 0:
                            tk.op(pe, lambda e: e.matmul(po[:, 0:129], lhsT=p_[:, 0:128], rhs=va[:, bi - Dl, 0:129], start=True, stop=False),
                                  reads=[p_, va], writes=[po], inc=False)
                        tk.op(pe, lambda e: e.matmul(po[:, 0:129], lhsT=p_[:, 128:256], rhs=va[:, bi, 0:129], start=(m == 0), stop=True),
                              reads=[p_, va], writes=[po])
                        tk.op(dve, lambda e: e.tensor_copy(out=ob_[:, bi, :], in_=po[:, 0:129]), reads=[po], writes=[ob_])
                odr = odd[g].rearrange("(m i r) h e -> i m r h e", i=128, r=Dl)
                for m in range(nsp):
                    for r0 in range(0, Dl, rn):
                        b0 = m * Dl + r0
                        tk.dma(pool, out=odr[:, m, r0:r0 + rn, h, :], in_=ob_[:, b0:b0 + rn, :], reads=[ob_], writes=[self.db('od', l)])
        tk.barrier()
        with ExitStack() as st2:
            o3 = [self.sb(st2, f'o3{i}', [128, 3, 8 * 129], F32) for i in range(2)]
            dgs = [self.sb(st2, f'dgs{i}', [128, 1024], BF16) for i in range(2)]
            rden = self.sb(st2, 'rden', [128, 8], F32)
            sm = self.sb(st2, 'sm3', [128, 8 * 129], F32)
            yn = self.sb(st2, 'yn3', [128, 8, 128], F32)
            yc = [self.sb(st2, f'yc{i}', [128, 1024], BF16) for i in range(2)]
            yd = d[f'y{l}']
            for gc in range(NCH):
                rows = slice(gc * 128, (gc + 1) * 128)
                o_, g_, y_ = o3[gc % 2], dgs[gc % 2], yc[gc % 2]
                tk.dma(sp, out=o_[:], in_=odd[:, rows, :, :].rearrange("g p h e -> p g (h e)"), reads=[self.db('od', l)], writes=[o_])
                tk.dma(sp, out=g_[:], in_=tmd[rows, TM_DG:TM_DG + 1024], reads=[self.db('tm', l, gc)], writes=[g_])
                tk.op(dve, lambda e: e.tensor_tensor(out=sm[:], in0=o_[:, 0, :], in1=o_[:, 1, :], op=ALU.add), reads=[o_], writes=[sm])
                tk.op(dve, lambda e: e.tensor_tensor(out=sm[:], in0=sm[:], in1=o_[:, 2, :], op=ALU.add), reads=[o_, sm], writes=[sm])
                sv = sm[:].rearrange("p (h e) -> p h e", h=8)
                tk.op(dve, lambda e: e.tensor_copy(out=rden[:].unsqueeze(2), in_=sv[:, :, 128:129]), reads=[sm], writes=[rden])
                tk.op(dve, lambda e: e.reciprocal(out=rden[:], in_=rden[:]), reads=[rden], writes=[rden])
                tk.op(dve, lambda e: e.tensor_tensor(out=yn[:], in0=sv[:, :, 0:128], in1=rden[:].unsqueeze(2).to_broadcast([128, 8, 128]), op=ALU.mult),
                      reads=[sm, rden], writes=[yn])
                if self.debug:
                    tk.dma(pool, out=d['dbgA'][rows, :], in_=sm[:], reads=[sm], writes=[self.db('dbg')])
                    tk.dma(pool, out=d['dbgB'][rows, :], in_=yn[:].rearrange("p h e -> p (h e)"), reads=[yn], writes=[self.db('dbg')])
                tk.op(dve, lambda e: e.tensor_tensor(out=y_[:].rearrange("p (h e) -> p h e", h=8), in0=yn[:],
                                                      in1=g_[:].rearrange("p (h e) -> p h e", h=8), op=ALU.mult), reads=[yn, g_], writes=[y_])
                tk.dma(pool, out=yd[rows, 4096:5120], in_=y_[:], reads=[y_], writes=[self.db('y', l, gc)])
            tk.barrier()


Prog.stage_B2 = stage_B2
Prog.stage_B3 = stage_B3


def stage_C(self, l, xin, xkey):
    tk, d, S, NCH = self.tk, self.dram, self.S, self.NCH
    pe, act, dve, pool, sp = tk.pe, tk.act, tk.dve, tk.pool, tk.sp
    NT = S // 512
    with ExitStack() as st:
        ytok = [self.sb(st, f'ytok{i}', [128, 5120], BF16) for i in range(2)]
        yT = self.sb(st, 'yT', [128, 40, 512], BF16)
        wa = [self.sb(st, f'wa{i}', [128, 40, 128], BF16) for i in range(2)]
        wo = [self.sb(st, f'wo{i}', [128, 16, 128], BF16) for i in range(2)]
        gt = [self.sb(st, f'gt{i}', [128, 3, 512], BF16) for i in range(2)]
        mT = self.sb(st, 'mT', [128, 16, 512], BF16)
        mf = [self.sb(st, f'mf{i}', [128, 512], F32) for i in range(2)]
        mg = [self.sb(st, f'mg{i}', [128, 512], F32) for i in range(2)]
        xt = [self.sb(st, f'xt{i}', [128, 512], F32) for i in range(2)]
        psT = [self.ps(st, f'psTc{i}', [128, 512], F32) for i in range(2)]
        psP = [self.ps(st, f'psP{i}', [128, 512], F32) for i in range(3)]
        psX = [self.ps(st, f'psX{i}', [128, 512], F32) for i in range(2)]
        yd, gtd, xnd = d[f'y{l}'], d[f'gT{l}'], d[f'xn{l}']
        wob = d[f'wob{l}'].rearrange("k p (j c) -> j p k c", c=128)
        gview = gtd.rearrange("(b j) p s -> j p b s", b=3)
        allwob = [self.db('wob', l, b) for b in range(56)]
        nt = 0
        for tt in range(NT):
            tok0 = tt * 512
            for c4 in range(4):
                gc = tt * 4 + c4
                y_ = ytok[gc % 2]
                tk.dma(sp, out=y_[:], in_=yd[gc * 128:(gc + 1) * 128, :], reads=[self.db('y', l, gc)], writes=[y_])
                for t8 in range(5):
                    p_ = psT[nt % 2]
                    nt += 1
                    for i in range(8):
                        _tr(tk, p_, i, y_[:, (t8 * 8 + i) * 128:(t8 * 8 + i + 1) * 128], [y_], self, last=(i == 7))
                    tk.op(act, lambda e: e.activation(out=yT[:, t8 * 8:(t8 + 1) * 8, c4 * 128:(c4 + 1) * 128],
                                                      in_=p_.t[:].bitcast(BF16).rearrange("p (t e) -> p t e", t=8), func=AF.Copy), reads=[p_], writes=[yT])
            for jj in range(16):
                w_, g_ = wa[jj % 2], gt[jj % 2]
                tk.dma(sp, out=w_[:], in_=wob[jj, :, 0:40, :], reads=allwob, writes=[w_])
                tk.dma(sp, out=g_[:], in_=gview[jj, :, :, tok0:tok0 + 512], reads=[self.db('gT', l, tt)], writes=[g_])
                for br, (k0, nk) in enumerate(((0, 16), (16, 16), (32, 8))):
                    for k in range(nk):
                        tk.op(pe, lambda e: e.matmul(psP[br][:], lhsT=w_[:, k0 + k, :], rhs=yT[:, k0 + k, :], start=(k == 0), stop=(k == nk - 1)),
                              reads=[w_, yT], writes=[psP[br]], inc=(k == nk - 1))
                f_, h_ = mf[jj % 2], mg[jj % 2]
                tk.op(dve, lambda e: e.tensor_tensor(out=f_[:], in0=psP[0][:], in1=g_[:, 0, :], op=ALU.mult), reads=[psP[0], g_], writes=[f_])
                tk.op(dve, lambda e: e.tensor_tensor(out=h_[:], in0=psP[1][:], in1=g_[:, 1, :], op=ALU.mult), reads=[psP[1], g_], writes=[h_])
                tk.op(dve, lambda e: e.tensor_tensor(out=f_[:], in0=f_[:], in1=h_[:], op=ALU.add), reads=[f_, h_], writes=[f_])
                tk.op(dve, lambda e: e.tensor_tensor(out=h_[:], in0=psP[2][:], in1=g_[:, 2, :], op=ALU.mult), reads=[psP[2], g_], writes=[h_])
                tk.op(dve, lambda e: e.tensor_tensor(out=mT[:, jj, :], in0=f_[:], in1=h_[:], op=ALU.add), reads=[f_, h_], writes=[mT])
            for jj in range(16):
                w_, x_, px = wo[jj % 2], xt[jj % 2], psX[jj % 2]
                tk.dma(sp, out=w_[:], in_=wob[jj, :, 40:56, :], reads=allwob, writes=[w_])
                tk.dma(sp, out=x_[:], in_=xin[jj * 128:(jj + 1) * 128, tok0:tok0 + 512], reads=[self.db(*xkey, tt)], writes=[x_])
                for k in range(16):
                    tk.op(pe, lambda e: e.matmul(px[:], lhsT=w_[:, k, :], rhs=mT[:, k, :], start=(k == 0), stop=(k == 15)),
                          reads=[w_, mT], writes=[px], inc=(k == 15))
                tk.op(dve, lambda e: e.tensor_tensor(out=x_[:], in0=px[:], in1=x_[:], op=ALU.add), reads=[px, x_], writes=[x_])
                tk.dma(pool, out=xnd[jj * 128:(jj + 1) * 128, tok0:tok0 + 512], in_=x_[:], reads=[x_], writes=[self.db('xn', l, tt)])
        tk.barrier()


def stage_final(self):
    tk, d, S, NCH = self.tk, self.dram, self.S, self.NCH
    pe, act, dve, pool, sp = tk.pe, tk.act, tk.dve, tk.pool, tk.sp
    l = self.depth - 1
    with ExitStack() as st:
        xs = [self.sb(st, f'fx{i}', [128, 16, 128], F32) for i in range(2)]
        sq = self.sb(st, 'fsq', [128, 16, 128], F32)
        rstd = self.sb(st, 'frstd', [128, 128], F32)
        gc_ = self.sb(st, 'fg', [128, 16], F32)
        pss = self.ps(st, 'fpss', [128, 128], F32)
        tk.dma(sp, out=gc_[:], in_=d['fgc'], writes=[gc_])
        xview = d[f'xn{l}'].rearrange("(k p) s -> p k s", p=128)
        oview = d['outT'].rearrange("(k p) s -> p k s", p=128)
        for gc in range(NCH):
            x_ = xs[gc % 2]
            cs = slice(gc * 128, (gc + 1) * 128)
            tk.dma(sp, out=x_[:], in_=xview[:, :, cs], reads=[self.db('xn', l, gc // 4)], writes=[x_])
            tk.op(act, lambda e: e.activation(out=sq[:], in_=x_[:], func=AF.Square), reads=[x_], writes=[sq])
            for k in range(16):
                tk.op(pe, lambda e: e.matmul(pss[:], lhsT=self.ones_f(), rhs=sq[:, k, :], start=(k == 0), stop=(k == 15)),
                      reads=[sq, self.c32], writes=[pss], inc=(k == 15))
            tk.op(act, lambda e: e.activation(out=rstd[:], in_=pss[:], func=AF.Sqrt, bias=EPS, scale=1.0 / D), reads=[pss], writes=[rstd])
            tk.op(dve, lambda e: e.reciprocal(out=rstd[:], in_=rstd[:]), reads=[rstd], writes=[rstd])
            for k in range(16):
                tk.op(dve, lambda e: e.scalar_tensor_tensor(out=x_[:, k, :], in0=x_[:, k, :], scalar=gc_[:, k:k + 1], in1=rstd[:],
                                                            op0=ALU.mult, op1=ALU.mult), reads=[x_, gc_, rstd], writes=[x_])
            tk.dma(pool, out=oview[:, :, cs], in_=x_[:], reads=[x_], writes=[self.db('out')])
        tk.barrier()


Prog.stage_C = stage_C
Prog.stage_final = stage_final
```
